# Optimizing a Trainium2 kernel written in Bass

```python
import math
import jax, jax.numpy as jnp
from jax import lax
import numpy as np

D_MODEL = 1024
BATCH = 8
SEQ = 2048
DEPTH = 4

HEAD_DIM = 64
D_MIX = D_MODEL
A_HEADS = (3 * D_MIX) // (8 * HEAD_DIM)
A_WIDTH = A_HEADS * HEAD_DIM
IDX_HEADS = 4
IDX_DIM = HEAD_DIM
DSA_TOPK_MAX = 256
B_HEADS = D_MIX // (4 * HEAD_DIM)
B_QK_DIM = HEAD_DIM // 2
B_WIDTH = B_HEADS * HEAD_DIM
SUBLN_EPS = 1e-5
C_HEADS = A_HEADS
C_WIDTH = C_HEADS * HEAD_DIM
MOBA_BLOCK = 256
MOBA_TOPK = 3
ROPE_THETA = 500000.0
ROPE_FRACTION = 4
MAX_POS_OFFSET = 1024
Q_BLOCK = 128
MOBA_Q_BLOCK = 32
D_FF = 2816
N_EXPERTS = 8
TOP_K_EXPERTS = 2
D_FF_EXPERT = 2816
EPS = 1e-6
IN_COLS = (A_WIDTH, HEAD_DIM, HEAD_DIM, IDX_HEADS * IDX_DIM, IDX_DIM, IDX_HEADS,
           2 * B_HEADS * B_QK_DIM, 2 * B_HEADS * B_QK_DIM, B_WIDTH,
           C_WIDTH, C_WIDTH, C_WIDTH)
D_IN = sum(IN_COLS)

kernel_name = 'hybrid_dsa_diff_moba_moe_adaln'

F32 = jnp.float32


def _split_points():
    pts, acc = [], 0
    for w in IN_COLS[:-1]:
        acc += w
        pts.append(acc)
    return pts


def rms_norm(x, g, eps=EPS):
    xf = x.astype(F32)
    y = xf * lax.rsqrt(jnp.mean(xf * xf, axis=-1, keepdims=True) + eps)
    return (y * g.astype(F32)).astype(x.dtype)


def rope_tables(positions, dim):
    rot = dim // ROPE_FRACTION
    inv = 1.0 / (ROPE_THETA ** (np.arange(0, rot, 2, dtype=np.float32) / rot))
    ang = positions.astype(F32)[..., None] * jnp.asarray(inv, F32)
    return jnp.cos(ang), jnp.sin(ang)


def partial_rope(x, cos, sin):
    half = cos.shape[-1]
    xf = x.astype(F32)
    x1, x2, xp = xf[..., :half], xf[..., half:2 * half], xf[..., 2 * half:]
    c, s = cos[:, :, None, :], sin[:, :, None, :]
    return jnp.concatenate([x1 * c - x2 * s, x2 * c + x1 * s, xp], axis=-1).astype(x.dtype)


def dsa_attention(q, k, v, q_idx, k_idx, w_idx):
    B, S, H, dh = q.shape
    k_top = min(DSA_TOPK_MAX, S // 4)
    key_pos = jnp.arange(S)
    kf_idx = k_idx.astype(F32)
    gather = jax.vmap(lambda zz, ii: zz[ii])

    def one_block(i):
        t0 = i * Q_BLOCK
        t = t0 + jnp.arange(Q_BLOCK)
        qb = lax.dynamic_slice_in_dim(q, t0, Q_BLOCK, axis=1).astype(F32)
        qib = lax.dynamic_slice_in_dim(q_idx, t0, Q_BLOCK, axis=1).astype(F32)
        wb = lax.dynamic_slice_in_dim(w_idx, t0, Q_BLOCK, axis=1).astype(F32) * IDX_HEADS ** -0.5
        rel = jax.nn.relu(jnp.einsum('bqhd,bsd->bqhs', qib, kf_idx) * IDX_DIM ** -0.5)
        score_idx = jnp.einsum('bqh,bqhs->bqs', wb, rel)
        causal = key_pos[None, :] <= t[:, None]
        score_idx = jnp.where(causal[None], score_idx, -jnp.inf)
        _, sel = lax.top_k(score_idx, k_top)
        valid = sel <= t[None, :, None]
        kg = gather(k, sel).astype(F32)
        vg = gather(v, sel).astype(F32)
        s = jnp.einsum('bqhd,bqkd->bqhk', qb, kg) * dh ** -0.5
        s = jnp.where(valid[:, :, None, :], s, -jnp.inf)
        p = jax.nn.softmax(s, axis=-1)
        return jnp.einsum('bqhk,bqkd->bqhd', p, vg).astype(q.dtype)

    out = lax.map(one_block, jnp.arange(S // Q_BLOCK))
    return jnp.moveaxis(out, 0, 1).reshape(B, S, H * dh)


def diff_attention(q, k, v, lam, g_sub, lam_init):
    B, S, H, _, dq = q.shape
    dv = v.shape[-1]
    key_pos = jnp.arange(S)
    kf = k.astype(F32)
    vf = v.astype(F32)

    def one_block(i):
        t0 = i * Q_BLOCK
        t = t0 + jnp.arange(Q_BLOCK)
        qb = lax.dynamic_slice_in_dim(q, t0, Q_BLOCK, axis=1).astype(F32)
        s = jnp.einsum('bqhcd,bshcd->bhcqs', qb, kf) * dq ** -0.5
        causal = key_pos[None, :] <= t[:, None]
        p = jax.nn.softmax(jnp.where(causal, s, -jnp.inf), axis=-1)
        a = p[:, :, 0] - lam * p[:, :, 1]
        return jnp.einsum('bhqs,bshd->bqhd', a, vf)

    o = lax.map(one_block, jnp.arange(S // Q_BLOCK))
    o = jnp.moveaxis(o, 0, 1).reshape(B, S, H, dv)
    o = rms_norm(o, g_sub, SUBLN_EPS) * (1.0 - lam_init)
    return o.reshape(B, S, H * dv).astype(v.dtype)


def moba_attention(q, k, v):
    B, S, H, dh = q.shape
    nb = -(-S // MOBA_BLOCK)
    n_sel = min(MOBA_TOPK, nb - 1)
    pad = nb * MOBA_BLOCK - S

    def to_blocks(z):
        z = jnp.pad(z, ((0, 0), (0, pad), (0, 0), (0, 0)))
        return z.reshape(B, nb, MOBA_BLOCK, H, dh).transpose(0, 3, 1, 2, 4)

    kb = to_blocks(k)
    vb = to_blocks(v)
    k_mean = jnp.mean(kb.astype(F32), axis=3)
    qh = q.transpose(0, 2, 1, 3)
    scale = dh ** -0.5
    in_block = jnp.arange(MOBA_BLOCK)
    gather = jax.vmap(jax.vmap(lambda zz, ii: zz[ii]))

    def one_block(i):
        t0 = i * MOBA_Q_BLOCK
        t = t0 + jnp.arange(MOBA_Q_BLOCK)
        own = t0 // MOBA_BLOCK
        qb = lax.dynamic_slice_in_dim(qh, t0, MOBA_Q_BLOCK, axis=2).astype(F32)
        k_own = lax.dynamic_index_in_dim(kb, own, axis=2, keepdims=False).astype(F32)
        v_own = lax.dynamic_index_in_dim(vb, own, axis=2, keepdims=False).astype(F32)
        s_own = jnp.einsum('bhqd,bhkd->bhqk', qb, k_own) * scale
        s_own = jnp.where((own * MOBA_BLOCK + in_block)[None, :] <= t[:, None], s_own, -jnp.inf)
        if n_sel == 0:
            p_own = jax.nn.softmax(s_own, axis=-1)
            return jnp.einsum('bhqk,bhkd->bhqd', p_own, v_own).astype(q.dtype)
        n_past = t // MOBA_BLOCK
        gate = jnp.einsum('bhqd,bhnd->bhqn', qb, k_mean)
        gate = jnp.where(jnp.arange(nb)[None, :] < n_past[:, None], gate, -jnp.inf)
        _, sel = lax.top_k(gate, n_sel)
        kg = gather(kb, sel).astype(F32)
        vg = gather(vb, sel).astype(F32)
        s_sel = jnp.einsum('bhqd,bhqnkd->bhqnk', qb, kg) * scale
        sel_ok = jnp.arange(n_sel)[None, :] < n_past[:, None]
        s_sel = jnp.where(sel_ok[None, None, :, :, None], s_sel, -jnp.inf)
        n_keys = n_sel * MOBA_BLOCK
        s_all = jnp.concatenate([s_sel.reshape(B, H, MOBA_Q_BLOCK, n_keys), s_own], axis=-1)
        p = jax.nn.softmax(s_all, axis=-1)
        p_sel = p[..., :n_keys].reshape(B, H, MOBA_Q_BLOCK, n_sel, MOBA_BLOCK)
        p_own = p[..., n_keys:]
        o = jnp.einsum('bhqnk,bhqnkd->bhqd', p_sel, vg) + jnp.einsum('bhqk,bhkd->bhqd', p_own, v_own)
        return o.astype(q.dtype)

    out = lax.map(one_block, jnp.arange(S // MOBA_Q_BLOCK))
    return out.transpose(1, 0, 3, 2, 4).reshape(B, S, H * dh)


def hybrid_mixer(h, cos64, sin64, cos32, sin32, w_in, w_out, lam_vec, g_sub, lam_init):
    B, S, _ = h.shape
    proj = jnp.einsum('bsd,dn->bsn', h, w_in)
    (q_a, k_a, v_a, q_i, k_i, w_i, q_b, k_b, v_b, q_c, k_c, v_c) = jnp.split(proj, _split_points(), axis=-1)
    q_a = partial_rope(q_a.reshape(B, S, A_HEADS, HEAD_DIM), cos64, sin64)
    k_a = partial_rope(k_a.reshape(B, S, 1, HEAD_DIM), cos64, sin64)[:, :, 0]
    q_i = partial_rope(q_i.reshape(B, S, IDX_HEADS, IDX_DIM), cos64, sin64)
    k_i = partial_rope(k_i.reshape(B, S, 1, IDX_DIM), cos64, sin64)[:, :, 0]
    y_a = dsa_attention(q_a, k_a, v_a, q_i, k_i, w_i)
    q_b = partial_rope(q_b.reshape(B, S, 2 * B_HEADS, B_QK_DIM), cos32, sin32).reshape(B, S, B_HEADS, 2, B_QK_DIM)
    k_b = partial_rope(k_b.reshape(B, S, 2 * B_HEADS, B_QK_DIM), cos32, sin32).reshape(B, S, B_HEADS, 2, B_QK_DIM)
    v_b = v_b.reshape(B, S, B_HEADS, HEAD_DIM)
    lf = lam_vec.astype(F32)
    lam = jnp.exp(jnp.sum(lf[0] * lf[1])) - jnp.exp(jnp.sum(lf[2] * lf[3])) + lam_init
    y_b = diff_attention(q_b, k_b, v_b, lam, g_sub, lam_init)
    q_c = partial_rope(q_c.reshape(B, S, C_HEADS, HEAD_DIM), cos64, sin64)
    k_c = partial_rope(k_c.reshape(B, S, C_HEADS, HEAD_DIM), cos64, sin64)
    y_c = moba_attention(q_c, k_c, v_c.reshape(B, S, C_HEADS, HEAD_DIM))
    y = jnp.concatenate([y_a, y_b, y_c], axis=-1)
    return jnp.einsum('bsm,md->bsd', y, w_out)


def swiglu(h, w_gate, w_up, w_down):
    a = jnp.einsum('bsd,df->bsf', h, w_gate)
    u = jnp.einsum('bsd,df->bsf', h, w_up)
    return jnp.einsum('bsf,fd->bsd', jax.nn.silu(a) * u, w_down)


def moe_swiglu(h, w_router, w_gate, w_up, w_down):
    logits = jnp.einsum('bsd,de->bse', h.astype(F32), w_router.astype(F32))
    top_val, top_idx = lax.top_k(logits, TOP_K_EXPERTS)
    top_w = jax.nn.softmax(top_val, axis=-1)
    combine = jnp.sum(jax.nn.one_hot(top_idx, N_EXPERTS, dtype=F32) * top_w[..., None], axis=-2)
    out = jnp.zeros_like(h)
    for e in range(N_EXPERTS):
        out = out + combine[..., e:e + 1].astype(h.dtype) * swiglu(h, w_gate[e], w_up[e], w_down[e])
    return out


def setup_inputs(seed: int = 0) -> dict:
    key = jax.random.key(seed)
    ks = jax.random.split(key, 20)
    n_dense = (DEPTH + 1) // 2
    n_moe = DEPTH // 2

    def nrm(k, shape, scale):
        return jax.random.normal(k, shape, F32) * scale

    x = nrm(ks[0], (BATCH, SEQ, D_MODEL), 1.0)
    c = nrm(ks[1], (BATCH, D_MODEL), 1.0)
    offset = jax.random.randint(ks[2], (BATCH, 1), 0, MAX_POS_OFFSET, dtype=jnp.int32)
    positions = offset + jnp.arange(SEQ, dtype=jnp.int32)[None, :]
    w_in = nrm(ks[3], (DEPTH, D_MODEL, D_IN), D_MODEL ** -0.5)
    w_out = nrm(ks[4], (DEPTH, D_MIX, D_MODEL), D_MIX ** -0.5)
    diff_lambda = nrm(ks[5], (DEPTH, 4, B_QK_DIM), 0.1)
    diff_subln = 1.0 + nrm(ks[6], (DEPTH, HEAD_DIM), 0.01)
    w_ada = nrm(ks[7], (DEPTH, D_MODEL, 6 * D_MODEL), 0.5 * D_MODEL ** -0.5)
    b_ada = nrm(ks[8], (DEPTH, 6 * D_MODEL), 0.01)
    g_attn = 1.0 + nrm(ks[9], (DEPTH, D_MODEL), 0.01)
    g_ffn = 1.0 + nrm(ks[10], (DEPTH, D_MODEL), 0.01)
    w_ff_gate = nrm(ks[11], (n_dense, D_MODEL, D_FF), D_MODEL ** -0.5)
    w_ff_up = nrm(ks[12], (n_dense, D_MODEL, D_FF), D_MODEL ** -0.5)
    w_ff_down = nrm(ks[13], (n_dense, D_FF, D_MODEL), D_FF ** -0.5)
    w_router = nrm(ks[14], (n_moe, D_MODEL, N_EXPERTS), D_MODEL ** -0.5)
    w_exp_gate = nrm(ks[15], (n_moe, N_EXPERTS, D_MODEL, D_FF_EXPERT), D_MODEL ** -0.5)
    w_exp_up = nrm(ks[16], (n_moe, N_EXPERTS, D_MODEL, D_FF_EXPERT), D_MODEL ** -0.5)
    w_exp_down = nrm(ks[17], (n_moe, N_EXPERTS, D_FF_EXPERT, D_MODEL), D_FF_EXPERT ** -0.5)
    g_final = 1.0 + nrm(ks[18], (D_MODEL,), 0.01)
    return {'x': x, 'c': c, 'positions': positions, 'w_in': w_in, 'w_out': w_out,
            'diff_lambda': diff_lambda, 'diff_subln': diff_subln, 'w_ada': w_ada, 'b_ada': b_ada,
            'g_attn': g_attn, 'g_ffn': g_ffn, 'w_ff_gate': w_ff_gate, 'w_ff_up': w_ff_up,
            'w_ff_down': w_ff_down, 'w_router': w_router, 'w_exp_gate': w_exp_gate,
            'w_exp_up': w_exp_up, 'w_exp_down': w_exp_down, 'g_final': g_final}


def reference(x, c, positions, w_in, w_out, diff_lambda, diff_subln, w_ada, b_ada,
              g_attn, g_ffn, w_ff_gate, w_ff_up, w_ff_down, w_router, w_exp_gate,
              w_exp_up, w_exp_down, g_final):
    cos64, sin64 = rope_tables(positions, HEAD_DIM)
    cos32, sin32 = rope_tables(positions, B_QK_DIM)
    c_act = jax.nn.silu(c)
    for layer in range(DEPTH):
        mod = jnp.einsum('bd,dn->bn', c_act, w_ada[layer]) + b_ada[layer]
        sh1, sc1, gt1, sh2, sc2, gt2 = [m[:, None, :] for m in jnp.split(mod, 6, axis=-1)]
        lam_init = 0.8 - 0.6 * math.exp(-0.3 * layer)
        h = rms_norm(x, g_attn[layer]) * (1.0 + sc1) + sh1
        y = hybrid_mixer(h, cos64, sin64, cos32, sin32, w_in[layer], w_out[layer],
                         diff_lambda[layer], diff_subln[layer], lam_init)
        x = x + gt1 * y
        h = rms_norm(x, g_ffn[layer]) * (1.0 + sc2) + sh2
        j = layer // 2
        if layer % 2 == 0:
            f = swiglu(h, w_ff_gate[j], w_ff_up[j], w_ff_down[j])
        else:
            f = moe_swiglu(h, w_router[j], w_exp_gate[j], w_exp_up[j], w_exp_down[j])
        x = x + gt2 * f
    return rms_norm(x, g_final)
```

```python
import math
import numpy as np
from contextlib import ExitStack
import concourse.bass as bass
import concourse.mybir as mybir
from concourse.bass_utils import run_bass_kernel_spmd

F32 = mybir.dt.float32
BF16 = mybir.dt.bfloat16
I32 = mybir.dt.int32
ALU = mybir.AluOpType
AF = mybir.ActivationFunctionType

L = 4
D = 1024
SEQ = 2048
NT = 16
NCH = 4
KC = 8
DFF = 2816
NF = 22
NEXP = 8
THETA = 500000.0
NEG_BIG = -1.0e30
REPL = -1.0e20
MB = 30000.0
NBIS = 15
PI = math.pi
COMPUTE = ("tensor", "vector", "scalar", "gpsimd")

O_QA, O_KA, O_VA, O_QI, O_KI, O_WI, O_QB, O_KB, O_VB, O_QC, O_KC, O_VC = (
    0, 384, 448, 512, 768, 832, 836, 1092, 1348, 1604, 1988, 2372)


class Buf:
    __slots__ = ("w", "r", "psum")

    def __init__(self):
        self.w = None
        self.r = {}
        self.psum = False


class TB:
    def __init__(self, t):
        self.t = t
        self.bufs = {}

    def b(self, *key):
        v = self.bufs.get(key)
        if v is None:
            v = self.bufs[key] = Buf()
        return v


class Sched:
    def __init__(self, nc, stack, n_dma_sems=8):
        self.nc = nc
        self.E = {"tensor": nc.tensor, "vector": nc.vector, "scalar": nc.scalar,
                  "gpsimd": nc.gpsimd, "sync": nc.sync}
        self.sem = {}
        self.count = {}
        self.waited = {}
        for e in COMPUTE:
            self.sem[e] = stack.enter_context(nc.semaphore("s_" + e))
            self.count[e] = 0
        self.dmaq = {"dsync": "sync", "dpool": "gpsimd", "dact": "scalar"}
        self.dsems = {}
        self.dstate = {}
        for q in self.dmaq:
            self.dsems[q] = [stack.enter_context(nc.semaphore(f"d_{q}_{i}")) for i in range(n_dma_sems)]
            self.dstate[q] = {"n": 0, "cnt": [0] * n_dma_sems}
        self.ninstr = 0
        self.pe_mode = None

    def _semh(self, k):
        if isinstance(k, str):
            return self.sem[k]
        return self.dsems[k[0]][k[1]]

    def _deps(self, reads, writes):
        deps = {}
        for b in reads:
            if b.w is not None:
                k, v = b.w
                if deps.get(k, -1) < v:
                    deps[k] = v
            if b.psum:
                for k, v in b.r.items():
                    if deps.get(k, -1) < v:
                        deps[k] = v
        for b in writes:
            if b.w is not None:
                k, v = b.w
                if deps.get(k, -1) < v:
                    deps[k] = v
            for k, v in b.r.items():
                if deps.get(k, -1) < v:
                    deps[k] = v
        return deps

    def _waits(self, stream, deps, pe_drain=False):
        e = self.E[stream]
        for k, v in deps.items():
            if k == "tensor" and stream == "tensor" and not pe_drain:
                continue
            wk = (stream, k)
            if self.waited.get(wk, -1) >= v:
                continue
            self.waited[wk] = v
            e.wait_ge(self._semh(k), v)

    def op(self, eng, fn, reads=(), writes=(), mode=None):
        deps = self._deps(reads, writes)
        drain = False
        if eng == "tensor":
            if mode in ("k32", "k64"):
                mode = "d"
            if mode != self.pe_mode and self.count["tensor"] > 0:
                deps["tensor"] = self.count["tensor"]
                drain = True
            self.pe_mode = mode
        self._waits(eng, deps, drain)
        self.count[eng] += 1
        idx = self.count[eng]
        fn(self.E[eng]).then_inc(self.sem[eng], 1)
        for b in reads:
            if b.r.get(eng, -1) < idx:
                b.r[eng] = idx
        tok = (eng, idx)
        for b in writes:
            b.w = tok
            b.r = {}
        self.ninstr += 1

    def dma(self, q, fn, reads=(), writes=()):
        stream = self.dmaq[q]
        st = self.dstate[q]
        i = st["n"] % len(self.dsems[q])
        st["n"] += 1
        k = (q, i)
        deps = self._deps(reads, writes)
        if st["cnt"][i] > 0:
            deps[k] = max(deps.get(k, -1), st["cnt"][i])
        self._waits(stream, deps)
        st["cnt"][i] += 16
        val = st["cnt"][i]
        fn(self.E[stream]).then_inc(self.dsems[q][i], 16)
        for b in reads:
            if b.r.get(k, -1) < val:
                b.r[k] = val
        tok = (k, val)
        for b in writes:
            b.w = tok
            b.r = {}
        self.ninstr += 1

    def all_counts(self):
        alld = {e: self.count[e] for e in COMPUTE if self.count[e] > 0}
        for q in self.dmaq:
            for i, c in enumerate(self.dstate[q]["cnt"]):
                if c > 0:
                    alld[(q, i)] = c
        return alld

    def barrier(self):
        alld = self.all_counts()
        for s in ("tensor", "vector", "scalar", "gpsimd", "sync"):
            self._waits(s, dict(alld))

    def finish(self):
        self._waits("sync", self.all_counts())


def _partner(col, base, dim):
    j = (col - base) % dim
    half = dim // 8
    if j < half:
        return col + half
    if j < 2 * half:
        return col - half
    return col


def _fm_tiles():
    tiles = []

    def add(cols, base, dim):
        tiles.append((list(cols), [_partner(c, base, dim) for c in cols], dim))
    for p in range(3):
        add(range(O_QA + 128 * p, O_QA + 128 * (p + 1)), O_QA, 64)
    add(list(range(O_KA, O_KA + 64)) * 2, O_KA, 64)
    for p in range(2):
        add(range(O_QI + 128 * p, O_QI + 128 * (p + 1)), O_QI, 64)
    add(list(range(O_KI, O_KI + 64)) * 2, O_KI, 64)
    for base in (O_QB, O_KB):
        for p in range(3):
            gs_ = [3 * p, 3 * p + 1, min(3 * p + 2, 7), min(3 * p + 2, 7)]
            cols = []
            for g in gs_:
                cols += list(range(base + 32 * g, base + 32 * g + 32))
            add(cols, base, 32)
    for p in range(3):
        add(range(O_QC + 128 * p, O_QC + 128 * (p + 1)), O_QC, 64)
    for p in range(3):
        add(range(O_KC + 128 * p, O_KC + 128 * (p + 1)), O_KC, 64)
    return tiles


def _kt(w):
    n = w.shape[1]
    return w.reshape(8, 128, n).transpose(1, 0, 2)


def host_layout(inp):
    f = np.float32
    out = {}
    w_in = np.asarray(inp["w_in"], f)
    tiles = _fm_tiles()
    winq = np.empty((L, 19, 128, 8, 128), f)
    for l in range(L):
        for t, (cols, pcols, _) in enumerate(tiles):
            winq[l, t] = _kt(w_in[l][:, cols])
    out["winq"] = winq
    cols_va = list(range(O_VA, O_VA + 64)) + list(range(O_WI, O_WI + 4))
    out["wva"] = np.ascontiguousarray(np.stack([_kt(w_in[l][:, cols_va]) for l in range(L)]))
    out["wvb"] = np.ascontiguousarray(np.stack([_kt(w_in[l][:, O_VB:O_VB + 256]) for l in range(L)]))
    out["wvc"] = np.ascontiguousarray(np.stack([_kt(w_in[l][:, O_VC:O_VC + 384]) for l in range(L)]))
    out["wout"] = np.ascontiguousarray(np.asarray(inp["w_out"], f).reshape(L, 8, 128, 1024))
    w_ada = np.asarray(inp["w_ada"], f)
    out["wada"] = np.ascontiguousarray(
        w_ada.reshape(L, 8, 128, 24, 2, 128).transpose(0, 3, 2, 4, 1, 5))
    out["bada"] = np.ascontiguousarray(np.asarray(inp["b_ada"], f).reshape(L, 48, 128).transpose(0, 2, 1))
    out["gattn"] = np.ascontiguousarray(np.asarray(inp["g_attn"], f).reshape(L, 8, 128).transpose(0, 2, 1))
    out["gffn"] = np.ascontiguousarray(np.asarray(inp["g_ffn"], f).reshape(L, 8, 128).transpose(0, 2, 1))
    out["gfin"] = np.ascontiguousarray(np.asarray(inp["g_final"], f).reshape(8, 128).T)
    ds = np.asarray(inp["diff_subln"], f)
    out["gsub"] = np.ascontiguousarray(np.concatenate([ds, ds], axis=1)[:, :, None])
    out["dlam"] = np.ascontiguousarray(np.asarray(inp["diff_lambda"], f).reshape(L, 1, 128))

    def gu(g, u):
        g = g.reshape(8, 128, NF, 128).transpose(2, 1, 0, 3)
        u = u.reshape(8, 128, NF, 128).transpose(2, 1, 0, 3)
        return np.stack([g, u], axis=2)

    def dn(w):
        return w.reshape(2, 11, 128, 8, 128).transpose(3, 0, 2, 1, 4)
    wg, wu, wd = (np.asarray(inp[k], f) for k in ("w_ff_gate", "w_ff_up", "w_ff_down"))
    out["fgu"] = np.ascontiguousarray(np.stack([gu(wg[j], wu[j]) for j in range(2)]))
    out["fdn"] = np.ascontiguousarray(np.stack([dn(wd[j]) for j in range(2)]))
    eg, eu, ed = (np.asarray(inp[k], f) for k in ("w_exp_gate", "w_exp_up", "w_exp_down"))
    out["egu"] = np.ascontiguousarray(
        np.stack([np.stack([gu(eg[j, e], eu[j, e]) for e in range(NEXP)]) for j in range(2)]))
    out["edn"] = np.ascontiguousarray(
        np.stack([np.stack([dn(ed[j, e]) for e in range(NEXP)]) for j in range(2)]))
    wr = np.asarray(inp["w_router"], f)
    out["wr"] = np.ascontiguousarray(wr.reshape(2, 8, 128, 8).transpose(0, 2, 1, 3))
    c = {}
    c["ident"] = np.eye(128, dtype=f)
    kk = np.arange(128)
    c["tri"] = (kk[:, None] <= kk[None, :]).astype(f)
    bo = np.zeros((128, 128), f)
    bo[:64, :64] = 1.0
    bo[64:, 64:] = 1.0
    c["bones"] = bo
    c["ones"] = np.ones((128, 128), f)
    cols = np.zeros((128, 4), f)
    inv64 = (1.0 / (THETA ** (np.arange(0, 16, 2, dtype=np.float32) / 16))).astype(f)
    inv32 = (1.0 / (THETA ** (np.arange(0, 8, 2, dtype=np.float32) / 8))).astype(f)
    for p in range(128):
        j = p % 64
        if j < 8:
            cols[p, 0], cols[p, 1] = inv64[j], -1.0
        elif j < 16:
            cols[p, 0], cols[p, 1] = inv64[j - 8], 1.0
        j = p % 32
        if j < 4:
            cols[p, 2], cols[p, 3] = inv32[j], -1.0
        elif j < 8:
            cols[p, 2], cols[p, 3] = inv32[j - 4], 1.0
    c["ropec"] = cols
    pm = np.zeros((2, 128, 128), f)
    for m in range(128):
        j = m % 64
        k = m + 8 if j < 8 else (m - 8 if j < 16 else m)
        pm[0, k, m] = 1.0
        j = m % 32
        k = m + 4 if j < 4 else (m - 4 if j < 8 else m)
        pm[1, k, m] = 1.0
    c["perm"] = pm
    ind = np.zeros((128, SEQ), f)
    for g in range(2):
        for n in range(8):
            ind[64 * g + n, 256 * n:256 * (n + 1)] = 1.0
    c["ind"] = ind
    c["ramp"] = np.ascontiguousarray(np.broadcast_to((-2e-7 * np.arange(SEQ, dtype=np.float64)).astype(f)[None, :], (128, SEQ)))
    c["cvec"] = np.ascontiguousarray(np.broadcast_to((2.0 ** -(np.arange(NBIS, dtype=np.float64) + 1)).astype(f)[None, :], (128, NBIS)))
    sel = np.zeros((8, NEXP, 128), f)
    for e in range(NEXP):
        sel[e, e, :] = 1.0
    c["sel"] = sel.reshape(8, NEXP * 128)
    out.update(c)
    return out


W_SHAPES = None


def build(shapes, nlayers=L, dbg=(), stop=None):
    nc = bass.Bass("TRN2", target_bir_lowering=False)
    dr = {}
    for name, (shp, dt) in shapes.items():
        dr[name] = nc.dram_tensor(name, list(shp), dt, kind="ExternalInput").ap()
    out_d = nc.dram_tensor("outT", [D, SEQ], F32, kind="ExternalOutput").ap()
    dbg_d = {}

    with ExitStack() as st:
        S = Sched(nc, st)

        uid = [0]

        def sb(name, shape, dt, stack=st):
            uid[0] += 1
            return TB(stack.enter_context(nc.sbuf_tensor(f"sb{uid[0]}_{name}", list(shape), dt)))

        psb = [TB(st.enter_context(nc.psum_tensor(f"ps{i}", [128, 512], F32))) for i in range(7)]
        pst = TB(st.enter_context(nc.psum_tensor("pst", [128, 1024], BF16)))
        for t_ in psb + [pst]:
            t_.b().psum = True
        rings = {"r1": [0, 1, 2], "acc": [3, 4], "r2": [5, 6], "r4": [0, 1, 2, 3], "r3": [4, 5, 6],
                 "s4": [0, 1, 2, 3], "acc2": [4, 5], "m1": [6]}
        rpos = {k: 0 for k in rings}

        def ps(ring):
            i = rings[ring][rpos[ring] % len(rings[ring])]
            rpos[ring] += 1
            return psb[i]

        xT = sb("xT", [128, KC, SEQ], F32)
        hT = sb("hT", [128, KC, SEQ], BF16)
        cos64 = sb("cos64", [128, SEQ], BF16)
        sin64 = sb("sin64", [128, SEQ], BF16)
        cos32 = sb("cos32", [128, SEQ], BF16)
        sin32 = sb("sin32", [128, SEQ], BF16)
        wring = [sb(f"wr{i}", [128, 2048], BF16) for i in range(3)]
        wpos = [0]
        scr = [sb(f"scr{i}", [128, 512], F32) for i in range(4)]
        spos = [0]
        identb = sb("identb", [128, 128], BF16)
        identf = sb("identf", [128, 128], F32)
        trib = sb("trib", [128, 128], BF16)
        bonesf = sb("bonesf", [128, 128], F32)
        onesf = sb("onesf", [128, 128], F32)
        ropec = sb("ropec", [128, 4], F32)
        modc = sb("modc", [128, L, 48], F32)
        scaleA = sb("scaleA", [128, L, 8], F32)
        scaleF = sb("scaleF", [128, L, 8], F32)
        gattn = sb("gattn", [128, L, 8], F32)
        gffn = sb("gffn", [128, L, 8], F32)
        gfin = sb("gfin", [128, 8], F32)
        gsub = sb("gsub", [128, L], F32)
        nlam = sb("nlam", [128, L], F32)
        cact = sb("cact", [128, 8], BF16)
        rstd = sb("rstd", [128, 512], F32)

        negbig_reg = nc.gpsimd.to_reg(NEG_BIG)

        def wbuf():
            w = wring[wpos[0] % 3]
            wpos[0] += 1
            return w

        def scratch():
            s = scr[spos[0] % 4]
            spos[0] += 1
            return s

        def DBG(name, ap, shape, dt, reads):
            if name not in dbg:
                return
            d = nc.dram_tensor("dbg_" + name, list(shape), dt, kind="ExternalOutput").ap()
            dbg_d[name] = d
            S.dma("dsync", lambda e: e.dma_start(out=d, in_=ap), reads=reads)

        def V(fn, r=(), w=()):
            S.op("vector", fn, r, w)

        def A(fn, r=(), w=()):
            S.op("scalar", fn, r, w)

        def G(fn, r=(), w=()):
            S.op("gpsimd", fn, r, w)

        def PE(fn, r=(), w=(), mode="d"):
            S.op("tensor", fn, r, w, mode=mode)

        def load(q, dst_ap, src_ap, w, cast=False):
            if cast:
                S.dma(q, lambda e: e.dma_start(out=dst_ap, in_=src_ap, max_dma_last_dim=4096), writes=w)
            else:
                S.dma(q, lambda e: e.dma_start(out=dst_ap, in_=src_ap), writes=w)

        load("dpool", identb.t[:], dr["ident"][:, :], [identb.b()], cast=True)
        load("dsync", identf.t[:], dr["ident"][:, :], [identf.b()])
        load("dpool", trib.t[:], dr["tri"][:, :], [trib.b()], cast=True)
        permb = sb("permb", [128, 2, 128], BF16)
        for i_ in range(2):
            load("dpool", permb.t[:, i_, :], dr["perm"][i_], [permb.b()], cast=True)
        load("dsync", bonesf.t[:], dr["bones"][:, :], [bonesf.b()])
        load("dsync", onesf.t[:], dr["ones"][:, :], [onesf.b()])
        load("dsync", ropec.t[:], dr["ropec"][:, :], [ropec.b()])
        for l in range(L):
            load("dsync", gattn.t[:, l, :], dr["gattn"][l], [gattn.b()])
            load("dsync", gffn.t[:, l, :], dr["gffn"][l], [gffn.b()])
            load("dsync", gsub.t[:, l:l + 1], dr["gsub"][l], [gsub.b()])
        load("dsync", gfin.t[:], dr["gfin"][:, :], [gfin.b()])
        xv = dr["xT"].rearrange("(k p) t -> p k t", p=128)
        for k in range(KC):
            load("dsync", xT.t[:, k, :], xv[:, k, :], [xT.b(k, c) for c in range(NCH)])

        with ExitStack() as ps0:
            posi = sb("posi", [128, SEQ], I32, ps0)
            posf = sb("posf", [128, SEQ], F32, ps0)
            ang = sb("ang", [128, SEQ], F32, ps0)
            kf = sb("kf", [128, SEQ], F32, ps0)
            ki = sb("ki", [128, SEQ], I32, ps0)
            tmp = sb("tmpw", [128, SEQ], F32, ps0)
            ccol = sb("ccol", [128, 8], F32, ps0)
            badac = sb("badac", [128, L, 48], F32, ps0)
            dl = sb("dl", [128, L, 128], F32, ps0)
            lt = sb("lt", [128, 8], F32, ps0)
            pring = [sb(f"pw{i}", [128, 2048], BF16, ps0) for i in range(4)] + wring
            ppos = [0]
            load("dsync", posi.t[:], dr["pos"][0, :].partition_broadcast(128), [posi.b()])
            load("dsync", ccol.t[:], dr["ccol"][:, :], [ccol.b()])
            for l in range(L):
                load("dsync", badac.t[:, l, :], dr["bada"][l], [badac.b()])
                load("dsync", dl.t[:, l, :], dr["dlam"][l, 0, :].partition_broadcast(128), [dl.b()])
            V(lambda e: e.tensor_copy(posf.t[:], posi.t[:]), [posi.b()], [posf.b()])

            def table(dst, invc, sgnc, shift):
                V(lambda e: e.tensor_scalar(ang.t[:], posf.t[:], ropec.t[:, invc:invc + 1], shift,
                                            op0=ALU.mult, op1=ALU.add), [posf.b(), ropec.b()], [ang.b()])
                V(lambda e: e.tensor_scalar(kf.t[:], ang.t[:], 1.0 / (2 * PI), None, op0=ALU.mult),
                  [ang.b()], [kf.b()])
                V(lambda e: e.tensor_copy(ki.t[:], kf.t[:]), [kf.b()], [ki.b()])
                V(lambda e: e.tensor_copy(kf.t[:], ki.t[:]), [ki.b()], [kf.b()])
                C1 = 6.28125
                C2 = 2 * PI - C1
                V(lambda e: e.scalar_tensor_tensor(ang.t[:], kf.t[:], -C1, ang.t[:], op0=ALU.mult, op1=ALU.add),
                  [kf.b()], [ang.b()])
                V(lambda e: e.scalar_tensor_tensor(ang.t[:], kf.t[:], -C2, ang.t[:], op0=ALU.mult, op1=ALU.add),
                  [kf.b()], [ang.b()])
                V(lambda e: e.tensor_scalar(tmp.t[:], ang.t[:], PI, -2 * PI, op0=ALU.is_gt, op1=ALU.mult),
                  [ang.b()], [tmp.b()])
                V(lambda e: e.tensor_tensor(ang.t[:], ang.t[:], tmp.t[:], ALU.add), [tmp.b()], [ang.b()])
                V(lambda e: e.tensor_scalar(tmp.t[:], ang.t[:], -PI, 2 * PI, op0=ALU.is_lt, op1=ALU.mult),
                  [ang.b()], [tmp.b()])
                V(lambda e: e.tensor_tensor(ang.t[:], ang.t[:], tmp.t[:], ALU.add), [tmp.b()], [ang.b()])
                V(lambda e: e.tensor_scalar(ang.t[:], ang.t[:], 3.1415925, -3.1415925, op0=ALU.min, op1=ALU.max),
                  [], [ang.b()])
                if sgnc is None:
                    A(lambda e: e.activation(dst.t[:], ang.t[:], AF.Sin), [ang.b()], [dst.b()])
                else:
                    A(lambda e: e.activation(tmp.t[:], ang.t[:], AF.Sin), [ang.b()], [tmp.b()])
                    V(lambda e: e.tensor_scalar(dst.t[:], tmp.t[:], ropec.t[:, sgnc:sgnc + 1], None, op0=ALU.mult),
                      [tmp.b(), ropec.b()], [dst.b()])

            table(sin64, 0, 1, 0.0)
            table(cos64, 0, None, PI / 2)
            table(sin32, 2, 3, 0.0)
            table(cos32, 2, None, PI / 2)
            A(lambda e: e.activation(cact.t[:], ccol.t[:], AF.Silu), [ccol.b()], [cact.b()])
            for l in range(L):
                mp = ps("r2")
                for g in range(24):
                    wt = pring[ppos[0] % 7]
                    ppos[0] += 1
                    wv = wt.t[:, 0:2048].rearrange("p (t k n) -> p t k n", t=2, k=8)
                    load("dpool", wt.t[:, 0:2048], dr["wada"][l, g].rearrange("p t k n -> p (t k n)"), [wt.b()], cast=True)
                    for t in range(2):
                        j = 2 * g + t
                        for k in range(KC):
                            PE(lambda e, k=k, t=t, j=j: e.matmul(mp.t[:, j:j + 1], wv[:, t, k, :], cact.t[:, k:k + 1],
                                                                 start=(k == 0), stop=(k == KC - 1), skip_group_check=True),
                               [wt.b(), cact.b()], [mp.b()])
                V(lambda e, l=l: e.tensor_tensor(modc.t[:, l, :], mp.t[:, 0:48], badac.t[:, l, :], ALU.add),
                  [mp.b(), badac.b()], [modc.b()])
                V(lambda e, l=l: e.scalar_tensor_tensor(scaleA.t[:, l, :], modc.t[:, l, 8:16], 1.0, gattn.t[:, l, :],
                                                        op0=ALU.add, op1=ALU.mult), [modc.b(), gattn.b()], [scaleA.b()])
                V(lambda e, l=l: e.scalar_tensor_tensor(scaleF.t[:, l, :], modc.t[:, l, 32:40], 1.0, gffn.t[:, l, :],
                                                        op0=ALU.add, op1=ALU.mult), [modc.b(), gffn.b()], [scaleF.b()])
                lam_init = 0.8 - 0.6 * math.exp(-0.3 * l)
                V(lambda e, l=l: e.tensor_tensor(tmp.t[:, 0:32], dl.t[:, l, 0:32], dl.t[:, l, 32:64], ALU.mult),
                  [dl.b()], [tmp.b()])
                V(lambda e, l=l: e.tensor_tensor(tmp.t[:, 32:64], dl.t[:, l, 64:96], dl.t[:, l, 96:128], ALU.mult),
                  [dl.b()], [tmp.b()])
                V(lambda e: e.tensor_reduce(lt.t[:, 0:2], tmp.t[:, 0:64].rearrange("p (a b) -> p a b", a=2),
                                            mybir.AxisListType.X, ALU.add), [tmp.b()], [lt.b()])
                A(lambda e: e.activation(lt.t[:, 2:4], lt.t[:, 0:2], AF.Exp), [lt.b()], [lt.b()])
                V(lambda e, l=l, li=lam_init: e.scalar_tensor_tensor(nlam.t[:, l:l + 1], lt.t[:, 3:4], -li, lt.t[:, 2:3],
                                                                     op0=ALU.add, op1=ALU.subtract), [lt.b()], [nlam.b()])
                V(lambda e, l=l, li=lam_init: e.tensor_scalar(gsub.t[:, l:l + 1], gsub.t[:, l:l + 1], 1.0 - li, None,
                                                              op0=ALU.mult), [], [gsub.b()])
            DBG("modc", modc.t[:], [128, L, 48], F32, [modc.b()])
            DBG("cos64", cos64.t[:], [128, SEQ], BF16, [cos64.b()])
            DBG("sin64", sin64.t[:], [128, SEQ], BF16, [sin64.b()])
            DBG("sin32", sin32.t[:], [128, SEQ], BF16, [sin32.b()])
            DBG("nlam", nlam.t[:], [128, L], F32, [nlam.b()])
            S.barrier()

        def norm_mod(scale_ap, shift_ap, eps, hf_cb=None, out_f32=None):
            for c in range(NCH):
                cs = slice(c * 512, (c + 1) * 512)
                ss = ps("r2")
                for k in range(KC):
                    sq = scratch()
                    A(lambda e, k=k, sq=sq: e.activation(sq.t[:], xT.t[:, k, cs], AF.Square), [xT.b(k, c)], [sq.b()])
                    PE(lambda e, k=k, sq=sq: e.matmul(ss.t[:], onesf.t[:], sq.t[:], start=(k == 0), stop=(k == KC - 1)),
                       [sq.b(), onesf.b()], [ss.b()], mode="f32")
                V(lambda e: e.tensor_scalar(rstd.t[:], ss.t[:], 1.0 / D, eps, op0=ALU.mult, op1=ALU.add), [ss.b()], [rstd.b()])
                A(lambda e: e.activation(rstd.t[:], rstd.t[:], AF.Sqrt), [], [rstd.b()])
                V(lambda e: e.reciprocal(rstd.t[:], rstd.t[:]), [], [rstd.b()])
                for k in range(KC):
                    tm = scratch()
                    V(lambda e, k=k, tm=tm: e.tensor_tensor(tm.t[:], xT.t[:, k, cs], rstd.t[:], ALU.mult),
                      [xT.b(k, c), rstd.b()], [tm.b()])
                    if out_f32 is not None:
                        A(lambda e, k=k, tm=tm: e.activation(out_f32.t[:, k, cs], tm.t[:], AF.Identity,
                                                             scale=scale_ap[:, k:k + 1]), [tm.b()], [out_f32.b(k, c)])
                    elif hf_cb is not None:
                        hf = scratch()
                        A(lambda e, k=k, tm=tm, hf=hf: e.activation(hf.t[:], tm.t[:], AF.Identity, bias=shift_ap[:, k:k + 1],
                                                                    scale=scale_ap[:, k:k + 1]), [tm.b()], [hf.b()])
                        G(lambda e, k=k, hf=hf: e.tensor_copy(hT.t[:, k, cs], hf.t[:]), [hf.b()], [hT.b(k, c)])
                        hf_cb(c, k, hf)
                    else:
                        A(lambda e, k=k, tm=tm: e.activation(hT.t[:, k, cs], tm.t[:], AF.Identity, bias=shift_ap[:, k:k + 1],
                                                             scale=scale_ap[:, k:k + 1]), [tm.b()], [hT.b(k, c)])

        hT_all = lambda: [hT.b(k, c) for k in range(KC) for c in range(NCH)]

        def proj_tile(l, t, dst_fn, dst_bufs_fn, kind):
            cosT, sinT = (cos64, sin64) if kind == 64 else (cos32, sin32)
            pi_ = 0 if kind == 64 else 1
            wt = wbuf()
            wv = wt.t[:, 0:1024].rearrange("p (k n) -> p k n", k=8)
            load("dpool", wt.t[:, 0:1024], dr["winq"][l, t].rearrange("p k n -> p (k n)"), [wt.b()], cast=True)

            def main(c):
                cs = slice(c * 512, (c + 1) * 512)
                pa = ps("r4")
                for k in range(KC):
                    PE(lambda e, k=k: e.matmul(pa.t[:], wv[:, k, :], hT.t[:, k, cs], start=(k == 0), stop=(k == KC - 1)),
                       [wt.b(), hT.b(k, c)], [pa.b()])
                xb = scratch()
                xbv = xb.t[:, :].bitcast(BF16)[:, 0:512]
                A(lambda e: e.activation(xbv, pa.t[:], AF.Copy), [pa.b()], [xb.b()])
                return pa, xb, xbv

            def rot(c, pa, xb, xbv):
                cs = slice(c * 512, (c + 1) * 512)
                pb = ps("r4")
                PE(lambda e: e.matmul(pb.t[:], permb.t[:, pi_, :], xbv, start=True, stop=True), [permb.b(), xb.b()], [pb.b()])
                t1, t2 = scratch(), scratch()
                V(lambda e: e.tensor_tensor(t1.t[:], pa.t[:], cosT.t[:, cs], ALU.mult), [pa.b(), cosT.b()], [t1.b()])
                V(lambda e: e.tensor_tensor(t2.t[:], pb.t[:], sinT.t[:, cs], ALU.mult), [pb.b(), sinT.b()], [t2.b()])
                G(lambda e: e.tensor_tensor(dst_fn(c), t1.t[:], t2.t[:], ALU.add), [t1.b(), t2.b()], dst_bufs_fn(c))
            prev = None
            for c in range(NCH):
                cur = main(c)
                if prev is not None:
                    rot(c - 1, *prev)
                prev = cur
            rot(NCH - 1, *prev)

        def proj_v(l, name, ncols, evac):
            wt = wbuf()
            nk = 8 if ncols <= 256 else 4
            parts = []
            for kh in range(8 // nk):
                w = wt if kh == 0 else wbuf()
                load("dpool", w.t[:, 0:nk * ncols],
                     dr[name][l][:, kh * nk:(kh + 1) * nk, :].rearrange("p k n -> p (k n)"), [w.b()], cast=True)
                parts.append(w)
            for t in range(NT):
                pv = ps("r3")
                for k in range(KC):
                    w = parts[k // nk]
                    kk = k % nk
                    PE(lambda e, k=k, w=w, kk=kk: e.matmul(pv.t[:, 0:ncols], hT.t[:, k, t * 128:(t + 1) * 128],
                                                            w.t[:, kk * ncols:(kk + 1) * ncols], start=(k == 0), stop=(k == KC - 1)),
                       [w.b(), hT.b(k, t // 4)], [pv.b()])
                evac(t, pv)

        def out_proj(l, wo, ntile, yT_fn, y_bufs, t0, n, gcol0, ring="r2"):
            per_bank = 512 // n
            c = t0 // 512
            for d0 in range(0, KC, per_bank):
                pw = ps(ring)
                for dd in range(per_bank):
                    d = d0 + dd
                    for p in range(ntile):
                        PE(lambda e, d=d, dd=dd, p=p: e.matmul(pw.t[:, dd * n:(dd + 1) * n], wo.t[:, p, d * 128:(d + 1) * 128],
                                                                yT_fn(p), start=(p == 0), stop=(p == ntile - 1), skip_group_check=True),
                           [wo.b()] + y_bufs, [pw.b()])
                for dd in range(per_bank):
                    d = d0 + dd
                    V(lambda e, d=d, dd=dd: e.scalar_tensor_tensor(xT.t[:, d, t0:t0 + n], pw.t[:, dd * n:(dd + 1) * n],
                                                                   modc.t[:, l, gcol0 + d:gcol0 + d + 1], xT.t[:, d, t0:t0 + n],
                                                                   op0=ALU.mult, op1=ALU.add),
                      [pw.b(), modc.b()], [xT.b(d, c)])

        def group_A(l):
            with ExitStack() as gs:
                qaT = sb("qaT", [128, 3, SEQ], BF16, gs)
                kaT = sb("kaT", [128, SEQ], BF16, gs)
                qiT = sb("qiT", [128, 2, SEQ], BF16, gs)
                kiT = sb("kiT", [128, SEQ], BF16, gs)
                va = sb("va", [128, NT, 192], BF16, gs)
                wi = sb("wi", [128, NT, 4], F32, gs)
                work = sb("work", [128, SEQ], F32, gs)
                mask = sb("mask", [128, SEQ], BF16, gs)
                maskT = sb("maskT", [128, SEQ], BF16, gs)
                Pt = [sb(f"PA{i}", [128, 768], BF16, gs) for i in range(2)]
                yA = [sb(f"yA{i}", [128, 3, 128], BF16, gs) for i in range(2)]
                woA = sb("woA", [128, 3, 1024], BF16, gs)
                m8 = sb("m8", [128, 8], F32, gs)
                ramp = sb("ramp", [128, SEQ], F32, gs)
                bst = sb("bst", [128, 8], F32, gs)
                tj = sb("tj", [128, NBIS], F32, gs)
                cvec = sb("cvec", [128, NBIS], F32, gs)
                load("dsync", cvec.t[:], dr["cvec"][:, :], [cvec.b()])
                load("dsync", ramp.t[:], dr["ramp"][:, :], [ramp.b()])
                for p in range(3):
                    load("dpool", woA.t[:, p, :], dr["wout"][l, p], [woA.b()], cast=True)
                for p in range(3):
                    proj_tile(l, p, lambda c, p=p: qaT.t[:, p, c * 512:(c + 1) * 512], lambda c, p=p: [qaT.b(p, c)], 64)
                proj_tile(l, 3, lambda c: kaT.t[:, c * 512:(c + 1) * 512], lambda c: [kaT.b(c)], 64)
                for p in range(2):
                    proj_tile(l, 4 + p, lambda c, p=p: qiT.t[:, p, c * 512:(c + 1) * 512], lambda c, p=p: [qiT.b(p, c)], 64)
                proj_tile(l, 6, lambda c: kiT.t[:, c * 512:(c + 1) * 512], lambda c: [kiT.b(c)], 64)
                G(lambda e: e.memset(va.t[:, :, 64:128], 1.0), [], [va.b(t) for t in range(NT)])

                def evac(t, pv):
                    A(lambda e: e.activation(va.t[:, t, 0:64], pv.t[:, 0:64], AF.Copy), [pv.b()], [va.b(t)])
                    A(lambda e: e.activation(va.t[:, t, 128:192], pv.t[:, 0:64], AF.Copy), [pv.b()], [va.b(t)])
                    V(lambda e: e.tensor_scalar(wi.t[:, t, :], pv.t[:, 64:68], 1.0 / 16.0, None, op0=ALU.mult), [pv.b()], [wi.b(t)])
                proj_v(l, "wva", 68, evac)
                if "qaT" in dbg and l == 0:
                    DBG("qaT", qaT.t[:], [128, 3, SEQ], BF16, [qaT.b(p, c) for p in range(3) for c in range(4)])
                    DBG("kaT", kaT.t[:], [128, SEQ], BF16, [kaT.b(c) for c in range(4)])
                    DBG("va", va.t[:], [128, NT, 192], BF16, [va.b(t) for t in range(NT)])
                    DBG("wi", wi.t[:], [128, NT, 4], F32, [wi.b(t) for t in range(NT)])

                def a_idx(i):
                    W = (i + 1) * 128
                    q0 = i * 128
                    for kc in range(0, W, 512):
                        n = min(512, W - kc)
                        for h in range(4):
                            half = slice(64 * (h % 2), 64 * (h % 2) + 64)
                            pi_ = ps("r2")
                            PE(lambda e, h=h, half=half: e.matmul(pi_.t[:, 0:n], qiT.t[half, h // 2, q0:q0 + 128], kiT.t[half, kc:kc + n],
                                                                   start=True, stop=True),
                               [qiT.b(h // 2, i // 4), kiT.b(kc // 512)], [pi_.b()], mode="k64")
                            r = scratch()
                            A(lambda e: e.activation(r.t[:, 0:n], pi_.t[:, 0:n], AF.Relu), [pi_.b()], [r.b()])
                            if h == 0:
                                V(lambda e: e.scalar_tensor_tensor(work.t[:, kc:kc + n], r.t[:, 0:n], wi.t[:, i, 0:1], ramp.t[:, kc:kc + n],
                                                                   op0=ALU.mult, op1=ALU.add),
                                  [r.b(), wi.b(i), ramp.b()], [work.b()])
                            else:
                                V(lambda e, h=h: e.scalar_tensor_tensor(work.t[:, kc:kc + n], r.t[:, 0:n], wi.t[:, i, h:h + 1],
                                                                        work.t[:, kc:kc + n], op0=ALU.mult, op1=ALU.add),
                                  [r.b(), wi.b(i)], [work.b()])
                    if i >= 2:
                        V(lambda e: e.tensor_reduce(bst.t[:, 0:1], work.t[:, 0:W], mybir.AxisListType.X, ALU.max), [work.b()], [bst.b()])
                        V(lambda e: e.tensor_reduce(bst.t[:, 1:2], work.t[:, 0:W], mybir.AxisListType.X, ALU.min), [work.b()], [bst.b()])
                        V(lambda e: e.tensor_tensor(bst.t[:, 2:3], bst.t[:, 0:1], bst.t[:, 1:2], ALU.subtract), [], [bst.b()])
                        V(lambda e: e.tensor_scalar(tj.t[:], cvec.t[:], bst.t[:, 2:3], None, op0=ALU.mult), [cvec.b(), bst.b()], [tj.b()])
                        V(lambda e: e.tensor_tensor(bst.t[:, 4:5], bst.t[:, 1:2], tj.t[:, 0:1], ALU.add), [tj.b()], [bst.b()])
                    G(lambda e: e.affine_select(work.t[:, q0:q0 + 128], work.t[:, q0:q0 + 128], [[-1, 128]], ALU.is_ge, negbig_reg,
                                                base=0, channel_multiplier=1), [], [work.b()])
                    if "idx" in dbg and l == 0 and i == 5:
                        DBG("idx", work.t[:, 0:W], [128, W], F32, [work.b()])
                    if i >= 2:
                        for j in range(NBIS):
                            V(lambda e: e.tensor_scalar(mask.t[:, 0:W], work.t[:, 0:W], bst.t[:, 4:5], None, op0=ALU.is_ge, op1=ALU.add,
                                                        accum_out=bst.t[:, 5:6]), [work.b(), bst.b()], [mask.b(), bst.b()])
                            V(lambda e, j=j: e.tensor_scalar(bst.t[:, 6:7], bst.t[:, 5:6], 256.0, tj.t[:, j:j + 1], op0=ALU.is_ge, op1=ALU.mult),
                              [tj.b()], [bst.b()])
                            jn = min(j + 1, NBIS - 1)
                            V(lambda e, jn=jn: e.scalar_tensor_tensor(bst.t[:, 4:5], bst.t[:, 6:7], tj.t[:, jn:jn + 1], bst.t[:, 4:5],
                                                                      op0=ALU.subtract, op1=ALU.add), [tj.b()], [bst.b()])
                        V(lambda e: e.tensor_scalar(mask.t[:, 0:W], work.t[:, 0:W], bst.t[:, 4:5], None, op0=ALU.is_ge),
                          [work.b(), bst.b()], [mask.b()])
                    else:
                        V(lambda e: e.tensor_scalar(mask.t[:, 0:W], work.t[:, 0:W], -1.0e29, None, op0=ALU.is_ge),
                          [work.b()], [mask.b()])
                    if "mask" in dbg and l == 0 and i == 5:
                        DBG("mask", mask.t[:, 0:W], [128, W], BF16, [mask.b()])

                def a_T(i):
                    for k0 in range(0, i + 1, 4):
                        nb = min(4, i + 1 - k0)
                        for j in range(nb):
                            kt = k0 + j
                            PE(lambda e, kt=kt, j=j: e.transpose(pst.t[:, j * 128:(j + 1) * 128], mask.t[:, kt * 128:(kt + 1) * 128], identb.t[:]),
                               [mask.b(), identb.b()], [pst.b()], mode="T")
                        A(lambda e, k0=k0, nb=nb: e.activation(maskT.t[:, k0 * 128:(k0 + nb) * 128], pst.t[:, 0:nb * 128], AF.Copy),
                          [pst.b()], [maskT.b()])

                def a_att(i):
                    W = (i + 1) * 128
                    q0 = i * 128
                    oX, oY = psb[3], psb[4]
                    for kt in range(i + 1):
                        ks = slice(kt * 128, (kt + 1) * 128)
                        sE, sO = ps("r1"), ps("r1")
                        for p in range(3):
                            PE(lambda e, p=p: e.matmul(sE.t[:, p * 128:(p + 1) * 128], kaT.t[0:64, ks], qaT.t[0:64, p, q0:q0 + 128],
                                                        start=True, stop=True), [kaT.b(kt // 4), qaT.b(p, i // 4)], [sE.b()], mode="k64")
                            PE(lambda e, p=p: e.matmul(sO.t[:, p * 128:(p + 1) * 128], kaT.t[64:128, ks], qaT.t[64:128, p, q0:q0 + 128],
                                                        start=True, stop=True), [kaT.b(kt // 4), qaT.b(p, i // 4)], [sO.b()], mode="k64")
                        P = Pt[kt % 2]
                        A(lambda e, P=P: e.activation(P.t[:, 0:384], sE.t[:, 0:384], AF.Exp, scale=0.125), [sE.b()], [P.b()])
                        A(lambda e, P=P: e.activation(P.t[:, 384:768], sO.t[:, 0:384], AF.Exp, scale=0.125), [sO.b()], [P.b()])
                        G(lambda e, P=P, ks=ks: e.tensor_tensor(P.t[:, :].rearrange("p (h q) -> p h q", h=6),
                                                                P.t[:, :].rearrange("p (h q) -> p h q", h=6),
                                                                maskT.t[:, ks].unsqueeze(1).to_broadcast([128, 6, 128]), ALU.mult),
                          [maskT.b()], [P.b()])
                        for p in range(3):
                            PE(lambda e, p=p, P=P, kt=kt: e.matmul(oX.t[:, p * 128:(p + 1) * 128], va.t[:, kt, 0:128], P.t[:, p * 128:(p + 1) * 128],
                                                                    start=(kt == 0 and p == 0), stop=(kt == i), skip_group_check=True),
                               [va.b(kt), P.b()], [oX.b()])
                            PE(lambda e, p=p, P=P, kt=kt: e.matmul(oY.t[:, p * 128:(p + 1) * 128], va.t[:, kt, 64:192], P.t[:, 384 + p * 128:384 + (p + 1) * 128],
                                                                    start=(kt == 0 and p == 0), stop=(kt == i), skip_group_check=True),
                               [va.b(kt), P.b()], [oY.b()])
                    y = yA[i % 2]
                    r1, r2 = scratch(), scratch()
                    A(lambda e: e.activation(r1.t[0:64, 0:384], oX.t[64:128, 0:384], AF.Ln), [oX.b()], [r1.b()])
                    A(lambda e: e.activation(r1.t[0:64, 0:384], r1.t[0:64, 0:384], AF.Exp, scale=-1.0), [], [r1.b()])
                    V(lambda e: e.tensor_tensor(y.t[0:64, :, :], oX.t[0:64, 0:384].rearrange("p (a b) -> p a b", a=3),
                                                r1.t[0:64, 0:384].rearrange("p (a b) -> p a b", a=3), ALU.mult), [oX.b(), r1.b()], [y.b()])
                    A(lambda e: e.activation(r2.t[64:128, 0:384], oY.t[0:64, 0:384], AF.Ln), [oY.b()], [r2.b()])
                    A(lambda e: e.activation(r2.t[64:128, 0:384], r2.t[64:128, 0:384], AF.Exp, scale=-1.0), [], [r2.b()])
                    V(lambda e: e.tensor_tensor(y.t[64:128, :, :], oY.t[64:128, 0:384].rearrange("p (a b) -> p a b", a=3),
                                                r2.t[64:128, 0:384].rearrange("p (a b) -> p a b", a=3), ALU.mult), [oY.b(), r2.b()], [y.b()])
                    if "yA" in dbg and l == 0 and i == 5:
                        DBG("yA", y.t[:], [128, 3, 128], BF16, [y.b()])
                    out_proj(l, woA, 3, lambda p, y=y: y.t[:, p, :], [y.b()], q0, 128, 16)

                a_idx(0)
                a_T(0)
                for i in range(NT):
                    if i + 1 < NT:
                        a_idx(i + 1)
                    a_att(i)
                    if i + 1 < NT:
                        a_T(i + 1)

        def attn_core(c, o, qk_emit, scale, v_ap, v_buf, Pt):
            nk = 4 * c + 4
            batches = [list(range(k0, min(k0 + 2, nk))) for k0 in range(0, nk, 2)]

            def do_qk(bt):
                info = []
                for kt in bt:
                    qs0 = max(c * 512, kt * 128)
                    n = (c + 1) * 512 - qs0
                    off = qs0 - c * 512
                    s_ = ps("s4")
                    qk_emit(kt, s_, n, qs0, off)
                    info.append((kt, s_, n, off))
                for kt, s_, n, off in info:
                    P = Pt[kt % 4]
                    A(lambda e: e.activation(P.t[:, 0:n], s_.t[:, 0:n], AF.Exp, scale=scale), [s_.b()], [P.b()])
                    if kt >= 4 * c:
                        G(lambda e: e.tensor_tensor(P.t[:, 0:128], P.t[:, 0:128], trib.t[:], ALU.mult), [trib.b()], [P.b()])
                return info

            def do_pv(info):
                for kt, s_, n, off in info:
                    P = Pt[kt % 4]
                    PE(lambda e: e.matmul(o.t[:, off:off + n], v_ap(kt), P.t[:, 0:n],
                                          start=(kt == 0), stop=(kt == nk - 1), skip_group_check=True),
                       [v_buf(kt), P.b()], [o.b()])
            prev = None
            for bt in batches:
                cur = do_qk(bt)
                if prev is not None:
                    do_pv(prev)
                prev = cur
            do_pv(prev)

        def group_B(l):
            with ExitStack() as gs:
                qbT = sb("qbT", [128, 3, SEQ], BF16, gs)
                kbT = sb("kbT", [128, 3, SEQ], BF16, gs)
                vb = sb("vb", [128, NT, 384], BF16, gs)
                Pt = [sb(f"PB{i}", [128, 512], BF16, gs) for i in range(4)]
                yB = [sb(f"yB{i}", [128, 2, 512], BF16, gs) for i in range(2)]
                woB = sb("woB", [128, 2, 1024], BF16, gs)
                yd = [sb(f"yd{i}", [128, 512], F32, gs) for i in range(2)]
                y1 = sb("y1", [128, 512], F32, gs)
                for p in range(2):
                    load("dpool", woB.t[:, p, :], dr["wout"][l, 3 + p], [woB.b()], cast=True)
                for p in range(3):
                    proj_tile(l, 7 + p, lambda c, p=p: qbT.t[:, p, c * 512:(c + 1) * 512], lambda c, p=p: [qbT.b(p, c)], 32)
                for p in range(3):
                    proj_tile(l, 10 + p, lambda c, p=p: kbT.t[:, p, c * 512:(c + 1) * 512], lambda c, p=p: [kbT.b(p, c)], 32)
                G(lambda e: e.memset(vb.t[:, :, 64:128], 1.0), [], [vb.b(t) for t in range(NT)])
                G(lambda e: e.memset(vb.t[:, :, 256:320], 1.0), [], [vb.b(t) for t in range(NT)])

                def evac(t, pv):
                    A(lambda e: e.activation(vb.t[:, t, 0:64], pv.t[:, 0:64], AF.Copy), [pv.b()], [vb.b(t)])
                    A(lambda e: e.activation(vb.t[:, t, 128:192], pv.t[:, 64:128], AF.Copy), [pv.b()], [vb.b(t)])
                    A(lambda e: e.activation(vb.t[:, t, 192:256], pv.t[:, 128:192], AF.Copy), [pv.b()], [vb.b(t)])
                    A(lambda e: e.activation(vb.t[:, t, 320:384], pv.t[:, 192:256], AF.Copy), [pv.b()], [vb.b(t)])
                proj_v(l, "wvb", 256, evac)

                def kT_fn(g, ks):
                    return kbT.t[32 * (g % 3):32 * (g % 3) + 32, g // 3, ks]

                def qT_fn(g, qs):
                    return qbT.t[32 * (g % 3):32 * (g % 3) + 32, g // 3, qs]

                def v_fn(g, kt):
                    h = g // 2
                    base = 192 * (h // 2) + 64 * (h % 2)
                    return vb.t[:, kt, base:base + 128]

                def finish(c, g, o):
                    h, comp = g // 2, g % 2
                    val = slice(0, 64) if h % 2 == 0 else slice(64, 128)
                    den = slice(64, 128) if h % 2 == 0 else slice(0, 64)
                    ydt = yd[(h // 2) % 2]
                    r = scratch()
                    A(lambda e: e.activation(r.t[val, :], o.t[den, :], AF.Ln), [o.b()], [r.b()])
                    A(lambda e: e.activation(r.t[val, :], r.t[val, :], AF.Exp, scale=-1.0), [], [r.b()])
                    if comp == 0:
                        V(lambda e: e.tensor_tensor(y1.t[val, :], o.t[val, :], r.t[val, :], ALU.mult), [o.b(), r.b()], [y1.b()])
                    else:
                        r2 = scratch()
                        V(lambda e: e.tensor_tensor(r2.t[val, :], o.t[val, :], r.t[val, :], ALU.mult), [o.b(), r.b()], [r2.b()])
                        V(lambda e: e.scalar_tensor_tensor(ydt.t[val, :], r2.t[val, :], nlam.t[val, l:l + 1], y1.t[val, :],
                                                           op0=ALU.mult, op1=ALU.add), [r2.b(), y1.b(), nlam.b()], [ydt.b()])
                        if h % 2 == 1:
                            pair = h // 2
                            sq = scratch()
                            A(lambda e: e.activation(sq.t[:], ydt.t[:], AF.Square), [ydt.b()], [sq.b()])
                            ss = ps("m1")
                            PE(lambda e: e.matmul(ss.t[:], bonesf.t[:], sq.t[:], start=True, stop=True), [sq.b(), bonesf.b()], [ss.b()], mode="f32")
                            rs = scratch()
                            V(lambda e: e.tensor_scalar(rs.t[:], ss.t[:], 1.0 / 64, 1e-5, op0=ALU.mult, op1=ALU.add), [ss.b()], [rs.b()])
                            A(lambda e: e.activation(rs.t[:], rs.t[:], AF.Sqrt), [], [rs.b()])
                            V(lambda e: e.reciprocal(rs.t[:], rs.t[:]), [], [rs.b()])
                            yb = yB[c % 2]
                            V(lambda e: e.scalar_tensor_tensor(yb.t[:, pair, :], ydt.t[:], gsub.t[:, l:l + 1], rs.t[:],
                                                               op0=ALU.mult, op1=ALU.mult), [ydt.b(), rs.b(), gsub.b()], [yb.b()])
                            if pair == 1:
                                if "yB" in dbg and l == 0 and c == 1:
                                    DBG("yB", yb.t[:], [128, 2, 512], BF16, [yb.b()])
                                out_proj(l, woB, 2, lambda p, yb=yb: yb.t[:, p, :], [yb.b()], c * 512, 512, 16, ring="m1")

                for c in range(NCH):
                    for g in range(8):
                        o = ps("acc2")

                        def qk_emit(kt, s_, n, qs0, off, g=g, c=c):
                            ks = slice(kt * 128, (kt + 1) * 128)
                            PE(lambda e: e.matmul(s_.t[:, 0:n], kT_fn(g, ks), qT_fn(g, slice(qs0, qs0 + n)), start=True, stop=True),
                               [kbT.b(g // 3, kt // 4), qbT.b(g // 3, c)], [s_.b()], mode="k32")
                        attn_core(c, o, qk_emit, 32 ** -0.5, lambda kt, g=g: v_fn(g, kt), lambda kt: vb.b(kt), Pt)
                        finish(c, g, o)

        def group_C(l):
            with ExitStack() as gs:
                qcT = sb("qcT", [128, 3, SEQ], BF16, gs)
                kcT = sb("kcT", [128, 3, SEQ], BF16, gs)
                vc = sb("vc", [128, NT, 576], BF16, gs)
                Pt = [sb(f"PC{i}", [128, 512], BF16, gs) for i in range(4)]
                yC = [sb(f"yC{i}", [128, 3, 512], BF16, gs) for i in range(1)]
                woC = sb("woC", [128, 3, 1024], BF16, gs)
                ind = sb("ind", [128, SEQ], BF16, gs)
                biasT = sb("biasT", [128, 3, 512], BF16, gs)
                kmf = sb("kmf", [128, 3, 8], F32, gs)
                kmb = sb("kmb", [128, 3, 8], BF16, gs)
                btm = sb("btm", [128, 6, 64], F32, gs)
                gt_ = sb("gt_", [128, 6, 8], F32, gs)
                m8 = sb("m8c", [128, 8], F32, gs)
                load("dpool", ind.t[:], dr["ind"][:, :], [ind.b()], cast=True)
                for p in range(3):
                    load("dpool", woC.t[:, p, :], dr["wout"][l, 5 + p], [woC.b()], cast=True)
                for p in range(3):
                    proj_tile(l, 13 + p, lambda c, p=p: qcT.t[:, p, c * 512:(c + 1) * 512], lambda c, p=p: [qcT.b(p, c)], 64)
                for p in range(3):
                    proj_tile(l, 16 + p, lambda c, p=p: kcT.t[:, p, c * 512:(c + 1) * 512], lambda c, p=p: [kcT.b(p, c)], 64)
                for pr in range(3):
                    G(lambda e, pr=pr: e.memset(vc.t[:, :, 192 * pr + 64:192 * pr + 128], 1.0), [], [vc.b(t) for t in range(NT)])

                def evac(t, pv):
                    for h in range(6):
                        base = 192 * (h // 2) + 128 * (h % 2)
                        A(lambda e, h=h, base=base: e.activation(vc.t[:, t, base:base + 64], pv.t[:, 64 * h:64 * h + 64], AF.Copy),
                          [pv.b()], [vc.b(t)])
                proj_v(l, "wvc", 384, evac)
                for p in range(3):
                    V(lambda e, p=p: e.tensor_reduce(kmf.t[:, p, :], kcT.t[:, p, :].rearrange("p (n k) -> p n k", n=8),
                                                     mybir.AxisListType.X, ALU.add), [kcT.b(p, c) for c in range(4)], [kmf.b()])
                V(lambda e: e.tensor_scalar(kmb.t[:], kmf.t[:], 1.0 / 256, None, op0=ALU.mult), [kmf.b()], [kmb.b()])
                if "kcT" in dbg and l == 0:
                    DBG("kcT", kcT.t[:], [128, 3, SEQ], BF16, [kcT.b(p, c) for p in range(3) for c in range(4)])
                    DBG("qcT", qcT.t[:], [128, 3, SEQ], BF16, [qcT.b(p, c) for p in range(3) for c in range(4)])

                def gating(c):
                    for tt in range(4):
                        t = 4 * c + tt
                        own = t // 2
                        gp = ps("m1")
                        for h in range(6):
                            half = slice(64 * (h % 2), 64 * (h % 2) + 64)
                            PE(lambda e, h=h, half=half: e.matmul(gp.t[:, h * 8:(h + 1) * 8], qcT.t[half, h // 2, t * 128:(t + 1) * 128],
                                                                   kmb.t[half, h // 2, :], start=True, stop=True),
                               [qcT.b(h // 2, c), kmb.b()], [gp.b()], mode="k64")
                        G(lambda e: e.memset(btm.t[:], 0.0), [], [btm.b()])
                        G(lambda e: e.memset(btm.t[:, :, 0:8], -MB), [], [btm.b()])
                        if own <= 3:
                            G(lambda e: e.memset(btm.t[:, :, 0:own + 1], 0.0), [], [btm.b()])
                        else:
                            V(lambda e: e.memset(gt_.t[:], NEG_BIG), [], [gt_.b()])
                            V(lambda e: e.tensor_copy(gt_.t[:, :, 0:own], gp.t[:, 0:48].rearrange("p (h n) -> p h n", h=6)[:, :, 0:own]),
                              [gp.b()], [gt_.b()])
                            for h in range(6):
                                V(lambda e, h=h: e.max(m8.t[:], gt_.t[:, h, :]), [gt_.b()], [m8.b()])
                                V(lambda e, h=h: e.tensor_scalar(btm.t[:, h, 0:own], gt_.t[:, h, 0:own], m8.t[:, 2:3], MB,
                                                                 op0=ALU.is_ge, op1=ALU.mult), [gt_.b(), m8.b()], [btm.b()])
                            V(lambda e: e.tensor_scalar(btm.t[:, :, 0:own], btm.t[:, :, 0:own], -MB, None, op0=ALU.add),
                              [], [btm.b()])
                            G(lambda e: e.memset(btm.t[:, :, own:own + 1], 0.0), [], [btm.b()])
                        for s2 in range(3):
                            tp = ps("r2")
                            PE(lambda e, s2=s2: e.transpose(tp.t[:, 0:128], btm.t[:, 2 * s2:2 * s2 + 2, :].rearrange("p a b -> p (a b)"), identf.t[:]),
                               [btm.b(), identf.b()], [tp.b()], mode="Tf")
                            A(lambda e, s2=s2: e.activation(biasT.t[:, s2, tt * 128:(tt + 1) * 128], tp.t[:, 0:128], AF.Copy),
                              [tp.b()], [biasT.b()])
                    if "biasT" in dbg and l == 0 and c == 3:
                        DBG("biasT", biasT.t[:], [128, 3, 512], BF16, [biasT.b()])

                for c in range(NCH):
                    gating(c)
                    for h in range(6):
                        o = ps("acc2")
                        hp = slice(64 * (h % 2), 64 * (h % 2) + 64)
                        base = 192 * (h // 2) + 64 * (h % 2)

                        def qk_emit(kt, s_, n, qs0, off, h=h, c=c, hp=hp):
                            ks = slice(kt * 128, (kt + 1) * 128)
                            qs = slice(qs0, qs0 + n)
                            PE(lambda e: e.matmul(s_.t[:, 0:n], kcT.t[hp, h // 2, ks], qcT.t[hp, h // 2, qs], start=True, stop=False),
                               [kcT.b(h // 2, kt // 4), qcT.b(h // 2, c)], [s_.b()], mode="k64")
                            PE(lambda e: e.matmul(s_.t[:, 0:n], ind.t[hp, ks], biasT.t[hp, h // 2, off:off + n], start=False, stop=True),
                               [ind.b(), biasT.b()], [s_.b()], mode="k64")
                        attn_core(c, o, qk_emit, 0.125, lambda kt, base=base: vc.t[:, kt, base:base + 128], lambda kt: vc.b(kt), Pt)
                        val = slice(0, 64) if h % 2 == 0 else slice(64, 128)
                        den = slice(64, 128) if h % 2 == 0 else slice(0, 64)
                        yc = yC[0]
                        r = scratch()
                        A(lambda e: e.activation(r.t[val, :], o.t[den, :], AF.Ln), [o.b()], [r.b()])
                        A(lambda e: e.activation(r.t[val, :], r.t[val, :], AF.Exp, scale=-1.0), [], [r.b()])
                        V(lambda e: e.tensor_tensor(yc.t[val, h // 2, :], o.t[val, :], r.t[val, :], ALU.mult), [o.b(), r.b()], [yc.b()])
                        if h == 5:
                            if "yC" in dbg and l == 0 and c == 3:
                                DBG("yC", yc.t[:], [128, 3, 512], BF16, [yc.b()])
                            out_proj(l, woC, 3, lambda p, yc=yc: yc.t[:, p, :], [yc.b()], c * 512, 512, 16, ring="m1")

        def ffn(l, gu_fn, dn_fn, comb=None, gT=None):
            with ExitStack() as gs:
                if gT is None:
                    gT = sb("gT", [128, NF, 1024], BF16, gs)
                for half in range(2):
                    for j in range(NF):
                        wt = wbuf()
                        wv = wt.t[:, 0:2048].rearrange("p (t k n) -> p t k n", t=2, k=8)
                        load("dpool", wt.t[:, 0:2048], gu_fn(j).rearrange("p t k n -> p (t k n)"), [wt.b()], cast=True)
                        for cc in range(2):
                            c = 2 * half + cc
                            cs = slice(c * 512, (c + 1) * 512)
                            pa, pu = ps("r4"), ps("r4")
                            for k in range(KC):
                                PE(lambda e, k=k: e.matmul(pa.t[:], wv[:, 0, k, :], hT.t[:, k, cs], start=(k == 0), stop=(k == KC - 1)),
                                   [wt.b(), hT.b(k, c)], [pa.b()])
                            for k in range(KC):
                                PE(lambda e, k=k: e.matmul(pu.t[:], wv[:, 1, k, :], hT.t[:, k, cs], start=(k == 0), stop=(k == KC - 1)),
                                   [wt.b(), hT.b(k, c)], [pu.b()])
                            sa = scratch()
                            A(lambda e: e.activation(sa.t[:], pa.t[:], AF.Silu), [pa.b()], [sa.b()])
                            V(lambda e, j=j, cc=cc: e.tensor_tensor(gT.t[:, j, cc * 512:(cc + 1) * 512], sa.t[:], pu.t[:], ALU.mult),
                              [sa.b(), pu.b()], [gT.b(j, cc)])
                    for d in range(KC):
                        wts = []
                        for hf in range(2):
                            wt = wbuf()
                            load("dpool", wt.t[:, 0:11 * 128], dn_fn(d, hf).rearrange("p j n -> p (j n)"), [wt.b()], cast=True)
                            wts.append(wt)
                        for cc in range(2):
                            c = 2 * half + cc
                            cs = slice(c * 512, (c + 1) * 512)
                            po = ps("r3")
                            for j in range(NF):
                                wt = wts[j // 11]
                                jj = j % 11
                                PE(lambda e, j=j, wt=wt, jj=jj: e.matmul(po.t[:], wt.t[:, jj * 128:(jj + 1) * 128], gT.t[:, j, cc * 512:(cc + 1) * 512],
                                                                          start=(j == 0), stop=(j == NF - 1)),
                                   [wt.b(), gT.b(j, cc)], [po.b()])
                            if comb is None:
                                V(lambda e, d=d: e.scalar_tensor_tensor(xT.t[:, d, cs], po.t[:], modc.t[:, l, 40 + d:41 + d], xT.t[:, d, cs],
                                                                        op0=ALU.mult, op1=ALU.add), [po.b(), modc.b()], [xT.b(d, c)])
                            else:
                                cb = comb(c)
                                tm = scratch()
                                V(lambda e, d=d, tm=tm: e.scalar_tensor_tensor(tm.t[:], po.t[:], modc.t[:, l, 40 + d:41 + d], cb.t[:, cs],
                                                                               op0=ALU.mult, op1=ALU.mult), [po.b(), modc.b(), cb.b(c)], [tm.b()])
                                V(lambda e, d=d, tm=tm: e.tensor_tensor(xT.t[:, d, cs], xT.t[:, d, cs], tm.t[:], ALU.add), [tm.b()], [xT.b(d, c)])

        def moe(l, j):
            with ExitStack() as gs:
                wr = sb("wrt", [128, 8, 8], F32, gs)
                ctm = sb("ctm", [128, NT, 8], F32, gs)
                combT = sb("combT", [8, SEQ], F32, gs)
                cbc = sb("cbc", [128, SEQ], F32, gs)
                selm = sb("selm", [8, NEXP * 128], F32, gs)
                m8 = sb("m8m", [128, 8], F32, gs)
                sm = sb("sm", [128, 8], F32, gs)
                load("dsync", wr.t[:], dr["wr"][j], [wr.b()])
                load("dsync", selm.t[:], dr["sel"][:, :], [selm.b()])
                lg = {}

                gTm = sb("gTm", [128, NF, 1024], BF16, gs)

                def hf_cb(c, k, hf):
                    if k == 0:
                        lg[c] = ps("acc")
                    pl = lg[c]
                    for tt in range(4):
                        PE(lambda e, tt=tt: e.matmul(pl.t[:, tt * 8:(tt + 1) * 8], hf.t[:, tt * 128:(tt + 1) * 128], wr.t[:, k, :],
                                                     start=(k == 0 and tt == 0), stop=(k == KC - 1), skip_group_check=True),
                           [hf.b(), wr.b()], [pl.b()], mode="f32")
                    if k == KC - 1:
                        for tt in range(4):
                            t = 4 * c + tt
                            lt_ = pl.t[:, tt * 8:(tt + 1) * 8]
                            V(lambda e, lt_=lt_: e.max(m8.t[:], lt_), [pl.b()], [m8.b()])
                            V(lambda e: e.tensor_scalar(sm.t[:, 0:1], m8.t[:, 0:1], -1.0, None, op0=ALU.mult), [m8.b()], [sm.b()])
                            A(lambda e, lt_=lt_, t=t: e.activation(ctm.t[:, t, :], lt_, AF.Exp, bias=sm.t[:, 0:1]), [pl.b(), sm.b()], [ctm.b(t)])
                            A(lambda e: e.activation(sm.t[:, 1:2], m8.t[:, 1:2], AF.Exp, bias=sm.t[:, 0:1]), [m8.b()], [sm.b()])
                            V(lambda e: e.tensor_scalar(sm.t[:, 2:3], sm.t[:, 1:2], 1.0, None, op0=ALU.add), [], [sm.b()])
                            V(lambda e: e.reciprocal(sm.t[:, 3:4], sm.t[:, 2:3]), [], [sm.b()])
                            V(lambda e, lt_=lt_: e.tensor_scalar(sm.t[:, 4:8], lt_[:, 0:4], m8.t[:, 1:2], None, op0=ALU.is_ge), [pl.b(), m8.b()], [sm.b()])
                            V(lambda e, lt_=lt_, t=t: e.scalar_tensor_tensor(ctm.t[:, t, 0:4], sm.t[:, 4:8], sm.t[:, 3:4], ctm.t[:, t, 0:4],
                                                                             op0=ALU.mult, op1=ALU.mult), [sm.b()], [ctm.b(t)])
                            V(lambda e, lt_=lt_: e.tensor_scalar(sm.t[:, 4:8], lt_[:, 4:8], m8.t[:, 1:2], None, op0=ALU.is_ge), [pl.b(), m8.b()], [sm.b()])
                            V(lambda e, lt_=lt_, t=t: e.scalar_tensor_tensor(ctm.t[:, t, 4:8], sm.t[:, 4:8], sm.t[:, 3:4], ctm.t[:, t, 4:8],
                                                                             op0=ALU.mult, op1=ALU.mult), [sm.b()], [ctm.b(t)])
                            tp = ps("r2")
                            PE(lambda e, t=t: e.transpose(tp.t[0:8, 0:128], ctm.t[:, t, :], identf.t[:]), [ctm.b(t), identf.b()], [tp.b()], mode="Tf")
                            A(lambda e, t=t: e.activation(combT.t[:, t * 128:(t + 1) * 128], tp.t[0:8, 0:128], AF.Copy), [tp.b()], [combT.b(t // 4)])

                norm_mod(scaleF.t[:, l, :], modc.t[:, l, 24:32], 1e-6, hf_cb=hf_cb)
                if "ctm" in dbg:
                    DBG("ctm", ctm.t[:], [128, NT, 8], F32, [ctm.b(t) for t in range(NT)])
                for ex in range(NEXP):
                    for c in range(NCH):
                        cs = slice(c * 512, (c + 1) * 512)
                        pb_ = ps("r2")
                        PE(lambda e, ex=ex: e.matmul(pb_.t[:], selm.t[:, ex * 128:(ex + 1) * 128], combT.t[:, cs], start=True, stop=True),
                           [selm.b(), combT.b(c)], [pb_.b()], mode="f32k8")
                        A(lambda e: e.activation(cbc.t[:, cs], pb_.t[:], AF.Copy), [pb_.b()], [cbc.b(c)])
                    ffn(l, lambda jf, ex=ex: dr["egu"][j, ex, jf], lambda d, hf, ex=ex: dr["edn"][j, ex, d, hf], comb=lambda c: cbc, gT=gTm)

        for l in range(nlayers):
            norm_mod(scaleA.t[:, l, :], modc.t[:, l, 0:8], 1e-6)
            if "hT" in dbg and l == 0:
                DBG("hT", hT.t[:], [128, KC, SEQ], BF16, hT_all())
            if stop == "norm":
                break
            group_A(l)
            if stop == "A":
                break
            S.barrier()
            group_B(l)
            if stop == "B":
                break
            S.barrier()
            group_C(l)
            if stop == "C":
                break
            S.barrier()
            if l % 2 == 0:
                norm_mod(scaleF.t[:, l, :], modc.t[:, l, 24:32], 1e-6)
                jd = l // 2
                ffn(l, lambda jf: dr["fgu"][jd, jf], lambda d, hf: dr["fdn"][jd, d, hf])
            else:
                moe(l, l // 2)
            S.barrier()
            if f"x{l}" in dbg:
                DBG(f"x{l}", xT.t[:], [128, KC, SEQ], F32, [xT.b(k, c) for k in range(KC) for c in range(NCH)])

        if stop is None:
            with ExitStack() as gs:
                of = sb("of", [128, KC, SEQ], F32, gs)
                norm_mod(gfin.t[:, :], None, 1e-6, out_f32=of)
                ov = out_d.rearrange("(k p) t -> p k t", p=128)
                for k in range(KC):
                    S.dma("dsync", lambda e, k=k: e.dma_start(out=ov[:, k, :], in_=of.t[:, k, :]),
                          reads=[of.b(k, c) for c in range(NCH)])
                S.finish()
        else:
            ov = out_d.rearrange("(k p) t -> p k t", p=128)
            for k in range(KC):
                S.dma("dsync", lambda e, k=k: e.dma_start(out=ov[:, k, :], in_=xT.t[:, k, :]),
                      reads=[xT.b(k, c) for c in range(NCH)])
            S.finish()
        print("instructions:", S.ninstr, {e: S.count[e] for e in COMPUTE})
    return nc, dbg_d


def make_in_maps(inputs):
    hl = host_layout(inputs)
    x = np.asarray(inputs["x"], np.float32)
    c = np.asarray(inputs["c"], np.float32)
    pos = np.asarray(inputs["positions"], np.int32)
    maps = []
    for b in range(8):
        m = dict(hl)
        m["xT"] = np.ascontiguousarray(x[b].T)
        m["ccol"] = np.ascontiguousarray(c[b].reshape(8, 128).T)
        m["pos"] = np.ascontiguousarray(pos[b:b + 1])
        maps.append(m)
    return maps


def shapes_of(m):
    sh = {}
    for k, v in m.items():
        sh[k] = (v.shape, I32 if v.dtype == np.int32 else F32)
    return sh


def kernel(**inputs):
    maps = make_in_maps(inputs)
    nc, _ = build(shapes_of(maps[0]))
    res = run_bass_kernel_spmd(nc, maps, core_ids=list(range(8)))
    out = np.stack([np.ascontiguousarray(res.results[b]["outT"].T) for b in range(8)])
    return out.astype(np.float32)
```

```python
import math
import numpy as np
from contextlib import ExitStack
import concourse.bass as bass
import concourse.mybir as mybir
from concourse.bass_utils import run_bass_kernel_spmd

F32 = mybir.dt.float32
BF16 = mybir.dt.bfloat16
I32 = mybir.dt.int32
ALU = mybir.AluOpType
AF = mybir.ActivationFunctionType

L = 4
D = 1024
SEQ = 2048
NT = 16
NCH = 4
KC = 8
DFF = 2816
NF = 22
NEXP = 8
THETA = 500000.0
NEG_BIG = -1.0e30
REPL = -1.0e20
MB = 30000.0
NBIS = 13
PI = math.pi
COMPUTE = ("tensor", "vector", "scalar", "gpsimd")

O_QA, O_KA, O_VA, O_QI, O_KI, O_WI, O_QB, O_KB, O_VB, O_QC, O_KC, O_VC = (
    0, 384, 448, 512, 768, 832, 836, 1092, 1348, 1604, 1988, 2372)


class Buf:
    __slots__ = ("w", "r", "psum")

    def __init__(self):
        self.w = None
        self.r = {}
        self.psum = False


class TB:
    def __init__(self, t):
        self.t = t
        self.bufs = {}

    def b(self, *key):
        v = self.bufs.get(key)
        if v is None:
            v = self.bufs[key] = Buf()
        return v


class Sched:
    def __init__(self, nc, stack, n_dma_sems=8):
        self.nc = nc
        self.E = {"tensor": nc.tensor, "vector": nc.vector, "scalar": nc.scalar,
                  "gpsimd": nc.gpsimd, "sync": nc.sync}
        self.sem = {}
        self.count = {}
        self.waited = {}
        for e in COMPUTE:
            self.sem[e] = stack.enter_context(nc.semaphore("s_" + e))
            self.count[e] = 0
        self.dmaq = {"dsync": "sync", "dpool": "gpsimd", "dact": "scalar"}
        self.dsems = {}
        self.dstate = {}
        for q in self.dmaq:
            self.dsems[q] = [stack.enter_context(nc.semaphore(f"d_{q}_{i}")) for i in range(n_dma_sems)]
            self.dstate[q] = {"n": 0, "cnt": [0] * n_dma_sems}
        self.ninstr = 0
        self.pe_mode = None

    def _semh(self, k):
        if isinstance(k, str):
            return self.sem[k]
        return self.dsems[k[0]][k[1]]

    def _deps(self, reads, writes):
        deps = {}
        for b in reads:
            if b.w is not None:
                k, v = b.w
                if deps.get(k, -1) < v:
                    deps[k] = v
            if b.psum:
                for k, v in b.r.items():
                    if deps.get(k, -1) < v:
                        deps[k] = v
        for b in writes:
            if b.w is not None:
                k, v = b.w
                if deps.get(k, -1) < v:
                    deps[k] = v
            for k, v in b.r.items():
                if deps.get(k, -1) < v:
                    deps[k] = v
        return deps

    def _waits(self, stream, deps, pe_drain=False):
        e = self.E[stream]
        for k, v in deps.items():
            if k == "tensor" and stream == "tensor" and not pe_drain:
                continue
            wk = (stream, k)
            if self.waited.get(wk, -1) >= v:
                continue
            self.waited[wk] = v
            e.wait_ge(self._semh(k), v)

    def op(self, eng, fn, reads=(), writes=(), mode=None):
        deps = self._deps(reads, writes)
        drain = False
        if eng == "tensor":
            if mode in ("k32", "k64"):
                mode = "d"
            if mode != self.pe_mode and self.count["tensor"] > 0:
                deps["tensor"] = self.count["tensor"]
                drain = True
            self.pe_mode = mode
        self._waits(eng, deps, drain)
        self.count[eng] += 1
        idx = self.count[eng]
        fn(self.E[eng]).then_inc(self.sem[eng], 1)
        for b in reads:
            if b.r.get(eng, -1) < idx:
                b.r[eng] = idx
        tok = (eng, idx)
        for b in writes:
            b.w = tok
            b.r = {}
        self.ninstr += 1

    def dma(self, q, fn, reads=(), writes=()):
        stream = self.dmaq[q]
        st = self.dstate[q]
        i = st["n"] % len(self.dsems[q])
        st["n"] += 1
        k = (q, i)
        deps = self._deps(reads, writes)
        if st["cnt"][i] > 0:
            deps[k] = max(deps.get(k, -1), st["cnt"][i])
        self._waits(stream, deps)
        st["cnt"][i] += 16
        val = st["cnt"][i]
        fn(self.E[stream]).then_inc(self.dsems[q][i], 16)
        for b in reads:
            if b.r.get(k, -1) < val:
                b.r[k] = val
        tok = (k, val)
        for b in writes:
            b.w = tok
            b.r = {}
        self.ninstr += 1

    def all_counts(self):
        alld = {e: self.count[e] for e in COMPUTE if self.count[e] > 0}
        for q in self.dmaq:
            for i, c in enumerate(self.dstate[q]["cnt"]):
                if c > 0:
                    alld[(q, i)] = c
        return alld

    def barrier(self):
        alld = self.all_counts()
        for s in ("tensor", "vector", "scalar", "gpsimd", "sync"):
            self._waits(s, dict(alld))

    def finish(self):
        self._waits("sync", self.all_counts())


def _partner(col, base, dim):
    j = (col - base) % dim
    half = dim // 8
    if j < half:
        return col + half
    if j < 2 * half:
        return col - half
    return col


def _fm_tiles():
    tiles = []

    def add(cols, base, dim):
        tiles.append((list(cols), [_partner(c, base, dim) for c in cols], dim))
    for p in range(3):
        add(range(O_QA + 128 * p, O_QA + 128 * (p + 1)), O_QA, 64)
    add(list(range(O_KA, O_KA + 64)) * 2, O_KA, 64)
    for p in range(2):
        add(range(O_QI + 128 * p, O_QI + 128 * (p + 1)), O_QI, 64)
    add(list(range(O_KI, O_KI + 64)) * 2, O_KI, 64)
    for base in (O_QB, O_KB):
        for p in range(3):
            gs_ = [3 * p, 3 * p + 1, min(3 * p + 2, 7), min(3 * p + 2, 7)]
            cols = []
            for g in gs_:
                cols += list(range(base + 32 * g, base + 32 * g + 32))
            add(cols, base, 32)
    for p in range(3):
        add(range(O_QC + 128 * p, O_QC + 128 * (p + 1)), O_QC, 64)
    for p in range(3):
        add(range(O_KC + 128 * p, O_KC + 128 * (p + 1)), O_KC, 64)
    return tiles


def _kt(w):
    n = w.shape[1]
    return w.reshape(8, 128, n).transpose(1, 0, 2)


def host_layout(inp):
    f = np.float32
    out = {}
    w_in = np.asarray(inp["w_in"], f)
    tiles = _fm_tiles()
    winq = np.empty((L, 19, 128, 8, 128), f)
    for l in range(L):
        for t, (cols, pcols, _) in enumerate(tiles):
            winq[l, t] = _kt(w_in[l][:, cols])
    out["winq"] = winq
    cols_va = list(range(O_VA, O_VA + 64)) + list(range(O_WI, O_WI + 4))
    out["wva"] = np.ascontiguousarray(np.stack([_kt(w_in[l][:, cols_va]) for l in range(L)]))
    out["wvb"] = np.ascontiguousarray(np.stack([_kt(w_in[l][:, O_VB:O_VB + 256]) for l in range(L)]))
    out["wvc"] = np.ascontiguousarray(np.stack([_kt(w_in[l][:, O_VC:O_VC + 384]) for l in range(L)]))
    out["wout"] = np.ascontiguousarray(np.asarray(inp["w_out"], f).reshape(L, 8, 128, 1024))
    w_ada = np.asarray(inp["w_ada"], f)
    out["wada"] = np.ascontiguousarray(
        w_ada.reshape(L, 8, 128, 24, 2, 128).transpose(0, 3, 2, 4, 1, 5))
    out["bada"] = np.ascontiguousarray(np.asarray(inp["b_ada"], f).reshape(L, 48, 128).transpose(0, 2, 1))
    out["gattn"] = np.ascontiguousarray(np.asarray(inp["g_attn"], f).reshape(L, 8, 128).transpose(0, 2, 1))
    out["gffn"] = np.ascontiguousarray(np.asarray(inp["g_ffn"], f).reshape(L, 8, 128).transpose(0, 2, 1))
    out["gfin"] = np.ascontiguousarray(np.asarray(inp["g_final"], f).reshape(8, 128).T)
    ds = np.asarray(inp["diff_subln"], f)
    out["gsub"] = np.ascontiguousarray(np.concatenate([ds, ds], axis=1)[:, :, None])
    out["dlam"] = np.ascontiguousarray(np.asarray(inp["diff_lambda"], f).reshape(L, 1, 128))

    def gu(g, u):
        g = g.reshape(8, 128, NF, 128).transpose(2, 1, 0, 3)
        u = u.reshape(8, 128, NF, 128).transpose(2, 1, 0, 3)
        return np.stack([g, u], axis=2)

    def dn(w):
        return w.reshape(2, 11, 128, 8, 128).transpose(3, 0, 2, 1, 4)
    wg, wu, wd = (np.asarray(inp[k], f) for k in ("w_ff_gate", "w_ff_up", "w_ff_down"))
    out["fgu"] = np.ascontiguousarray(np.stack([gu(wg[j], wu[j]) for j in range(2)]))
    out["fdn"] = np.ascontiguousarray(np.stack([dn(wd[j]) for j in range(2)]))
    eg, eu, ed = (np.asarray(inp[k], f) for k in ("w_exp_gate", "w_exp_up", "w_exp_down"))
    out["egu"] = np.ascontiguousarray(
        np.stack([np.stack([gu(eg[j, e], eu[j, e]) for e in range(NEXP)]) for j in range(2)]))
    out["edn"] = np.ascontiguousarray(
        np.stack([np.stack([dn(ed[j, e]) for e in range(NEXP)]) for j in range(2)]))
    wr = np.asarray(inp["w_router"], f)
    out["wr"] = np.ascontiguousarray(wr.reshape(2, 8, 128, 8).transpose(0, 2, 1, 3))
    c = {}
    c["ident"] = np.eye(128, dtype=f)
    kk = np.arange(128)
    c["tri"] = (kk[:, None] <= kk[None, :]).astype(f)
    bo = np.zeros((128, 128), f)
    bo[:64, :64] = 1.0
    bo[64:, 64:] = 1.0
    c["bones"] = bo
    c["ones"] = np.ones((128, 128), f)
    cols = np.zeros((128, 4), f)
    inv64 = (1.0 / (THETA ** (np.arange(0, 16, 2, dtype=np.float32) / 16))).astype(f)
    inv32 = (1.0 / (THETA ** (np.arange(0, 8, 2, dtype=np.float32) / 8))).astype(f)
    for p in range(128):
        j = p % 64
        if j < 8:
            cols[p, 0], cols[p, 1] = inv64[j], -1.0
        elif j < 16:
            cols[p, 0], cols[p, 1] = inv64[j - 8], 1.0
        j = p % 32
        if j < 4:
            cols[p, 2], cols[p, 3] = inv32[j], -1.0
        elif j < 8:
            cols[p, 2], cols[p, 3] = inv32[j - 4], 1.0
    c["ropec"] = cols
    pm = np.zeros((2, 128, 128), f)
    for m in range(128):
        j = m % 64
        k = m + 8 if j < 8 else (m - 8 if j < 16 else m)
        pm[0, k, m] = 1.0
        j = m % 32
        k = m + 4 if j < 4 else (m - 4 if j < 8 else m)
        pm[1, k, m] = 1.0
    c["perm"] = pm
    ind = np.zeros((128, SEQ), f)
    for g in range(2):
        for n in range(8):
            ind[64 * g + n, 256 * n:256 * (n + 1)] = 1.0
    c["ind"] = ind
    c["ramp"] = np.ascontiguousarray(np.broadcast_to((-2e-7 * np.arange(SEQ, dtype=np.float64)).astype(f)[None, :], (128, SEQ)))
    c["cvec"] = np.ascontiguousarray(np.broadcast_to((2.0 ** -(np.arange(NBIS, dtype=np.float64) + 1)).astype(f)[None, :], (128, NBIS)))
    sel = np.zeros((8, NEXP, 128), f)
    for e in range(NEXP):
        sel[e, e, :] = 1.0
    c["sel"] = sel.reshape(8, NEXP * 128)
    out.update(c)
    return out


W_SHAPES = None


def build(shapes, nlayers=L, dbg=(), stop=None):
    nc = bass.Bass("TRN2", target_bir_lowering=False)
    dr = {}
    for name, (shp, dt) in shapes.items():
        dr[name] = nc.dram_tensor(name, list(shp), dt, kind="ExternalInput").ap()
    out_d = nc.dram_tensor("outT", [D, SEQ], F32, kind="ExternalOutput").ap()
    dbg_d = {}

    with ExitStack() as st:
        S = Sched(nc, st)

        uid = [0]

        def sb(name, shape, dt, stack=st):
            uid[0] += 1
            return TB(stack.enter_context(nc.sbuf_tensor(f"sb{uid[0]}_{name}", list(shape), dt)))

        psb = [TB(st.enter_context(nc.psum_tensor(f"ps{i}", [128, 512], F32))) for i in range(7)]
        pst = TB(st.enter_context(nc.psum_tensor("pst", [128, 1024], BF16)))
        for t_ in psb + [pst]:
            t_.b().psum = True
        rings = {"r1": [0, 1, 2], "acc": [3, 4], "r2": [5, 6], "r4": [0, 1, 2, 3], "r3": [4, 5, 6],
                 "s4": [0, 1, 2, 3], "acc2": [4, 5], "m1": [6]}
        rpos = {k: 0 for k in rings}

        def ps(ring):
            i = rings[ring][rpos[ring] % len(rings[ring])]
            rpos[ring] += 1
            return psb[i]

        xT = sb("xT", [128, KC, SEQ], F32)
        hT = sb("hT", [128, KC, SEQ], BF16)
        cos64 = sb("cos64", [128, SEQ], BF16)
        sin64 = sb("sin64", [128, SEQ], BF16)
        cos32 = sb("cos32", [128, SEQ], BF16)
        sin32 = sb("sin32", [128, SEQ], BF16)
        wring = [sb(f"wr{i}", [128, 2048], BF16) for i in range(3)]
        wpos = [0]
        scr = [sb(f"scr{i}", [128, 512], F32) for i in range(4)]
        spos = [0]
        identb = sb("identb", [128, 128], BF16)
        identf = sb("identf", [128, 128], F32)
        trib = sb("trib", [128, 128], BF16)
        bonesf = sb("bonesf", [128, 128], F32)
        onesf = sb("onesf", [128, 128], F32)
        ropec = sb("ropec", [128, 4], F32)
        modc = sb("modc", [128, L, 48], F32)
        scaleA = sb("scaleA", [128, L, 8], F32)
        scaleF = sb("scaleF", [128, L, 8], F32)
        gattn = sb("gattn", [128, L, 8], F32)
        gffn = sb("gffn", [128, L, 8], F32)
        gfin = sb("gfin", [128, 8], F32)
        gsub = sb("gsub", [128, L], F32)
        nlam = sb("nlam", [128, L], F32)
        cact = sb("cact", [128, 8], BF16)
        rstd = sb("rstd", [128, 512], F32)

        negbig_reg = nc.gpsimd.to_reg(NEG_BIG)

        def wbuf():
            w = wring[wpos[0] % 3]
            wpos[0] += 1
            return w

        def scratch():
            s = scr[spos[0] % 4]
            spos[0] += 1
            return s

        def DBG(name, ap, shape, dt, reads):
            if name not in dbg:
                return
            d = nc.dram_tensor("dbg_" + name, list(shape), dt, kind="ExternalOutput").ap()
            dbg_d[name] = d
            S.dma("dsync", lambda e: e.dma_start(out=d, in_=ap), reads=reads)

        def V(fn, r=(), w=()):
            S.op("vector", fn, r, w)

        def A(fn, r=(), w=()):
            S.op("scalar", fn, r, w)

        def G(fn, r=(), w=()):
            S.op("gpsimd", fn, r, w)

        def PE(fn, r=(), w=(), mode="d"):
            S.op("tensor", fn, r, w, mode=mode)

        def load(q, dst_ap, src_ap, w, cast=False):
            if cast:
                S.dma(q, lambda e: e.dma_start(out=dst_ap, in_=src_ap, max_dma_last_dim=4096), writes=w)
            else:
                S.dma(q, lambda e: e.dma_start(out=dst_ap, in_=src_ap), writes=w)

        load("dpool", identb.t[:], dr["ident"][:, :], [identb.b()], cast=True)
        load("dsync", identf.t[:], dr["ident"][:, :], [identf.b()])
        load("dpool", trib.t[:], dr["tri"][:, :], [trib.b()], cast=True)
        permb = sb("permb", [128, 2, 128], BF16)
        for i_ in range(2):
            load("dpool", permb.t[:, i_, :], dr["perm"][i_], [permb.b()], cast=True)
        load("dsync", bonesf.t[:], dr["bones"][:, :], [bonesf.b()])
        load("dsync", onesf.t[:], dr["ones"][:, :], [onesf.b()])
        load("dsync", ropec.t[:], dr["ropec"][:, :], [ropec.b()])
        for l in range(L):
            load("dsync", gattn.t[:, l, :], dr["gattn"][l], [gattn.b()])
            load("dsync", gffn.t[:, l, :], dr["gffn"][l], [gffn.b()])
            load("dsync", gsub.t[:, l:l + 1], dr["gsub"][l], [gsub.b()])
        load("dsync", gfin.t[:], dr["gfin"][:, :], [gfin.b()])
        xv = dr["xT"].rearrange("(k p) t -> p k t", p=128)
        for k in range(KC):
            load("dsync", xT.t[:, k, :], xv[:, k, :], [xT.b(k, c) for c in range(NCH)])

        with ExitStack() as ps0:
            posi = sb("posi", [128, SEQ], I32, ps0)
            posf = sb("posf", [128, SEQ], F32, ps0)
            ang = sb("ang", [128, SEQ], F32, ps0)
            kf = sb("kf", [128, SEQ], F32, ps0)
            ki = sb("ki", [128, SEQ], I32, ps0)
            tmp = sb("tmpw", [128, SEQ], F32, ps0)
            ccol = sb("ccol", [128, 8], F32, ps0)
            badac = sb("badac", [128, L, 48], F32, ps0)
            dl = sb("dl", [128, L, 128], F32, ps0)
            lt = sb("lt", [128, 8], F32, ps0)
            pring = [sb(f"pw{i}", [128, 2048], BF16, ps0) for i in range(4)] + wring
            ppos = [0]
            load("dsync", posi.t[:], dr["pos"][0, :].partition_broadcast(128), [posi.b()])
            load("dsync", ccol.t[:], dr["ccol"][:, :], [ccol.b()])
            for l in range(L):
                load("dsync", badac.t[:, l, :], dr["bada"][l], [badac.b()])
                load("dsync", dl.t[:, l, :], dr["dlam"][l, 0, :].partition_broadcast(128), [dl.b()])
            V(lambda e: e.tensor_copy(posf.t[:], posi.t[:]), [posi.b()], [posf.b()])

            def table(dst, invc, sgnc, shift):
                V(lambda e: e.tensor_scalar(ang.t[:], posf.t[:], ropec.t[:, invc:invc + 1], shift,
                                            op0=ALU.mult, op1=ALU.add), [posf.b(), ropec.b()], [ang.b()])
                V(lambda e: e.tensor_scalar(kf.t[:], ang.t[:], 1.0 / (2 * PI), None, op0=ALU.mult),
                  [ang.b()], [kf.b()])
                V(lambda e: e.tensor_copy(ki.t[:], kf.t[:]), [kf.b()], [ki.b()])
                V(lambda e: e.tensor_copy(kf.t[:], ki.t[:]), [ki.b()], [kf.b()])
                C1 = 6.28125
                C2 = 2 * PI - C1
                V(lambda e: e.scalar_tensor_tensor(ang.t[:], kf.t[:], -C1, ang.t[:], op0=ALU.mult, op1=ALU.add),
                  [kf.b()], [ang.b()])
                V(lambda e: e.scalar_tensor_tensor(ang.t[:], kf.t[:], -C2, ang.t[:], op0=ALU.mult, op1=ALU.add),
                  [kf.b()], [ang.b()])
                V(lambda e: e.tensor_scalar(tmp.t[:], ang.t[:], PI, -2 * PI, op0=ALU.is_gt, op1=ALU.mult),
                  [ang.b()], [tmp.b()])
                V(lambda e: e.tensor_tensor(ang.t[:], ang.t[:], tmp.t[:], ALU.add), [tmp.b()], [ang.b()])
                V(lambda e: e.tensor_scalar(tmp.t[:], ang.t[:], -PI, 2 * PI, op0=ALU.is_lt, op1=ALU.mult),
                  [ang.b()], [tmp.b()])
                V(lambda e: e.tensor_tensor(ang.t[:], ang.t[:], tmp.t[:], ALU.add), [tmp.b()], [ang.b()])
                V(lambda e: e.tensor_scalar(ang.t[:], ang.t[:], 3.1415925, -3.1415925, op0=ALU.min, op1=ALU.max),
                  [], [ang.b()])
                if sgnc is None:
                    A(lambda e: e.activation(dst.t[:], ang.t[:], AF.Sin), [ang.b()], [dst.b()])
                else:
                    A(lambda e: e.activation(tmp.t[:], ang.t[:], AF.Sin), [ang.b()], [tmp.b()])
                    V(lambda e: e.tensor_scalar(dst.t[:], tmp.t[:], ropec.t[:, sgnc:sgnc + 1], None, op0=ALU.mult),
                      [tmp.b(), ropec.b()], [dst.b()])

            table(sin64, 0, 1, 0.0)
            table(cos64, 0, None, PI / 2)
            table(sin32, 2, 3, 0.0)
            table(cos32, 2, None, PI / 2)
            A(lambda e: e.activation(cact.t[:], ccol.t[:], AF.Silu), [ccol.b()], [cact.b()])
            for l in range(L):
                mp = ps("r2")
                for g in range(24):
                    wt = pring[ppos[0] % 7]
                    ppos[0] += 1
                    wv = wt.t[:, 0:2048].rearrange("p (t k n) -> p t k n", t=2, k=8)
                    load("dpool", wt.t[:, 0:2048], dr["wada"][l, g].rearrange("p t k n -> p (t k n)"), [wt.b()], cast=True)
                    for t in range(2):
                        j = 2 * g + t
                        for k in range(KC):
                            PE(lambda e, k=k, t=t, j=j: e.matmul(mp.t[:, j:j + 1], wv[:, t, k, :], cact.t[:, k:k + 1],
                                                                 start=(k == 0), stop=(k == KC - 1), skip_group_check=True),
                               [wt.b(), cact.b()], [mp.b()])
                V(lambda e, l=l: e.tensor_tensor(modc.t[:, l, :], mp.t[:, 0:48], badac.t[:, l, :], ALU.add),
                  [mp.b(), badac.b()], [modc.b()])
                V(lambda e, l=l: e.scalar_tensor_tensor(scaleA.t[:, l, :], modc.t[:, l, 8:16], 1.0, gattn.t[:, l, :],
                                                        op0=ALU.add, op1=ALU.mult), [modc.b(), gattn.b()], [scaleA.b()])
                V(lambda e, l=l: e.scalar_tensor_tensor(scaleF.t[:, l, :], modc.t[:, l, 32:40], 1.0, gffn.t[:, l, :],
                                                        op0=ALU.add, op1=ALU.mult), [modc.b(), gffn.b()], [scaleF.b()])
                lam_init = 0.8 - 0.6 * math.exp(-0.3 * l)
                V(lambda e, l=l: e.tensor_tensor(tmp.t[:, 0:32], dl.t[:, l, 0:32], dl.t[:, l, 32:64], ALU.mult),
                  [dl.b()], [tmp.b()])
                V(lambda e, l=l: e.tensor_tensor(tmp.t[:, 32:64], dl.t[:, l, 64:96], dl.t[:, l, 96:128], ALU.mult),
                  [dl.b()], [tmp.b()])
                V(lambda e: e.tensor_reduce(lt.t[:, 0:2], tmp.t[:, 0:64].rearrange("p (a b) -> p a b", a=2),
                                            mybir.AxisListType.X, ALU.add), [tmp.b()], [lt.b()])
                A(lambda e: e.activation(lt.t[:, 2:4], lt.t[:, 0:2], AF.Exp), [lt.b()], [lt.b()])
                V(lambda e, l=l, li=lam_init: e.scalar_tensor_tensor(nlam.t[:, l:l + 1], lt.t[:, 3:4], -li, lt.t[:, 2:3],
                                                                     op0=ALU.add, op1=ALU.subtract), [lt.b()], [nlam.b()])
                V(lambda e, l=l, li=lam_init: e.tensor_scalar(gsub.t[:, l:l + 1], gsub.t[:, l:l + 1], 1.0 - li, None,
                                                              op0=ALU.mult), [], [gsub.b()])
            DBG("modc", modc.t[:], [128, L, 48], F32, [modc.b()])
            DBG("cos64", cos64.t[:], [128, SEQ], BF16, [cos64.b()])
            DBG("sin64", sin64.t[:], [128, SEQ], BF16, [sin64.b()])
            DBG("sin32", sin32.t[:], [128, SEQ], BF16, [sin32.b()])
            DBG("nlam", nlam.t[:], [128, L], F32, [nlam.b()])
            S.barrier()

        def norm_mod(scale_ap, shift_ap, eps, hf_cb=None, out_f32=None):
            for c in range(NCH):
                cs = slice(c * 512, (c + 1) * 512)
                ss = ps("r2")
                for k in range(KC):
                    sq = scratch()
                    A(lambda e, k=k, sq=sq: e.activation(sq.t[:], xT.t[:, k, cs], AF.Square), [xT.b(k, c)], [sq.b()])
                    PE(lambda e, k=k, sq=sq: e.matmul(ss.t[:], onesf.t[:], sq.t[:], start=(k == 0), stop=(k == KC - 1)),
                       [sq.b(), onesf.b()], [ss.b()], mode="f32")
                V(lambda e: e.tensor_scalar(rstd.t[:], ss.t[:], 1.0 / D, eps, op0=ALU.mult, op1=ALU.add), [ss.b()], [rstd.b()])
                A(lambda e: e.activation(rstd.t[:], rstd.t[:], AF.Ln), [], [rstd.b()])
                A(lambda e: e.activation(rstd.t[:], rstd.t[:], AF.Exp, scale=-0.5), [], [rstd.b()])
                for k in range(KC):
                    tm = scratch()
                    V(lambda e, k=k, tm=tm: e.tensor_tensor(tm.t[:], xT.t[:, k, cs], rstd.t[:], ALU.mult),
                      [xT.b(k, c), rstd.b()], [tm.b()])
                    if out_f32 is not None:
                        A(lambda e, k=k, tm=tm: e.activation(out_f32.t[:, k, cs], tm.t[:], AF.Identity,
                                                             scale=scale_ap[:, k:k + 1]), [tm.b()], [out_f32.b(k, c)])
                    elif hf_cb is not None:
                        hf = scratch()
                        A(lambda e, k=k, tm=tm, hf=hf: e.activation(hf.t[:], tm.t[:], AF.Identity, bias=shift_ap[:, k:k + 1],
                                                                    scale=scale_ap[:, k:k + 1]), [tm.b()], [hf.b()])
                        G(lambda e, k=k, hf=hf: e.tensor_copy(hT.t[:, k, cs], hf.t[:]), [hf.b()], [hT.b(k, c)])
                        hf_cb(c, k, hf)
                    else:
                        A(lambda e, k=k, tm=tm: e.activation(hT.t[:, k, cs], tm.t[:], AF.Identity, bias=shift_ap[:, k:k + 1],
                                                             scale=scale_ap[:, k:k + 1]), [tm.b()], [hT.b(k, c)])

        hT_all = lambda: [hT.b(k, c) for k in range(KC) for c in range(NCH)]

        def proj_tile(l, t, dst_fn, dst_bufs_fn, kind):
            cosT, sinT = (cos64, sin64) if kind == 64 else (cos32, sin32)
            pi_ = 0 if kind == 64 else 1
            wt = wbuf()
            wv = wt.t[:, 0:1024].rearrange("p (k n) -> p k n", k=8)
            load("dpool", wt.t[:, 0:1024], dr["winq"][l, t].rearrange("p k n -> p (k n)"), [wt.b()], cast=True)

            def main(c):
                cs = slice(c * 512, (c + 1) * 512)
                pa = ps("r4")
                for k in range(KC):
                    PE(lambda e, k=k: e.matmul(pa.t[:], wv[:, k, :], hT.t[:, k, cs], start=(k == 0), stop=(k == KC - 1)),
                       [wt.b(), hT.b(k, c)], [pa.b()])
                xb = scratch()
                xbv = xb.t[:, :].bitcast(BF16)[:, 0:512]
                A(lambda e: e.activation(xbv, pa.t[:], AF.Copy), [pa.b()], [xb.b()])
                return pa, xb, xbv

            def rot(c, pa, xb, xbv):
                cs = slice(c * 512, (c + 1) * 512)
                pb = ps("r4")
                PE(lambda e: e.matmul(pb.t[:], permb.t[:, pi_, :], xbv, start=True, stop=True), [permb.b(), xb.b()], [pb.b()])
                t1, t2 = scratch(), scratch()
                V(lambda e: e.tensor_tensor(t1.t[:], pa.t[:], cosT.t[:, cs], ALU.mult), [pa.b(), cosT.b()], [t1.b()])
                V(lambda e: e.tensor_tensor(t2.t[:], pb.t[:], sinT.t[:, cs], ALU.mult), [pb.b(), sinT.b()], [t2.b()])
                G(lambda e: e.tensor_tensor(dst_fn(c), t1.t[:], t2.t[:], ALU.add), [t1.b(), t2.b()], dst_bufs_fn(c))
            prev = None
            for c in range(NCH):
                cur = main(c)
                if prev is not None:
                    rot(c - 1, *prev)
                prev = cur
            rot(NCH - 1, *prev)

        def proj_v(l, name, ncols, evac):
            wt = wbuf()
            nk = 8 if ncols <= 256 else 4
            parts = []
            for kh in range(8 // nk):
                w = wt if kh == 0 else wbuf()
                load("dpool", w.t[:, 0:nk * ncols],
                     dr[name][l][:, kh * nk:(kh + 1) * nk, :].rearrange("p k n -> p (k n)"), [w.b()], cast=True)
                parts.append(w)
            for t in range(NT):
                pv = ps("r3")
                for k in range(KC):
                    w = parts[k // nk]
                    kk = k % nk
                    PE(lambda e, k=k, w=w, kk=kk: e.matmul(pv.t[:, 0:ncols], hT.t[:, k, t * 128:(t + 1) * 128],
                                                            w.t[:, kk * ncols:(kk + 1) * ncols], start=(k == 0), stop=(k == KC - 1)),
                       [w.b(), hT.b(k, t // 4)], [pv.b()])
                evac(t, pv)

        def out_proj(l, wo, ntile, yT_fn, y_bufs, t0, n, gcol0, ring="r2"):
            per_bank = 512 // n
            c = t0 // 512
            for d0 in range(0, KC, per_bank):
                pw = ps(ring)
                for dd in range(per_bank):
                    d = d0 + dd
                    for p in range(ntile):
                        PE(lambda e, d=d, dd=dd, p=p: e.matmul(pw.t[:, dd * n:(dd + 1) * n], wo.t[:, p, d * 128:(d + 1) * 128],
                                                                yT_fn(p), start=(p == 0), stop=(p == ntile - 1), skip_group_check=True),
                           [wo.b()] + y_bufs, [pw.b()])
                for dd in range(per_bank):
                    d = d0 + dd
                    V(lambda e, d=d, dd=dd: e.scalar_tensor_tensor(xT.t[:, d, t0:t0 + n], pw.t[:, dd * n:(dd + 1) * n],
                                                                   modc.t[:, l, gcol0 + d:gcol0 + d + 1], xT.t[:, d, t0:t0 + n],
                                                                   op0=ALU.mult, op1=ALU.add),
                      [pw.b(), modc.b()], [xT.b(d, c)])

        def group_A(l):
            with ExitStack() as gs:
                qaT = sb("qaT", [128, 3, SEQ], BF16, gs)
                kaT = sb("kaT", [128, SEQ], BF16, gs)
                qiT = sb("qiT", [128, 2, SEQ], BF16, gs)
                kiT = sb("kiT", [128, SEQ], BF16, gs)
                va = sb("va", [128, NT, 192], BF16, gs)
                wi = sb("wi", [128, NT, 4], F32, gs)
                work = sb("work", [128, SEQ], F32, gs)
                mask = sb("mask", [128, SEQ], BF16, gs)
                maskT = sb("maskT", [128, SEQ], BF16, gs)
                Pt = [sb(f"PA{i}", [128, 768], BF16, gs) for i in range(2)]
                yA = [sb(f"yA{i}", [128, 3, 128], BF16, gs) for i in range(2)]
                woA = sb("woA", [128, 3, 1024], BF16, gs)
                m8 = sb("m8", [128, 8], F32, gs)
                ramp = sb("ramp", [128, SEQ], F32, gs)
                bst = sb("bst", [128, 8], F32, gs)
                tj = sb("tj", [128, NBIS], F32, gs)
                cvec = sb("cvec", [128, NBIS], F32, gs)
                load("dsync", cvec.t[:], dr["cvec"][:, :], [cvec.b()])
                load("dsync", ramp.t[:], dr["ramp"][:, :], [ramp.b()])
                for p in range(3):
                    load("dpool", woA.t[:, p, :], dr["wout"][l, p], [woA.b()], cast=True)
                for p in range(3):
                    proj_tile(l, p, lambda c, p=p: qaT.t[:, p, c * 512:(c + 1) * 512], lambda c, p=p: [qaT.b(p, c)], 64)
                proj_tile(l, 3, lambda c: kaT.t[:, c * 512:(c + 1) * 512], lambda c: [kaT.b(c)], 64)
                for p in range(2):
                    proj_tile(l, 4 + p, lambda c, p=p: qiT.t[:, p, c * 512:(c + 1) * 512], lambda c, p=p: [qiT.b(p, c)], 64)
                proj_tile(l, 6, lambda c: kiT.t[:, c * 512:(c + 1) * 512], lambda c: [kiT.b(c)], 64)
                G(lambda e: e.memset(va.t[:, :, 64:128], 1.0), [], [va.b(t) for t in range(NT)])

                def evac(t, pv):
                    A(lambda e: e.activation(va.t[:, t, 0:64], pv.t[:, 0:64], AF.Copy), [pv.b()], [va.b(t)])
                    A(lambda e: e.activation(va.t[:, t, 128:192], pv.t[:, 0:64], AF.Copy), [pv.b()], [va.b(t)])
                    V(lambda e: e.tensor_scalar(wi.t[:, t, :], pv.t[:, 64:68], 1.0 / 16.0, None, op0=ALU.mult), [pv.b()], [wi.b(t)])
                proj_v(l, "wva", 68, evac)
                if "qaT" in dbg and l == 0:
                    DBG("qaT", qaT.t[:], [128, 3, SEQ], BF16, [qaT.b(p, c) for p in range(3) for c in range(4)])
                    DBG("kaT", kaT.t[:], [128, SEQ], BF16, [kaT.b(c) for c in range(4)])
                    DBG("va", va.t[:], [128, NT, 192], BF16, [va.b(t) for t in range(NT)])
                    DBG("wi", wi.t[:], [128, NT, 4], F32, [wi.b(t) for t in range(NT)])

                def a_idx(i):
                    W = (i + 1) * 128
                    q0 = i * 128
                    for kc in range(0, W, 512):
                        n = min(512, W - kc)
                        for h in range(4):
                            half = slice(64 * (h % 2), 64 * (h % 2) + 64)
                            pi_ = ps("r2")
                            PE(lambda e, h=h, half=half: e.matmul(pi_.t[:, 0:n], qiT.t[half, h // 2, q0:q0 + 128], kiT.t[half, kc:kc + n],
                                                                   start=True, stop=True),
                               [qiT.b(h // 2, i // 4), kiT.b(kc // 512)], [pi_.b()], mode="k64")
                            r = scratch()
                            A(lambda e: e.activation(r.t[:, 0:n], pi_.t[:, 0:n], AF.Relu), [pi_.b()], [r.b()])
                            if h == 0:
                                V(lambda e: e.scalar_tensor_tensor(work.t[:, kc:kc + n], r.t[:, 0:n], wi.t[:, i, 0:1], ramp.t[:, kc:kc + n],
                                                                   op0=ALU.mult, op1=ALU.add),
                                  [r.b(), wi.b(i), ramp.b()], [work.b()])
                            else:
                                V(lambda e, h=h: e.scalar_tensor_tensor(work.t[:, kc:kc + n], r.t[:, 0:n], wi.t[:, i, h:h + 1],
                                                                        work.t[:, kc:kc + n], op0=ALU.mult, op1=ALU.add),
                                  [r.b(), wi.b(i)], [work.b()])
                    if i >= 2:
                        V(lambda e: e.tensor_reduce(bst.t[:, 0:1], work.t[:, 0:W], mybir.AxisListType.X, ALU.max), [work.b()], [bst.b()])
                        V(lambda e: e.tensor_reduce(bst.t[:, 1:2], work.t[:, 0:W], mybir.AxisListType.X, ALU.min), [work.b()], [bst.b()])
                        V(lambda e: e.tensor_tensor(bst.t[:, 2:3], bst.t[:, 0:1], bst.t[:, 1:2], ALU.subtract), [], [bst.b()])
                        V(lambda e: e.tensor_scalar(tj.t[:], cvec.t[:], bst.t[:, 2:3], None, op0=ALU.mult), [cvec.b(), bst.b()], [tj.b()])
                        V(lambda e: e.tensor_tensor(bst.t[:, 4:5], bst.t[:, 1:2], tj.t[:, 0:1], ALU.add), [tj.b()], [bst.b()])
                    G(lambda e: e.affine_select(work.t[:, q0:q0 + 128], work.t[:, q0:q0 + 128], [[-1, 128]], ALU.is_ge, negbig_reg,
                                                base=0, channel_multiplier=1), [], [work.b()])
                    if "idx" in dbg and l == 0 and i == 5:
                        DBG("idx", work.t[:, 0:W], [128, W], F32, [work.b()])
                    if i >= 2:
                        for j in range(NBIS):
                            V(lambda e: e.tensor_scalar(mask.t[:, 0:W], work.t[:, 0:W], bst.t[:, 4:5], None, op0=ALU.is_ge, op1=ALU.add,
                                                        accum_out=bst.t[:, 5:6]), [work.b(), bst.b()], [mask.b(), bst.b()])
                            V(lambda e, j=j: e.tensor_scalar(bst.t[:, 6:7], bst.t[:, 5:6], 256.0, tj.t[:, j:j + 1], op0=ALU.is_ge, op1=ALU.mult),
                              [tj.b()], [bst.b()])
                            jn = min(j + 1, NBIS - 1)
                            V(lambda e, jn=jn: e.scalar_tensor_tensor(bst.t[:, 4:5], bst.t[:, 6:7], tj.t[:, jn:jn + 1], bst.t[:, 4:5],
                                                                      op0=ALU.subtract, op1=ALU.add), [tj.b()], [bst.b()])
                        V(lambda e: e.tensor_scalar(mask.t[:, 0:W], work.t[:, 0:W], bst.t[:, 4:5], None, op0=ALU.is_ge),
                          [work.b(), bst.b()], [mask.b()])
                    else:
                        V(lambda e: e.tensor_scalar(mask.t[:, 0:W], work.t[:, 0:W], -1.0e29, None, op0=ALU.is_ge),
                          [work.b()], [mask.b()])
                    if "mask" in dbg and l == 0 and i == 5:
                        DBG("mask", mask.t[:, 0:W], [128, W], BF16, [mask.b()])

                def a_T(i):
                    for k0 in range(0, i + 1, 4):
                        nb = min(4, i + 1 - k0)
                        for j in range(nb):
                            kt = k0 + j
                            PE(lambda e, kt=kt, j=j: e.transpose(pst.t[:, j * 128:(j + 1) * 128], mask.t[:, kt * 128:(kt + 1) * 128], identb.t[:]),
                               [mask.b(), identb.b()], [pst.b()], mode="T")
                        A(lambda e, k0=k0, nb=nb: e.activation(maskT.t[:, k0 * 128:(k0 + nb) * 128], pst.t[:, 0:nb * 128], AF.Copy),
                          [pst.b()], [maskT.b()])

                def a_att(i):
                    W = (i + 1) * 128
                    q0 = i * 128
                    oX, oY = psb[3], psb[4]
                    for kt in range(i + 1):
                        ks = slice(kt * 128, (kt + 1) * 128)
                        sE, sO = ps("r1"), ps("r1")
                        for p in range(3):
                            PE(lambda e, p=p: e.matmul(sE.t[:, p * 128:(p + 1) * 128], kaT.t[0:64, ks], qaT.t[0:64, p, q0:q0 + 128],
                                                        start=True, stop=True), [kaT.b(kt // 4), qaT.b(p, i // 4)], [sE.b()], mode="k64")
                            PE(lambda e, p=p: e.matmul(sO.t[:, p * 128:(p + 1) * 128], kaT.t[64:128, ks], qaT.t[64:128, p, q0:q0 + 128],
                                                        start=True, stop=True), [kaT.b(kt // 4), qaT.b(p, i // 4)], [sO.b()], mode="k64")
                        P = Pt[kt % 2]
                        A(lambda e, P=P: e.activation(P.t[:, 0:384], sE.t[:, 0:384], AF.Exp, scale=0.125), [sE.b()], [P.b()])
                        A(lambda e, P=P: e.activation(P.t[:, 384:768], sO.t[:, 0:384], AF.Exp, scale=0.125), [sO.b()], [P.b()])
                        G(lambda e, P=P, ks=ks: e.tensor_tensor(P.t[:, :].rearrange("p (h q) -> p h q", h=6),
                                                                P.t[:, :].rearrange("p (h q) -> p h q", h=6),
                                                                maskT.t[:, ks].unsqueeze(1).to_broadcast([128, 6, 128]), ALU.mult),
                          [maskT.b()], [P.b()])
                        for p in range(3):
                            PE(lambda e, p=p, P=P, kt=kt: e.matmul(oX.t[:, p * 128:(p + 1) * 128], va.t[:, kt, 0:128], P.t[:, p * 128:(p + 1) * 128],
                                                                    start=(kt == 0 and p == 0), stop=(kt == i), skip_group_check=True),
                               [va.b(kt), P.b()], [oX.b()])
                            PE(lambda e, p=p, P=P, kt=kt: e.matmul(oY.t[:, p * 128:(p + 1) * 128], va.t[:, kt, 64:192], P.t[:, 384 + p * 128:384 + (p + 1) * 128],
                                                                    start=(kt == 0 and p == 0), stop=(kt == i), skip_group_check=True),
                               [va.b(kt), P.b()], [oY.b()])
                    y = yA[i % 2]
                    r1, r2 = scratch(), scratch()
                    A(lambda e: e.activation(r1.t[0:64, 0:384], oX.t[64:128, 0:384], AF.Ln), [oX.b()], [r1.b()])
                    A(lambda e: e.activation(r1.t[0:64, 0:384], r1.t[0:64, 0:384], AF.Exp, scale=-1.0), [], [r1.b()])
                    V(lambda e: e.tensor_tensor(y.t[0:64, :, :], oX.t[0:64, 0:384].rearrange("p (a b) -> p a b", a=3),
                                                r1.t[0:64, 0:384].rearrange("p (a b) -> p a b", a=3), ALU.mult), [oX.b(), r1.b()], [y.b()])
                    A(lambda e: e.activation(r2.t[64:128, 0:384], oY.t[0:64, 0:384], AF.Ln), [oY.b()], [r2.b()])
                    A(lambda e: e.activation(r2.t[64:128, 0:384], r2.t[64:128, 0:384], AF.Exp, scale=-1.0), [], [r2.b()])
                    V(lambda e: e.tensor_tensor(y.t[64:128, :, :], oY.t[64:128, 0:384].rearrange("p (a b) -> p a b", a=3),
                                                r2.t[64:128, 0:384].rearrange("p (a b) -> p a b", a=3), ALU.mult), [oY.b(), r2.b()], [y.b()])
                    if "yA" in dbg and l == 0 and i == 5:
                        DBG("yA", y.t[:], [128, 3, 128], BF16, [y.b()])
                    out_proj(l, woA, 3, lambda p, y=y: y.t[:, p, :], [y.b()], q0, 128, 16)

                a_idx(0)
                a_T(0)
                for i in range(NT):
                    if i + 1 < NT:
                        a_idx(i + 1)
                    a_att(i)
                    if i + 1 < NT:
                        a_T(i + 1)

        def attn_core(c, o, qk_emit, scale, v_ap, v_buf, Pt):
            nk = 4 * c + 4
            batches = [list(range(k0, min(k0 + 2, nk))) for k0 in range(0, nk, 2)]

            def do_qk(bt):
                info = []
                for kt in bt:
                    qs0 = max(c * 512, kt * 128)
                    n = (c + 1) * 512 - qs0
                    off = qs0 - c * 512
                    s_ = ps("s4")
                    qk_emit(kt, s_, n, qs0, off)
                    info.append((kt, s_, n, off))
                for kt, s_, n, off in info:
                    P = Pt[kt % 4]
                    A(lambda e: e.activation(P.t[:, 0:n], s_.t[:, 0:n], AF.Exp, scale=scale), [s_.b()], [P.b()])
                    if kt >= 4 * c:
                        G(lambda e: e.tensor_tensor(P.t[:, 0:128], P.t[:, 0:128], trib.t[:], ALU.mult), [trib.b()], [P.b()])
                return info

            def do_pv(info):
                for kt, s_, n, off in info:
                    P = Pt[kt % 4]
                    PE(lambda e: e.matmul(o.t[:, off:off + n], v_ap(kt), P.t[:, 0:n],
                                          start=(kt == 0), stop=(kt == nk - 1), skip_group_check=True),
                       [v_buf(kt), P.b()], [o.b()])
            prev = None
            for bt in batches:
                cur = do_qk(bt)
                if prev is not None:
                    do_pv(prev)
                prev = cur
            do_pv(prev)

        def group_B(l):
            with ExitStack() as gs:
                qbT = sb("qbT", [128, 3, SEQ], BF16, gs)
                kbT = sb("kbT", [128, 3, SEQ], BF16, gs)
                vb = sb("vb", [128, NT, 384], BF16, gs)
                Pt = [sb(f"PB{i}", [128, 512], BF16, gs) for i in range(4)]
                yB = [sb(f"yB{i}", [128, 2, 512], BF16, gs) for i in range(2)]
                woB = sb("woB", [128, 2, 1024], BF16, gs)
                yd = [sb(f"yd{i}", [128, 512], F32, gs) for i in range(2)]
                y1 = sb("y1", [128, 512], F32, gs)
                for p in range(2):
                    load("dpool", woB.t[:, p, :], dr["wout"][l, 3 + p], [woB.b()], cast=True)
                for p in range(3):
                    proj_tile(l, 7 + p, lambda c, p=p: qbT.t[:, p, c * 512:(c + 1) * 512], lambda c, p=p: [qbT.b(p, c)], 32)
                for p in range(3):
                    proj_tile(l, 10 + p, lambda c, p=p: kbT.t[:, p, c * 512:(c + 1) * 512], lambda c, p=p: [kbT.b(p, c)], 32)
                G(lambda e: e.memset(vb.t[:, :, 64:128], 1.0), [], [vb.b(t) for t in range(NT)])
                G(lambda e: e.memset(vb.t[:, :, 256:320], 1.0), [], [vb.b(t) for t in range(NT)])

                def evac(t, pv):
                    A(lambda e: e.activation(vb.t[:, t, 0:64], pv.t[:, 0:64], AF.Copy), [pv.b()], [vb.b(t)])
                    A(lambda e: e.activation(vb.t[:, t, 128:192], pv.t[:, 64:128], AF.Copy), [pv.b()], [vb.b(t)])
                    A(lambda e: e.activation(vb.t[:, t, 192:256], pv.t[:, 128:192], AF.Copy), [pv.b()], [vb.b(t)])
                    A(lambda e: e.activation(vb.t[:, t, 320:384], pv.t[:, 192:256], AF.Copy), [pv.b()], [vb.b(t)])
                proj_v(l, "wvb", 256, evac)

                def kT_fn(g, ks):
                    return kbT.t[32 * (g % 3):32 * (g % 3) + 32, g // 3, ks]

                def qT_fn(g, qs):
                    return qbT.t[32 * (g % 3):32 * (g % 3) + 32, g // 3, qs]

                def v_fn(g, kt):
                    h = g // 2
                    base = 192 * (h // 2) + 64 * (h % 2)
                    return vb.t[:, kt, base:base + 128]

                def finish(c, g, o):
                    h, comp = g // 2, g % 2
                    val = slice(0, 64) if h % 2 == 0 else slice(64, 128)
                    den = slice(64, 128) if h % 2 == 0 else slice(0, 64)
                    ydt = yd[(h // 2) % 2]
                    r = scratch()
                    A(lambda e: e.activation(r.t[val, :], o.t[den, :], AF.Ln), [o.b()], [r.b()])
                    A(lambda e: e.activation(r.t[val, :], r.t[val, :], AF.Exp, scale=-1.0), [], [r.b()])
                    if comp == 0:
                        V(lambda e: e.tensor_tensor(y1.t[val, :], o.t[val, :], r.t[val, :], ALU.mult), [o.b(), r.b()], [y1.b()])
                    else:
                        r2 = scratch()
                        V(lambda e: e.tensor_tensor(r2.t[val, :], o.t[val, :], r.t[val, :], ALU.mult), [o.b(), r.b()], [r2.b()])
                        V(lambda e: e.scalar_tensor_tensor(ydt.t[val, :], r2.t[val, :], nlam.t[val, l:l + 1], y1.t[val, :],
                                                           op0=ALU.mult, op1=ALU.add), [r2.b(), y1.b(), nlam.b()], [ydt.b()])
                        if h % 2 == 1:
                            pair = h // 2
                            sq = scratch()
                            A(lambda e: e.activation(sq.t[:], ydt.t[:], AF.Square), [ydt.b()], [sq.b()])
                            ss = ps("m1")
                            PE(lambda e: e.matmul(ss.t[:], bonesf.t[:], sq.t[:], start=True, stop=True), [sq.b(), bonesf.b()], [ss.b()], mode="f32")
                            rs = scratch()
                            V(lambda e: e.tensor_scalar(rs.t[:], ss.t[:], 1.0 / 64, 1e-5, op0=ALU.mult, op1=ALU.add), [ss.b()], [rs.b()])
                            A(lambda e: e.activation(rs.t[:], rs.t[:], AF.Ln), [], [rs.b()])
                            A(lambda e: e.activation(rs.t[:], rs.t[:], AF.Exp, scale=-0.5), [], [rs.b()])
                            yb = yB[c % 2]
                            V(lambda e: e.scalar_tensor_tensor(yb.t[:, pair, :], ydt.t[:], gsub.t[:, l:l + 1], rs.t[:],
                                                               op0=ALU.mult, op1=ALU.mult), [ydt.b(), rs.b(), gsub.b()], [yb.b()])
                            if pair == 1:
                                if "yB" in dbg and l == 0 and c == 1:
                                    DBG("yB", yb.t[:], [128, 2, 512], BF16, [yb.b()])
                                out_proj(l, woB, 2, lambda p, yb=yb: yb.t[:, p, :], [yb.b()], c * 512, 512, 16, ring="m1")

                for c in range(NCH):
                    for g in range(8):
                        o = ps("acc2")

                        def qk_emit(kt, s_, n, qs0, off, g=g, c=c):
                            ks = slice(kt * 128, (kt + 1) * 128)
                            PE(lambda e: e.matmul(s_.t[:, 0:n], kT_fn(g, ks), qT_fn(g, slice(qs0, qs0 + n)), start=True, stop=True),
                               [kbT.b(g // 3, kt // 4), qbT.b(g // 3, c)], [s_.b()], mode="k32")
                        attn_core(c, o, qk_emit, 32 ** -0.5, lambda kt, g=g: v_fn(g, kt), lambda kt: vb.b(kt), Pt)
                        finish(c, g, o)

        def group_C(l):
            with ExitStack() as gs:
                qcT = sb("qcT", [128, 3, SEQ], BF16, gs)
                kcT = sb("kcT", [128, 3, SEQ], BF16, gs)
                vc = sb("vc", [128, NT, 576], BF16, gs)
                Pt = [sb(f"PC{i}", [128, 512], BF16, gs) for i in range(4)]
                yC = [sb(f"yC{i}", [128, 3, 512], BF16, gs) for i in range(1)]
                woC = sb("woC", [128, 3, 1024], BF16, gs)
                ind = sb("ind", [128, SEQ], BF16, gs)
                biasT = sb("biasT", [128, 3, 512], BF16, gs)
                kmf = sb("kmf", [128, 3, 8], F32, gs)
                kmb = sb("kmb", [128, 3, 8], BF16, gs)
                btm = sb("btm", [128, 6, 64], F32, gs)
                gt_ = sb("gt_", [128, 6, 8], F32, gs)
                m8 = sb("m8c", [128, 8], F32, gs)
                load("dpool", ind.t[:], dr["ind"][:, :], [ind.b()], cast=True)
                for p in range(3):
                    load("dpool", woC.t[:, p, :], dr["wout"][l, 5 + p], [woC.b()], cast=True)
                for p in range(3):
                    proj_tile(l, 13 + p, lambda c, p=p: qcT.t[:, p, c * 512:(c + 1) * 512], lambda c, p=p: [qcT.b(p, c)], 64)
                for p in range(3):
                    proj_tile(l, 16 + p, lambda c, p=p: kcT.t[:, p, c * 512:(c + 1) * 512], lambda c, p=p: [kcT.b(p, c)], 64)
                for pr in range(3):
                    G(lambda e, pr=pr: e.memset(vc.t[:, :, 192 * pr + 64:192 * pr + 128], 1.0), [], [vc.b(t) for t in range(NT)])

                def evac(t, pv):
                    for h in range(6):
                        base = 192 * (h // 2) + 128 * (h % 2)
                        A(lambda e, h=h, base=base: e.activation(vc.t[:, t, base:base + 64], pv.t[:, 64 * h:64 * h + 64], AF.Copy),
                          [pv.b()], [vc.b(t)])
                proj_v(l, "wvc", 384, evac)
                for p in range(3):
                    V(lambda e, p=p: e.tensor_reduce(kmf.t[:, p, :], kcT.t[:, p, :].rearrange("p (n k) -> p n k", n=8),
                                                     mybir.AxisListType.X, ALU.add), [kcT.b(p, c) for c in range(4)], [kmf.b()])
                V(lambda e: e.tensor_scalar(kmb.t[:], kmf.t[:], 1.0 / 256, None, op0=ALU.mult), [kmf.b()], [kmb.b()])
                if "kcT" in dbg and l == 0:
                    DBG("kcT", kcT.t[:], [128, 3, SEQ], BF16, [kcT.b(p, c) for p in range(3) for c in range(4)])
                    DBG("qcT", qcT.t[:], [128, 3, SEQ], BF16, [qcT.b(p, c) for p in range(3) for c in range(4)])

                def gating(c):
                    for tt in range(4):
                        t = 4 * c + tt
                        own = t // 2
                        gp = ps("m1")
                        for h in range(6):
                            half = slice(64 * (h % 2), 64 * (h % 2) + 64)
                            PE(lambda e, h=h, half=half: e.matmul(gp.t[:, h * 8:(h + 1) * 8], qcT.t[half, h // 2, t * 128:(t + 1) * 128],
                                                                   kmb.t[half, h // 2, :], start=True, stop=True),
                               [qcT.b(h // 2, c), kmb.b()], [gp.b()], mode="k64")
                        G(lambda e: e.memset(btm.t[:], 0.0), [], [btm.b()])
                        G(lambda e: e.memset(btm.t[:, :, 0:8], -MB), [], [btm.b()])
                        if own <= 3:
                            G(lambda e: e.memset(btm.t[:, :, 0:own + 1], 0.0), [], [btm.b()])
                        else:
                            V(lambda e: e.memset(gt_.t[:], NEG_BIG), [], [gt_.b()])
                            V(lambda e: e.tensor_copy(gt_.t[:, :, 0:own], gp.t[:, 0:48].rearrange("p (h n) -> p h n", h=6)[:, :, 0:own]),
                              [gp.b()], [gt_.b()])
                            for h in range(6):
                                V(lambda e, h=h: e.max(m8.t[:], gt_.t[:, h, :]), [gt_.b()], [m8.b()])
                                V(lambda e, h=h: e.tensor_scalar(btm.t[:, h, 0:own], gt_.t[:, h, 0:own], m8.t[:, 2:3], MB,
                                                                 op0=ALU.is_ge, op1=ALU.mult), [gt_.b(), m8.b()], [btm.b()])
                            V(lambda e: e.tensor_scalar(btm.t[:, :, 0:own], btm.t[:, :, 0:own], -MB, None, op0=ALU.add),
                              [], [btm.b()])
                            G(lambda e: e.memset(btm.t[:, :, own:own + 1], 0.0), [], [btm.b()])
                        for s2 in range(3):
                            tp = ps("r2")
                            PE(lambda e, s2=s2: e.transpose(tp.t[:, 0:128], btm.t[:, 2 * s2:2 * s2 + 2, :].rearrange("p a b -> p (a b)"), identf.t[:]),
                               [btm.b(), identf.b()], [tp.b()], mode="Tf")
                            A(lambda e, s2=s2: e.activation(biasT.t[:, s2, tt * 128:(tt + 1) * 128], tp.t[:, 0:128], AF.Copy),
                              [tp.b()], [biasT.b()])
                    if "biasT" in dbg and l == 0 and c == 3:
                        DBG("biasT", biasT.t[:], [128, 3, 512], BF16, [biasT.b()])

                for c in range(NCH):
                    gating(c)
                    for h in range(6):
                        o = ps("acc2")
                        hp = slice(64 * (h % 2), 64 * (h % 2) + 64)
                        base = 192 * (h // 2) + 64 * (h % 2)

                        def qk_emit(kt, s_, n, qs0, off, h=h, c=c, hp=hp):
                            ks = slice(kt * 128, (kt + 1) * 128)
                            qs = slice(qs0, qs0 + n)
                            PE(lambda e: e.matmul(s_.t[:, 0:n], kcT.t[hp, h // 2, ks], qcT.t[hp, h // 2, qs], start=True, stop=False),
                               [kcT.b(h // 2, kt // 4), qcT.b(h // 2, c)], [s_.b()], mode="k64")
                            PE(lambda e: e.matmul(s_.t[:, 0:n], ind.t[hp, ks], biasT.t[hp, h // 2, off:off + n], start=False, stop=True),
                               [ind.b(), biasT.b()], [s_.b()], mode="k64")
                        attn_core(c, o, qk_emit, 0.125, lambda kt, base=base: vc.t[:, kt, base:base + 128], lambda kt: vc.b(kt), Pt)
                        val = slice(0, 64) if h % 2 == 0 else slice(64, 128)
                        den = slice(64, 128) if h % 2 == 0 else slice(0, 64)
                        yc = yC[0]
                        r = scratch()
                        A(lambda e: e.activation(r.t[val, :], o.t[den, :], AF.Ln), [o.b()], [r.b()])
                        A(lambda e: e.activation(r.t[val, :], r.t[val, :], AF.Exp, scale=-1.0), [], [r.b()])
                        V(lambda e: e.tensor_tensor(yc.t[val, h // 2, :], o.t[val, :], r.t[val, :], ALU.mult), [o.b(), r.b()], [yc.b()])
                        if h == 5:
                            if "yC" in dbg and l == 0 and c == 3:
                                DBG("yC", yc.t[:], [128, 3, 512], BF16, [yc.b()])
                            out_proj(l, woC, 3, lambda p, yc=yc: yc.t[:, p, :], [yc.b()], c * 512, 512, 16, ring="m1")

        def ffn(l, gu_fn, dn_fn, comb=None, gT=None):
            with ExitStack() as gs:
                if gT is None:
                    gT = sb("gT", [128, NF, 1024], BF16, gs)
                for half in range(2):
                    for j in range(NF):
                        wt = wbuf()
                        wv = wt.t[:, 0:2048].rearrange("p (t k n) -> p t k n", t=2, k=8)
                        load("dpool", wt.t[:, 0:2048], gu_fn(j).rearrange("p t k n -> p (t k n)"), [wt.b()], cast=True)
                        for cc in range(2):
                            c = 2 * half + cc
                            cs = slice(c * 512, (c + 1) * 512)
                            pa, pu = ps("r4"), ps("r4")
                            for k in range(KC):
                                PE(lambda e, k=k: e.matmul(pa.t[:], wv[:, 0, k, :], hT.t[:, k, cs], start=(k == 0), stop=(k == KC - 1)),
                                   [wt.b(), hT.b(k, c)], [pa.b()])
                            for k in range(KC):
                                PE(lambda e, k=k: e.matmul(pu.t[:], wv[:, 1, k, :], hT.t[:, k, cs], start=(k == 0), stop=(k == KC - 1)),
                                   [wt.b(), hT.b(k, c)], [pu.b()])
                            sa = scratch()
                            A(lambda e: e.activation(sa.t[:], pa.t[:], AF.Silu), [pa.b()], [sa.b()])
                            V(lambda e, j=j, cc=cc: e.tensor_tensor(gT.t[:, j, cc * 512:(cc + 1) * 512], sa.t[:], pu.t[:], ALU.mult),
                              [sa.b(), pu.b()], [gT.b(j, cc)])
                    for d in range(KC):
                        wts = []
                        for hf in range(2):
                            wt = wbuf()
                            load("dpool", wt.t[:, 0:11 * 128], dn_fn(d, hf).rearrange("p j n -> p (j n)"), [wt.b()], cast=True)
                            wts.append(wt)
                        for cc in range(2):
                            c = 2 * half + cc
                            cs = slice(c * 512, (c + 1) * 512)
                            po = ps("r3")
                            for j in range(NF):
                                wt = wts[j // 11]
                                jj = j % 11
                                PE(lambda e, j=j, wt=wt, jj=jj: e.matmul(po.t[:], wt.t[:, jj * 128:(jj + 1) * 128], gT.t[:, j, cc * 512:(cc + 1) * 512],
                                                                          start=(j == 0), stop=(j == NF - 1)),
                                   [wt.b(), gT.b(j, cc)], [po.b()])
                            if comb is None:
                                V(lambda e, d=d: e.scalar_tensor_tensor(xT.t[:, d, cs], po.t[:], modc.t[:, l, 40 + d:41 + d], xT.t[:, d, cs],
                                                                        op0=ALU.mult, op1=ALU.add), [po.b(), modc.b()], [xT.b(d, c)])
                            else:
                                cb = comb(c)
                                tm = scratch()
                                V(lambda e, d=d, tm=tm: e.scalar_tensor_tensor(tm.t[:], po.t[:], modc.t[:, l, 40 + d:41 + d], cb.t[:, cs],
                                                                               op0=ALU.mult, op1=ALU.mult), [po.b(), modc.b(), cb.b(c)], [tm.b()])
                                V(lambda e, d=d, tm=tm: e.tensor_tensor(xT.t[:, d, cs], xT.t[:, d, cs], tm.t[:], ALU.add), [tm.b()], [xT.b(d, c)])

        def moe(l, j):
            with ExitStack() as gs:
                wr = sb("wrt", [128, 8, 8], F32, gs)
                ctm = sb("ctm", [128, NT, 8], F32, gs)
                combT = sb("combT", [8, SEQ], F32, gs)
                cbc = sb("cbc", [128, SEQ], F32, gs)
                selm = sb("selm", [8, NEXP * 128], F32, gs)
                m8 = sb("m8m", [128, 8], F32, gs)
                sm = sb("sm", [128, 8], F32, gs)
                load("dsync", wr.t[:], dr["wr"][j], [wr.b()])
                load("dsync", selm.t[:], dr["sel"][:, :], [selm.b()])
                lg = {}

                gTm = sb("gTm", [128, NF, 1024], BF16, gs)

                def hf_cb(c, k, hf):
                    if k == 0:
                        lg[c] = ps("acc")
                    pl = lg[c]
                    for tt in range(4):
                        PE(lambda e, tt=tt: e.matmul(pl.t[:, tt * 8:(tt + 1) * 8], hf.t[:, tt * 128:(tt + 1) * 128], wr.t[:, k, :],
                                                     start=(k == 0 and tt == 0), stop=(k == KC - 1), skip_group_check=True),
                           [hf.b(), wr.b()], [pl.b()], mode="f32")
                    if k == KC - 1:
                        for tt in range(4):
                            t = 4 * c + tt
                            lt_ = pl.t[:, tt * 8:(tt + 1) * 8]
                            V(lambda e, lt_=lt_: e.max(m8.t[:], lt_), [pl.b()], [m8.b()])
                            V(lambda e: e.tensor_scalar(sm.t[:, 0:1], m8.t[:, 0:1], -1.0, None, op0=ALU.mult), [m8.b()], [sm.b()])
                            A(lambda e, lt_=lt_, t=t: e.activation(ctm.t[:, t, :], lt_, AF.Exp, bias=sm.t[:, 0:1]), [pl.b(), sm.b()], [ctm.b(t)])
                            A(lambda e: e.activation(sm.t[:, 1:2], m8.t[:, 1:2], AF.Exp, bias=sm.t[:, 0:1]), [m8.b()], [sm.b()])
                            V(lambda e: e.tensor_scalar(sm.t[:, 2:3], sm.t[:, 1:2], 1.0, None, op0=ALU.add), [], [sm.b()])
                            V(lambda e: e.reciprocal(sm.t[:, 3:4], sm.t[:, 2:3]), [], [sm.b()])
                            V(lambda e, lt_=lt_: e.tensor_scalar(sm.t[:, 4:8], lt_[:, 0:4], m8.t[:, 1:2], None, op0=ALU.is_ge), [pl.b(), m8.b()], [sm.b()])
                            V(lambda e, lt_=lt_, t=t: e.scalar_tensor_tensor(ctm.t[:, t, 0:4], sm.t[:, 4:8], sm.t[:, 3:4], ctm.t[:, t, 0:4],
                                                                             op0=ALU.mult, op1=ALU.mult), [sm.b()], [ctm.b(t)])
                            V(lambda e, lt_=lt_: e.tensor_scalar(sm.t[:, 4:8], lt_[:, 4:8], m8.t[:, 1:2], None, op0=ALU.is_ge), [pl.b(), m8.b()], [sm.b()])
                            V(lambda e, lt_=lt_, t=t: e.scalar_tensor_tensor(ctm.t[:, t, 4:8], sm.t[:, 4:8], sm.t[:, 3:4], ctm.t[:, t, 4:8],
                                                                             op0=ALU.mult, op1=ALU.mult), [sm.b()], [ctm.b(t)])
                            tp = ps("r2")
                            PE(lambda e, t=t: e.transpose(tp.t[0:8, 0:128], ctm.t[:, t, :], identf.t[:]), [ctm.b(t), identf.b()], [tp.b()], mode="Tf")
                            A(lambda e, t=t: e.activation(combT.t[:, t * 128:(t + 1) * 128], tp.t[0:8, 0:128], AF.Copy), [tp.b()], [combT.b(t // 4)])

                norm_mod(scaleF.t[:, l, :], modc.t[:, l, 24:32], 1e-6, hf_cb=hf_cb)
                if "ctm" in dbg:
                    DBG("ctm", ctm.t[:], [128, NT, 8], F32, [ctm.b(t) for t in range(NT)])
                for ex in range(NEXP):
                    for c in range(NCH):
                        cs = slice(c * 512, (c + 1) * 512)
                        pb_ = ps("r2")
                        PE(lambda e, ex=ex: e.matmul(pb_.t[:], selm.t[:, ex * 128:(ex + 1) * 128], combT.t[:, cs], start=True, stop=True),
                           [selm.b(), combT.b(c)], [pb_.b()], mode="f32k8")
                        A(lambda e: e.activation(cbc.t[:, cs], pb_.t[:], AF.Copy), [pb_.b()], [cbc.b(c)])
                    ffn(l, lambda jf, ex=ex: dr["egu"][j, ex, jf], lambda d, hf, ex=ex: dr["edn"][j, ex, d, hf], comb=lambda c: cbc, gT=gTm)

        for l in range(nlayers):
            norm_mod(scaleA.t[:, l, :], modc.t[:, l, 0:8], 1e-6)
            if "hT" in dbg and l == 0:
                DBG("hT", hT.t[:], [128, KC, SEQ], BF16, hT_all())
            if stop == "norm":
                break
            group_A(l)
            if stop == "A":
                break
            S.barrier()
            group_B(l)
            if stop == "B":
                break
            S.barrier()
            group_C(l)
            if stop == "C":
                break
            S.barrier()
            if l % 2 == 0:
                norm_mod(scaleF.t[:, l, :], modc.t[:, l, 24:32], 1e-6)
                jd = l // 2
                ffn(l, lambda jf: dr["fgu"][jd, jf], lambda d, hf: dr["fdn"][jd, d, hf])
            else:
                moe(l, l // 2)
            S.barrier()
            if f"x{l}" in dbg:
                DBG(f"x{l}", xT.t[:], [128, KC, SEQ], F32, [xT.b(k, c) for k in range(KC) for c in range(NCH)])

        if stop is None:
            with ExitStack() as gs:
                of = sb("of", [128, KC, SEQ], F32, gs)
                norm_mod(gfin.t[:, :], None, 1e-6, out_f32=of)
                ov = out_d.rearrange("(k p) t -> p k t", p=128)
                for k in range(KC):
                    S.dma("dsync", lambda e, k=k: e.dma_start(out=ov[:, k, :], in_=of.t[:, k, :]),
                          reads=[of.b(k, c) for c in range(NCH)])
                S.finish()
        else:
            ov = out_d.rearrange("(k p) t -> p k t", p=128)
            for k in range(KC):
                S.dma("dsync", lambda e, k=k: e.dma_start(out=ov[:, k, :], in_=xT.t[:, k, :]),
                      reads=[xT.b(k, c) for c in range(NCH)])
            S.finish()
        print("instructions:", S.ninstr, {e: S.count[e] for e in COMPUTE})
    return nc, dbg_d


def make_in_maps(inputs):
    hl = host_layout(inputs)
    x = np.asarray(inputs["x"], np.float32)
    c = np.asarray(inputs["c"], np.float32)
    pos = np.asarray(inputs["positions"], np.int32)
    maps = []
    for b in range(8):
        m = dict(hl)
        m["xT"] = np.ascontiguousarray(x[b].T)
        m["ccol"] = np.ascontiguousarray(c[b].reshape(8, 128).T)
        m["pos"] = np.ascontiguousarray(pos[b:b + 1])
        maps.append(m)
    return maps


def shapes_of(m):
    sh = {}
    for k, v in m.items():
        sh[k] = (v.shape, I32 if v.dtype == np.int32 else F32)
    return sh


def kernel(**inputs):
    maps = make_in_maps(inputs)
    nc, _ = build(shapes_of(maps[0]))
    res = run_bass_kernel_spmd(nc, maps, core_ids=list(range(8)))
    out = np.stack([np.ascontiguousarray(res.results[b]["outT"].T) for b in range(8)])
    return out.astype(np.float32)
```

```python
import math
import numpy as np
from contextlib import ExitStack
import concourse.bass as bass
import concourse.mybir as mybir
from concourse.bass_utils import run_bass_kernel_spmd

F32 = mybir.dt.float32
BF16 = mybir.dt.bfloat16
I32 = mybir.dt.int32
ALU = mybir.AluOpType
AF = mybir.ActivationFunctionType

L = 4
D = 1024
SEQ = 2048
NT = 16
NCH = 4
KC = 8
DFF = 2816
NF = 22
NEXP = 8
THETA = 500000.0
NEG_BIG = -1.0e30
REPL = -1.0e20
MB = 30000.0
NBIS = 13
PI = math.pi
COMPUTE = ("tensor", "vector", "scalar", "gpsimd")

O_QA, O_KA, O_VA, O_QI, O_KI, O_WI, O_QB, O_KB, O_VB, O_QC, O_KC, O_VC = (
    0, 384, 448, 512, 768, 832, 836, 1092, 1348, 1604, 1988, 2372)


class Buf:
    __slots__ = ("w", "r", "psum")

    def __init__(self):
        self.w = None
        self.r = {}
        self.psum = False


class TB:
    def __init__(self, t):
        self.t = t
        self.bufs = {}

    def b(self, *key):
        v = self.bufs.get(key)
        if v is None:
            v = self.bufs[key] = Buf()
        return v


class Sched:
    def __init__(self, nc, stack, n_dma_sems=8):
        self.nc = nc
        self.E = {"tensor": nc.tensor, "vector": nc.vector, "scalar": nc.scalar,
                  "gpsimd": nc.gpsimd, "sync": nc.sync}
        self.sem = {}
        self.count = {}
        self.waited = {}
        for e in COMPUTE:
            self.sem[e] = stack.enter_context(nc.semaphore("s_" + e))
            self.count[e] = 0
        self.dmaq = {"dsync": "sync", "dpool": "gpsimd", "dact": "scalar"}
        self.dsems = {}
        self.dstate = {}
        for q in self.dmaq:
            self.dsems[q] = [stack.enter_context(nc.semaphore(f"d_{q}_{i}")) for i in range(n_dma_sems)]
            self.dstate[q] = {"n": 0, "cnt": [0] * n_dma_sems}
        self.ninstr = 0
        self.pe_mode = None

    def _semh(self, k):
        if isinstance(k, str):
            return self.sem[k]
        return self.dsems[k[0]][k[1]]

    def _deps(self, reads, writes):
        deps = {}
        for b in reads:
            if b.w is not None:
                k, v = b.w
                if deps.get(k, -1) < v:
                    deps[k] = v
            if b.psum:
                for k, v in b.r.items():
                    if deps.get(k, -1) < v:
                        deps[k] = v
        for b in writes:
            if b.w is not None:
                k, v = b.w
                if deps.get(k, -1) < v:
                    deps[k] = v
            for k, v in b.r.items():
                if deps.get(k, -1) < v:
                    deps[k] = v
        return deps

    def _waits(self, stream, deps, pe_drain=False):
        e = self.E[stream]
        for k, v in deps.items():
            if k == "tensor" and stream == "tensor" and not pe_drain:
                continue
            wk = (stream, k)
            if self.waited.get(wk, -1) >= v:
                continue
            self.waited[wk] = v
            e.wait_ge(self._semh(k), v)

    def op(self, eng, fn, reads=(), writes=(), mode=None):
        deps = self._deps(reads, writes)
        drain = False
        if eng == "tensor":
            if mode in ("k32", "k64"):
                mode = "d"
            if mode != self.pe_mode and self.count["tensor"] > 0:
                deps["tensor"] = self.count["tensor"]
                drain = True
            self.pe_mode = mode
        self._waits(eng, deps, drain)
        self.count[eng] += 1
        idx = self.count[eng]
        fn(self.E[eng]).then_inc(self.sem[eng], 1)
        for b in reads:
            if b.r.get(eng, -1) < idx:
                b.r[eng] = idx
        tok = (eng, idx)
        for b in writes:
            b.w = tok
            b.r = {}
        self.ninstr += 1

    def dma(self, q, fn, reads=(), writes=()):
        stream = self.dmaq[q]
        st = self.dstate[q]
        i = st["n"] % len(self.dsems[q])
        st["n"] += 1
        k = (q, i)
        deps = self._deps(reads, writes)
        if st["cnt"][i] > 0:
            deps[k] = max(deps.get(k, -1), st["cnt"][i])
        self._waits(stream, deps)
        st["cnt"][i] += 16
        val = st["cnt"][i]
        fn(self.E[stream]).then_inc(self.dsems[q][i], 16)
        for b in reads:
            if b.r.get(k, -1) < val:
                b.r[k] = val
        tok = (k, val)
        for b in writes:
            b.w = tok
            b.r = {}
        self.ninstr += 1

    def all_counts(self):
        alld = {e: self.count[e] for e in COMPUTE if self.count[e] > 0}
        for q in self.dmaq:
            for i, c in enumerate(self.dstate[q]["cnt"]):
                if c > 0:
                    alld[(q, i)] = c
        return alld

    def barrier(self):
        alld = self.all_counts()
        for s in ("tensor", "vector", "scalar", "gpsimd", "sync"):
            self._waits(s, dict(alld))

    def finish(self):
        self._waits("sync", self.all_counts())


def _partner(col, base, dim):
    j = (col - base) % dim
    half = dim // 8
    if j < half:
        return col + half
    if j < 2 * half:
        return col - half
    return col


def _fm_tiles():
    tiles = []

    def add(cols, base, dim):
        tiles.append((list(cols), [_partner(c, base, dim) for c in cols], dim))
    for p in range(3):
        add(range(O_QA + 128 * p, O_QA + 128 * (p + 1)), O_QA, 64)
    add(list(range(O_KA, O_KA + 64)) * 2, O_KA, 64)
    for p in range(2):
        add(range(O_QI + 128 * p, O_QI + 128 * (p + 1)), O_QI, 64)
    add(list(range(O_KI, O_KI + 64)) * 2, O_KI, 64)
    for base in (O_QB, O_KB):
        for p in range(3):
            gs_ = [3 * p, 3 * p + 1, min(3 * p + 2, 7), min(3 * p + 2, 7)]
            cols = []
            for g in gs_:
                cols += list(range(base + 32 * g, base + 32 * g + 32))
            add(cols, base, 32)
    for p in range(3):
        add(range(O_QC + 128 * p, O_QC + 128 * (p + 1)), O_QC, 64)
    for p in range(3):
        add(range(O_KC + 128 * p, O_KC + 128 * (p + 1)), O_KC, 64)
    return tiles


def _kt(w):
    n = w.shape[1]
    return w.reshape(8, 128, n).transpose(1, 0, 2)


def host_layout(inp):
    f = np.float32
    out = {}
    w_in = np.asarray(inp["w_in"], f)
    tiles = _fm_tiles()
    winq = np.empty((L, 19, 128, 8, 128), f)
    for l in range(L):
        for t, (cols, pcols, _) in enumerate(tiles):
            winq[l, t] = _kt(w_in[l][:, cols])
    out["winq"] = winq
    cols_va = list(range(O_VA, O_VA + 64)) + list(range(O_WI, O_WI + 4))
    out["wva"] = np.ascontiguousarray(np.stack([_kt(w_in[l][:, cols_va]) for l in range(L)]))
    out["wvb"] = np.ascontiguousarray(np.stack([_kt(w_in[l][:, O_VB:O_VB + 256]) for l in range(L)]))
    out["wvc"] = np.ascontiguousarray(np.stack([_kt(w_in[l][:, O_VC:O_VC + 384]) for l in range(L)]))
    out["wout"] = np.ascontiguousarray(np.asarray(inp["w_out"], f).reshape(L, 8, 128, 1024))
    w_ada = np.asarray(inp["w_ada"], f)
    out["wada"] = np.ascontiguousarray(
        w_ada.reshape(L, 8, 128, 24, 2, 128).transpose(0, 3, 2, 4, 1, 5))
    out["bada"] = np.ascontiguousarray(np.asarray(inp["b_ada"], f).reshape(L, 48, 128).transpose(0, 2, 1))
    out["gattn"] = np.ascontiguousarray(np.asarray(inp["g_attn"], f).reshape(L, 8, 128).transpose(0, 2, 1))
    out["gffn"] = np.ascontiguousarray(np.asarray(inp["g_ffn"], f).reshape(L, 8, 128).transpose(0, 2, 1))
    out["gfin"] = np.ascontiguousarray(np.asarray(inp["g_final"], f).reshape(8, 128).T)
    ds = np.asarray(inp["diff_subln"], f)
    out["gsub"] = np.ascontiguousarray(np.concatenate([ds, ds], axis=1)[:, :, None])
    out["dlam"] = np.ascontiguousarray(np.asarray(inp["diff_lambda"], f).reshape(L, 1, 128))

    def gu(g, u):
        g = g.reshape(8, 128, NF, 128).transpose(2, 1, 0, 3)
        u = u.reshape(8, 128, NF, 128).transpose(2, 1, 0, 3)
        return np.stack([g, u], axis=2)

    def dn(w):
        return w.reshape(2, 11, 128, 8, 128).transpose(3, 0, 2, 1, 4)
    wg, wu, wd = (np.asarray(inp[k], f) for k in ("w_ff_gate", "w_ff_up", "w_ff_down"))
    out["fgu"] = np.ascontiguousarray(np.stack([gu(wg[j], wu[j]) for j in range(2)]))
    out["fdn"] = np.ascontiguousarray(np.stack([dn(wd[j]) for j in range(2)]))
    eg, eu, ed = (np.asarray(inp[k], f) for k in ("w_exp_gate", "w_exp_up", "w_exp_down"))
    out["egu"] = np.ascontiguousarray(
        np.stack([np.stack([gu(eg[j, e], eu[j, e]) for e in range(NEXP)]) for j in range(2)]))
    out["edn"] = np.ascontiguousarray(
        np.stack([np.stack([dn(ed[j, e]) for e in range(NEXP)]) for j in range(2)]))
    wr = np.asarray(inp["w_router"], f)
    out["wr"] = np.ascontiguousarray(wr.reshape(2, 8, 128, 8).transpose(0, 2, 1, 3))
    c = {}
    c["ident"] = np.eye(128, dtype=f)
    kk = np.arange(128)
    c["tri"] = (kk[:, None] <= kk[None, :]).astype(f)
    bo = np.zeros((128, 128), f)
    bo[:64, :64] = 1.0
    bo[64:, 64:] = 1.0
    c["bones"] = bo
    c["ones"] = np.ones((128, 128), f)
    cols = np.zeros((128, 4), f)
    inv64 = (1.0 / (THETA ** (np.arange(0, 16, 2, dtype=np.float32) / 16))).astype(f)
    inv32 = (1.0 / (THETA ** (np.arange(0, 8, 2, dtype=np.float32) / 8))).astype(f)
    for p in range(128):
        j = p % 64
        if j < 8:
            cols[p, 0], cols[p, 1] = inv64[j], -1.0
        elif j < 16:
            cols[p, 0], cols[p, 1] = inv64[j - 8], 1.0
        j = p % 32
        if j < 4:
            cols[p, 2], cols[p, 3] = inv32[j], -1.0
        elif j < 8:
            cols[p, 2], cols[p, 3] = inv32[j - 4], 1.0
    c["ropec"] = cols
    pm = np.zeros((2, 128, 128), f)
    for m in range(128):
        j = m % 64
        k = m + 8 if j < 8 else (m - 8 if j < 16 else m)
        pm[0, k, m] = 1.0
        j = m % 32
        k = m + 4 if j < 4 else (m - 4 if j < 8 else m)
        pm[1, k, m] = 1.0
    c["perm"] = pm
    ind = np.zeros((128, SEQ), f)
    for g in range(2):
        for n in range(8):
            ind[64 * g + n, 256 * n:256 * (n + 1)] = 1.0
    c["ind"] = ind
    c["ramp"] = np.ascontiguousarray(np.broadcast_to((-2e-7 * np.arange(SEQ, dtype=np.float64)).astype(f)[None, :], (128, SEQ)))
    c["cvec"] = np.ascontiguousarray(np.broadcast_to((2.0 ** -(np.arange(NBIS, dtype=np.float64) + 1)).astype(f)[None, :], (128, NBIS)))
    sel = np.zeros((8, NEXP, 128), f)
    for e in range(NEXP):
        sel[e, e, :] = 1.0
    c["sel"] = sel.reshape(8, NEXP * 128)
    out.update(c)
    return out


W_SHAPES = None


def build(shapes, nlayers=L, dbg=(), stop=None):
    nc = bass.Bass("TRN2", target_bir_lowering=False)
    dr = {}
    for name, (shp, dt) in shapes.items():
        dr[name] = nc.dram_tensor(name, list(shp), dt, kind="ExternalInput").ap()
    out_d = nc.dram_tensor("outT", [D, SEQ], F32, kind="ExternalOutput").ap()
    dbg_d = {}

    with ExitStack() as st:
        S = Sched(nc, st)

        uid = [0]

        def sb(name, shape, dt, stack=st):
            uid[0] += 1
            return TB(stack.enter_context(nc.sbuf_tensor(f"sb{uid[0]}_{name}", list(shape), dt)))

        psb = [TB(st.enter_context(nc.psum_tensor(f"ps{i}", [128, 512], F32))) for i in range(7)]
        pst = TB(st.enter_context(nc.psum_tensor("pst", [128, 1024], BF16)))
        for t_ in psb + [pst]:
            t_.b().psum = True
        rings = {"r1": [0, 1, 2], "acc": [3, 4], "r2": [5, 6], "r4": [0, 1, 2, 3], "r3": [4, 5, 6],
                 "s4": [0, 1, 2, 3], "acc2": [4, 5], "m1": [6]}
        rpos = {k: 0 for k in rings}

        def ps(ring):
            i = rings[ring][rpos[ring] % len(rings[ring])]
            rpos[ring] += 1
            return psb[i]

        xT = sb("xT", [128, KC, SEQ], F32)
        hT = sb("hT", [128, KC, SEQ], BF16)
        cos64 = sb("cos64", [128, SEQ], BF16)
        sin64 = sb("sin64", [128, SEQ], BF16)
        cos32 = sb("cos32", [128, SEQ], BF16)
        sin32 = sb("sin32", [128, SEQ], BF16)
        wring = [sb(f"wr{i}", [128, 2048], BF16) for i in range(3)]
        wpos = [0]
        scr = [sb(f"scr{i}", [128, 512], F32) for i in range(4)]
        spos = [0]
        identb = sb("identb", [128, 128], BF16)
        identf = sb("identf", [128, 128], F32)
        trib = sb("trib", [128, 128], BF16)
        bonesf = sb("bonesf", [128, 128], F32)
        onesf = sb("onesf", [128, 128], F32)
        ropec = sb("ropec", [128, 4], F32)
        modc = sb("modc", [128, L, 48], F32)
        scaleA = sb("scaleA", [128, L, 8], F32)
        scaleF = sb("scaleF", [128, L, 8], F32)
        gattn = sb("gattn", [128, L, 8], F32)
        gffn = sb("gffn", [128, L, 8], F32)
        gfin = sb("gfin", [128, 8], F32)
        gsub = sb("gsub", [128, L], F32)
        nlam = sb("nlam", [128, L], F32)
        cact = sb("cact", [128, 8], BF16)
        rstd = sb("rstd", [128, 512], F32)

        negbig_reg = nc.gpsimd.to_reg(NEG_BIG)

        def wbuf():
            w = wring[wpos[0] % 3]
            wpos[0] += 1
            return w

        def scratch():
            s = scr[spos[0] % 4]
            spos[0] += 1
            return s

        def DBG(name, ap, shape, dt, reads):
            if name not in dbg:
                return
            d = nc.dram_tensor("dbg_" + name, list(shape), dt, kind="ExternalOutput").ap()
            dbg_d[name] = d
            S.dma("dsync", lambda e: e.dma_start(out=d, in_=ap), reads=reads)

        def V(fn, r=(), w=()):
            S.op("vector", fn, r, w)

        def A(fn, r=(), w=()):
            S.op("scalar", fn, r, w)

        def G(fn, r=(), w=()):
            S.op("gpsimd", fn, r, w)

        def PE(fn, r=(), w=(), mode="d"):
            S.op("tensor", fn, r, w, mode=mode)

        def load(q, dst_ap, src_ap, w, cast=False):
            if cast:
                S.dma(q, lambda e: e.dma_start(out=dst_ap, in_=src_ap, max_dma_last_dim=4096), writes=w)
            else:
                S.dma(q, lambda e: e.dma_start(out=dst_ap, in_=src_ap), writes=w)

        load("dpool", identb.t[:], dr["ident"][:, :], [identb.b()], cast=True)
        load("dsync", identf.t[:], dr["ident"][:, :], [identf.b()])
        load("dpool", trib.t[:], dr["tri"][:, :], [trib.b()], cast=True)
        permb = sb("permb", [128, 2, 128], BF16)
        for i_ in range(2):
            load("dpool", permb.t[:, i_, :], dr["perm"][i_], [permb.b()], cast=True)
        load("dsync", bonesf.t[:], dr["bones"][:, :], [bonesf.b()])
        load("dsync", onesf.t[:], dr["ones"][:, :], [onesf.b()])
        load("dsync", ropec.t[:], dr["ropec"][:, :], [ropec.b()])
        for l in range(L):
            load("dsync", gattn.t[:, l, :], dr["gattn"][l], [gattn.b()])
            load("dsync", gffn.t[:, l, :], dr["gffn"][l], [gffn.b()])
            load("dsync", gsub.t[:, l:l + 1], dr["gsub"][l], [gsub.b()])
        load("dsync", gfin.t[:], dr["gfin"][:, :], [gfin.b()])
        xv = dr["xT"].rearrange("(k p) t -> p k t", p=128)
        for k in range(KC):
            load("dsync", xT.t[:, k, :], xv[:, k, :], [xT.b(k, c) for c in range(NCH)])

        with ExitStack() as ps0:
            posi = sb("posi", [128, SEQ], I32, ps0)
            posf = sb("posf", [128, SEQ], F32, ps0)
            ang = sb("ang", [128, SEQ], F32, ps0)
            kf = sb("kf", [128, SEQ], F32, ps0)
            ki = sb("ki", [128, SEQ], I32, ps0)
            tmp = sb("tmpw", [128, SEQ], F32, ps0)
            ccol = sb("ccol", [128, 8], F32, ps0)
            badac = sb("badac", [128, L, 48], F32, ps0)
            dl = sb("dl", [128, L, 128], F32, ps0)
            lt = sb("lt", [128, 8], F32, ps0)
            pring = [sb(f"pw{i}", [128, 2048], BF16, ps0) for i in range(4)] + wring
            ppos = [0]
            load("dsync", posi.t[:], dr["pos"][0, :].partition_broadcast(128), [posi.b()])
            load("dsync", ccol.t[:], dr["ccol"][:, :], [ccol.b()])
            for l in range(L):
                load("dsync", badac.t[:, l, :], dr["bada"][l], [badac.b()])
                load("dsync", dl.t[:, l, :], dr["dlam"][l, 0, :].partition_broadcast(128), [dl.b()])
            V(lambda e: e.tensor_copy(posf.t[:], posi.t[:]), [posi.b()], [posf.b()])

            def table(dst, invc, sgnc, shift):
                V(lambda e: e.tensor_scalar(ang.t[:], posf.t[:], ropec.t[:, invc:invc + 1], shift,
                                            op0=ALU.mult, op1=ALU.add), [posf.b(), ropec.b()], [ang.b()])
                V(lambda e: e.tensor_scalar(kf.t[:], ang.t[:], 1.0 / (2 * PI), None, op0=ALU.mult),
                  [ang.b()], [kf.b()])
                V(lambda e: e.tensor_copy(ki.t[:], kf.t[:]), [kf.b()], [ki.b()])
                V(lambda e: e.tensor_copy(kf.t[:], ki.t[:]), [ki.b()], [kf.b()])
                C1 = 6.28125
                C2 = 2 * PI - C1
                V(lambda e: e.scalar_tensor_tensor(ang.t[:], kf.t[:], -C1, ang.t[:], op0=ALU.mult, op1=ALU.add),
                  [kf.b()], [ang.b()])
                V(lambda e: e.scalar_tensor_tensor(ang.t[:], kf.t[:], -C2, ang.t[:], op0=ALU.mult, op1=ALU.add),
                  [kf.b()], [ang.b()])
                V(lambda e: e.tensor_scalar(tmp.t[:], ang.t[:], PI, -2 * PI, op0=ALU.is_gt, op1=ALU.mult),
                  [ang.b()], [tmp.b()])
                V(lambda e: e.tensor_tensor(ang.t[:], ang.t[:], tmp.t[:], ALU.add), [tmp.b()], [ang.b()])
                V(lambda e: e.tensor_scalar(tmp.t[:], ang.t[:], -PI, 2 * PI, op0=ALU.is_lt, op1=ALU.mult),
                  [ang.b()], [tmp.b()])
                V(lambda e: e.tensor_tensor(ang.t[:], ang.t[:], tmp.t[:], ALU.add), [tmp.b()], [ang.b()])
                V(lambda e: e.tensor_scalar(ang.t[:], ang.t[:], 3.1415925, -3.1415925, op0=ALU.min, op1=ALU.max),
                  [], [ang.b()])
                if sgnc is None:
                    A(lambda e: e.activation(dst.t[:], ang.t[:], AF.Sin), [ang.b()], [dst.b()])
                else:
                    A(lambda e: e.activation(tmp.t[:], ang.t[:], AF.Sin), [ang.b()], [tmp.b()])
                    V(lambda e: e.tensor_scalar(dst.t[:], tmp.t[:], ropec.t[:, sgnc:sgnc + 1], None, op0=ALU.mult),
                      [tmp.b(), ropec.b()], [dst.b()])

            table(sin64, 0, 1, 0.0)
            table(cos64, 0, None, PI / 2)
            table(sin32, 2, 3, 0.0)
            table(cos32, 2, None, PI / 2)
            A(lambda e: e.activation(cact.t[:], ccol.t[:], AF.Silu), [ccol.b()], [cact.b()])
            for l in range(L):
                mp = ps("r2")
                for g in range(24):
                    wt = pring[ppos[0] % 7]
                    ppos[0] += 1
                    wv = wt.t[:, 0:2048].rearrange("p (t k n) -> p t k n", t=2, k=8)
                    load("dpool", wt.t[:, 0:2048], dr["wada"][l, g].rearrange("p t k n -> p (t k n)"), [wt.b()], cast=True)
                    for t in range(2):
                        j = 2 * g + t
                        for k in range(KC):
                            PE(lambda e, k=k, t=t, j=j: e.matmul(mp.t[:, j:j + 1], wv[:, t, k, :], cact.t[:, k:k + 1],
                                                                 start=(k == 0), stop=(k == KC - 1), skip_group_check=True),
                               [wt.b(), cact.b()], [mp.b()])
                V(lambda e, l=l: e.tensor_tensor(modc.t[:, l, :], mp.t[:, 0:48], badac.t[:, l, :], ALU.add),
                  [mp.b(), badac.b()], [modc.b()])
                V(lambda e, l=l: e.scalar_tensor_tensor(scaleA.t[:, l, :], modc.t[:, l, 8:16], 1.0, gattn.t[:, l, :],
                                                        op0=ALU.add, op1=ALU.mult), [modc.b(), gattn.b()], [scaleA.b()])
                V(lambda e, l=l: e.scalar_tensor_tensor(scaleF.t[:, l, :], modc.t[:, l, 32:40], 1.0, gffn.t[:, l, :],
                                                        op0=ALU.add, op1=ALU.mult), [modc.b(), gffn.b()], [scaleF.b()])
                lam_init = 0.8 - 0.6 * math.exp(-0.3 * l)
                V(lambda e, l=l: e.tensor_tensor(tmp.t[:, 0:32], dl.t[:, l, 0:32], dl.t[:, l, 32:64], ALU.mult),
                  [dl.b()], [tmp.b()])
                V(lambda e, l=l: e.tensor_tensor(tmp.t[:, 32:64], dl.t[:, l, 64:96], dl.t[:, l, 96:128], ALU.mult),
                  [dl.b()], [tmp.b()])
                V(lambda e: e.tensor_reduce(lt.t[:, 0:2], tmp.t[:, 0:64].rearrange("p (a b) -> p a b", a=2),
                                            mybir.AxisListType.X, ALU.add), [tmp.b()], [lt.b()])
                A(lambda e: e.activation(lt.t[:, 2:4], lt.t[:, 0:2], AF.Exp), [lt.b()], [lt.b()])
                V(lambda e, l=l, li=lam_init: e.scalar_tensor_tensor(nlam.t[:, l:l + 1], lt.t[:, 3:4], -li, lt.t[:, 2:3],
                                                                     op0=ALU.add, op1=ALU.subtract), [lt.b()], [nlam.b()])
                V(lambda e, l=l, li=lam_init: e.tensor_scalar(gsub.t[:, l:l + 1], gsub.t[:, l:l + 1], 1.0 - li, None,
                                                              op0=ALU.mult), [], [gsub.b()])
            DBG("modc", modc.t[:], [128, L, 48], F32, [modc.b()])
            DBG("cos64", cos64.t[:], [128, SEQ], BF16, [cos64.b()])
            DBG("sin64", sin64.t[:], [128, SEQ], BF16, [sin64.b()])
            DBG("sin32", sin32.t[:], [128, SEQ], BF16, [sin32.b()])
            DBG("nlam", nlam.t[:], [128, L], F32, [nlam.b()])
            S.barrier()

        def norm_mod(scale_ap, shift_ap, eps, hf_cb=None, out_f32=None):
            for c in range(NCH):
                cs = slice(c * 512, (c + 1) * 512)
                ss = ps("r2")
                for k in range(KC):
                    sq = scratch()
                    A(lambda e, k=k, sq=sq: e.activation(sq.t[:], xT.t[:, k, cs], AF.Square), [xT.b(k, c)], [sq.b()])
                    PE(lambda e, k=k, sq=sq: e.matmul(ss.t[:], onesf.t[:], sq.t[:], start=(k == 0), stop=(k == KC - 1)),
                       [sq.b(), onesf.b()], [ss.b()], mode="f32")
                V(lambda e: e.tensor_scalar(rstd.t[:], ss.t[:], 1.0 / D, eps, op0=ALU.mult, op1=ALU.add), [ss.b()], [rstd.b()])
                A(lambda e: e.activation(rstd.t[:], rstd.t[:], AF.Ln), [], [rstd.b()])
                A(lambda e: e.activation(rstd.t[:], rstd.t[:], AF.Exp, scale=-0.5), [], [rstd.b()])
                for k in range(KC):
                    tm = scratch()
                    V(lambda e, k=k, tm=tm: e.tensor_tensor(tm.t[:], xT.t[:, k, cs], rstd.t[:], ALU.mult),
                      [xT.b(k, c), rstd.b()], [tm.b()])
                    if out_f32 is not None:
                        A(lambda e, k=k, tm=tm: e.activation(out_f32.t[:, k, cs], tm.t[:], AF.Identity,
                                                             scale=scale_ap[:, k:k + 1]), [tm.b()], [out_f32.b(k, c)])
                    elif hf_cb is not None:
                        hf = scratch()
                        A(lambda e, k=k, tm=tm, hf=hf: e.activation(hf.t[:], tm.t[:], AF.Identity, bias=shift_ap[:, k:k + 1],
                                                                    scale=scale_ap[:, k:k + 1]), [tm.b()], [hf.b()])
                        G(lambda e, k=k, hf=hf: e.tensor_copy(hT.t[:, k, cs], hf.t[:]), [hf.b()], [hT.b(k, c)])
                        hf_cb(c, k, hf)
                    else:
                        A(lambda e, k=k, tm=tm: e.activation(hT.t[:, k, cs], tm.t[:], AF.Identity, bias=shift_ap[:, k:k + 1],
                                                             scale=scale_ap[:, k:k + 1]), [tm.b()], [hT.b(k, c)])

        hT_all = lambda: [hT.b(k, c) for k in range(KC) for c in range(NCH)]

        def proj_tile(l, t, dst_fn, dst_bufs_fn, kind):
            cosT, sinT = (cos64, sin64) if kind == 64 else (cos32, sin32)
            pi_ = 0 if kind == 64 else 1
            wt = wbuf()
            wv = wt.t[:, 0:1024].rearrange("p (k n) -> p k n", k=8)
            load("dpool", wt.t[:, 0:1024], dr["winq"][l, t].rearrange("p k n -> p (k n)"), [wt.b()], cast=True)

            def main(c):
                cs = slice(c * 512, (c + 1) * 512)
                pa = ps("r4")
                for k in range(KC):
                    PE(lambda e, k=k: e.matmul(pa.t[:], wv[:, k, :], hT.t[:, k, cs], start=(k == 0), stop=(k == KC - 1)),
                       [wt.b(), hT.b(k, c)], [pa.b()])
                xb = scratch()
                xbv = xb.t[:, :].bitcast(BF16)[:, 0:512]
                A(lambda e: e.activation(xbv, pa.t[:], AF.Copy), [pa.b()], [xb.b()])
                return pa, xb, xbv

            def rot(c, pa, xb, xbv):
                cs = slice(c * 512, (c + 1) * 512)
                pb = ps("r4")
                PE(lambda e: e.matmul(pb.t[:], permb.t[:, pi_, :], xbv, start=True, stop=True), [permb.b(), xb.b()], [pb.b()])
                t1, t2 = scratch(), scratch()
                V(lambda e: e.tensor_tensor(t1.t[:], pa.t[:], cosT.t[:, cs], ALU.mult), [pa.b(), cosT.b()], [t1.b()])
                V(lambda e: e.tensor_tensor(t2.t[:], pb.t[:], sinT.t[:, cs], ALU.mult), [pb.b(), sinT.b()], [t2.b()])
                G(lambda e: e.tensor_tensor(dst_fn(c), t1.t[:], t2.t[:], ALU.add), [t1.b(), t2.b()], dst_bufs_fn(c))
            prev = None
            for c in range(NCH):
                cur = main(c)
                if prev is not None:
                    rot(c - 1, *prev)
                prev = cur
            rot(NCH - 1, *prev)

        def proj_v(l, name, ncols, evac):
            wt = wbuf()
            nk = 8 if ncols <= 256 else 4
            parts = []
            for kh in range(8 // nk):
                w = wt if kh == 0 else wbuf()
                load("dpool", w.t[:, 0:nk * ncols],
                     dr[name][l][:, kh * nk:(kh + 1) * nk, :].rearrange("p k n -> p (k n)"), [w.b()], cast=True)
                parts.append(w)
            for t in range(NT):
                pv = ps("r3")
                for k in range(KC):
                    w = parts[k // nk]
                    kk = k % nk
                    PE(lambda e, k=k, w=w, kk=kk: e.matmul(pv.t[:, 0:ncols], hT.t[:, k, t * 128:(t + 1) * 128],
                                                            w.t[:, kk * ncols:(kk + 1) * ncols], start=(k == 0), stop=(k == KC - 1)),
                       [w.b(), hT.b(k, t // 4)], [pv.b()])
                evac(t, pv)

        def out_proj(l, wo, ntile, yT_fn, y_bufs, t0, n, gcol0, ring="r2"):
            per_bank = 512 // n
            c = t0 // 512
            for d0 in range(0, KC, per_bank):
                pw = ps(ring)
                for dd in range(per_bank):
                    d = d0 + dd
                    for p in range(ntile):
                        PE(lambda e, d=d, dd=dd, p=p: e.matmul(pw.t[:, dd * n:(dd + 1) * n], wo.t[:, p, d * 128:(d + 1) * 128],
                                                                yT_fn(p), start=(p == 0), stop=(p == ntile - 1), skip_group_check=True),
                           [wo.b()] + y_bufs, [pw.b()])
                for dd in range(per_bank):
                    d = d0 + dd
                    V(lambda e, d=d, dd=dd: e.scalar_tensor_tensor(xT.t[:, d, t0:t0 + n], pw.t[:, dd * n:(dd + 1) * n],
                                                                   modc.t[:, l, gcol0 + d:gcol0 + d + 1], xT.t[:, d, t0:t0 + n],
                                                                   op0=ALU.mult, op1=ALU.add),
                      [pw.b(), modc.b()], [xT.b(d, c)])

        def group_A(l):
            with ExitStack() as gs:
                qaT = sb("qaT", [128, 3, SEQ], BF16, gs)
                kaT = sb("kaT", [128, SEQ], BF16, gs)
                qiT = sb("qiT", [128, 2, SEQ], BF16, gs)
                kiT = sb("kiT", [128, SEQ], BF16, gs)
                va = sb("va", [128, NT, 192], BF16, gs)
                wi = sb("wi", [128, NT, 4], F32, gs)
                work = sb("work", [128, SEQ], F32, gs)
                mask = sb("mask", [128, SEQ], BF16, gs)
                maskT = sb("maskT", [128, SEQ], BF16, gs)
                Pt = [sb(f"PA{i}", [128, 768], BF16, gs) for i in range(2)]
                yA = [sb(f"yA{i}", [128, 3, 128], BF16, gs) for i in range(2)]
                woA = sb("woA", [128, 3, 1024], BF16, gs)
                m8 = sb("m8", [128, 8], F32, gs)
                ramp = sb("ramp", [128, SEQ], F32, gs)
                bst = sb("bst", [128, 8], F32, gs)
                tj = sb("tj", [128, NBIS], F32, gs)
                cvec = sb("cvec", [128, NBIS], F32, gs)
                load("dsync", cvec.t[:], dr["cvec"][:, :], [cvec.b()])
                load("dsync", ramp.t[:], dr["ramp"][:, :], [ramp.b()])
                for p in range(3):
                    load("dpool", woA.t[:, p, :], dr["wout"][l, p], [woA.b()], cast=True)
                for p in range(3):
                    proj_tile(l, p, lambda c, p=p: qaT.t[:, p, c * 512:(c + 1) * 512], lambda c, p=p: [qaT.b(p, c)], 64)
                proj_tile(l, 3, lambda c: kaT.t[:, c * 512:(c + 1) * 512], lambda c: [kaT.b(c)], 64)
                for p in range(2):
                    proj_tile(l, 4 + p, lambda c, p=p: qiT.t[:, p, c * 512:(c + 1) * 512], lambda c, p=p: [qiT.b(p, c)], 64)
                proj_tile(l, 6, lambda c: kiT.t[:, c * 512:(c + 1) * 512], lambda c: [kiT.b(c)], 64)
                G(lambda e: e.memset(va.t[:, :, 64:128], 1.0), [], [va.b(t) for t in range(NT)])

                def evac(t, pv):
                    A(lambda e: e.activation(va.t[:, t, 0:64], pv.t[:, 0:64], AF.Copy), [pv.b()], [va.b(t)])
                    A(lambda e: e.activation(va.t[:, t, 128:192], pv.t[:, 0:64], AF.Copy), [pv.b()], [va.b(t)])
                    V(lambda e: e.tensor_scalar(wi.t[:, t, :], pv.t[:, 64:68], 1.0 / 16.0, None, op0=ALU.mult), [pv.b()], [wi.b(t)])
                proj_v(l, "wva", 68, evac)
                if "qaT" in dbg and l == 0:
                    DBG("qaT", qaT.t[:], [128, 3, SEQ], BF16, [qaT.b(p, c) for p in range(3) for c in range(4)])
                    DBG("kaT", kaT.t[:], [128, SEQ], BF16, [kaT.b(c) for c in range(4)])
                    DBG("va", va.t[:], [128, NT, 192], BF16, [va.b(t) for t in range(NT)])
                    DBG("wi", wi.t[:], [128, NT, 4], F32, [wi.b(t) for t in range(NT)])

                def a_idx(i):
                    W = (i + 1) * 128
                    q0 = i * 128
                    for kc in range(0, W, 512):
                        n = min(512, W - kc)
                        for h in range(4):
                            half = slice(64 * (h % 2), 64 * (h % 2) + 64)
                            pi_ = ps("r2")
                            PE(lambda e, h=h, half=half: e.matmul(pi_.t[:, 0:n], qiT.t[half, h // 2, q0:q0 + 128], kiT.t[half, kc:kc + n],
                                                                   start=True, stop=True),
                               [qiT.b(h // 2, i // 4), kiT.b(kc // 512)], [pi_.b()], mode="k64")
                            r = scratch()
                            A(lambda e: e.activation(r.t[:, 0:n], pi_.t[:, 0:n], AF.Relu), [pi_.b()], [r.b()])
                            if h == 0:
                                V(lambda e: e.scalar_tensor_tensor(work.t[:, kc:kc + n], r.t[:, 0:n], wi.t[:, i, 0:1], ramp.t[:, kc:kc + n],
                                                                   op0=ALU.mult, op1=ALU.add),
                                  [r.b(), wi.b(i), ramp.b()], [work.b()])
                            else:
                                V(lambda e, h=h: e.scalar_tensor_tensor(work.t[:, kc:kc + n], r.t[:, 0:n], wi.t[:, i, h:h + 1],
                                                                        work.t[:, kc:kc + n], op0=ALU.mult, op1=ALU.add),
                                  [r.b(), wi.b(i)], [work.b()])
                    if i >= 2:
                        V(lambda e: e.tensor_reduce(bst.t[:, 0:1], work.t[:, 0:W], mybir.AxisListType.X, ALU.max), [work.b()], [bst.b()])
                        V(lambda e: e.tensor_reduce(bst.t[:, 1:2], work.t[:, 0:W], mybir.AxisListType.X, ALU.min), [work.b()], [bst.b()])
                        V(lambda e: e.tensor_tensor(bst.t[:, 2:3], bst.t[:, 0:1], bst.t[:, 1:2], ALU.subtract), [], [bst.b()])
                        V(lambda e: e.tensor_scalar(tj.t[:], cvec.t[:], bst.t[:, 2:3], None, op0=ALU.mult), [cvec.b(), bst.b()], [tj.b()])
                        V(lambda e: e.tensor_tensor(bst.t[:, 4:5], bst.t[:, 1:2], tj.t[:, 0:1], ALU.add), [tj.b()], [bst.b()])
                    G(lambda e: e.affine_select(work.t[:, q0:q0 + 128], work.t[:, q0:q0 + 128], [[-1, 128]], ALU.is_ge, negbig_reg,
                                                base=0, channel_multiplier=1), [], [work.b()])
                    if "idx" in dbg and l == 0 and i == 5:
                        DBG("idx", work.t[:, 0:W], [128, W], F32, [work.b()])
                    if i >= 2:
                        for j in range(NBIS):
                            V(lambda e: e.tensor_scalar(mask.t[:, 0:W], work.t[:, 0:W], bst.t[:, 4:5], None, op0=ALU.is_ge, op1=ALU.add,
                                                        accum_out=bst.t[:, 5:6]), [work.b(), bst.b()], [mask.b(), bst.b()])
                            V(lambda e, j=j: e.tensor_scalar(bst.t[:, 6:7], bst.t[:, 5:6], 256.0, tj.t[:, j:j + 1], op0=ALU.is_ge, op1=ALU.mult),
                              [tj.b()], [bst.b()])
                            jn = min(j + 1, NBIS - 1)
                            V(lambda e, jn=jn: e.scalar_tensor_tensor(bst.t[:, 4:5], bst.t[:, 6:7], tj.t[:, jn:jn + 1], bst.t[:, 4:5],
                                                                      op0=ALU.subtract, op1=ALU.add), [tj.b()], [bst.b()])
                        V(lambda e: e.tensor_scalar(mask.t[:, 0:W], work.t[:, 0:W], bst.t[:, 4:5], None, op0=ALU.is_ge),
                          [work.b(), bst.b()], [mask.b()])
                    else:
                        V(lambda e: e.tensor_scalar(mask.t[:, 0:W], work.t[:, 0:W], -1.0e29, None, op0=ALU.is_ge),
                          [work.b()], [mask.b()])
                    if "mask" in dbg and l == 0 and i == 5:
                        DBG("mask", mask.t[:, 0:W], [128, W], BF16, [mask.b()])

                def a_T(i):
                    for k0 in range(0, i + 1, 4):
                        nb = min(4, i + 1 - k0)
                        for j in range(nb):
                            kt = k0 + j
                            PE(lambda e, kt=kt, j=j: e.transpose(pst.t[:, j * 128:(j + 1) * 128], mask.t[:, kt * 128:(kt + 1) * 128], identb.t[:]),
                               [mask.b(), identb.b()], [pst.b()], mode="T")
                        A(lambda e, k0=k0, nb=nb: e.activation(maskT.t[:, k0 * 128:(k0 + nb) * 128], pst.t[:, 0:nb * 128], AF.Copy),
                          [pst.b()], [maskT.b()])

                def a_att(i):
                    W = (i + 1) * 128
                    q0 = i * 128
                    oX, oY = psb[3], psb[4]
                    for kt in range(i + 1):
                        ks = slice(kt * 128, (kt + 1) * 128)
                        sE, sO = ps("r1"), ps("r1")
                        for p in range(3):
                            PE(lambda e, p=p: e.matmul(sE.t[:, p * 128:(p + 1) * 128], kaT.t[0:64, ks], qaT.t[0:64, p, q0:q0 + 128],
                                                        start=True, stop=True), [kaT.b(kt // 4), qaT.b(p, i // 4)], [sE.b()], mode="k64")
                            PE(lambda e, p=p: e.matmul(sO.t[:, p * 128:(p + 1) * 128], kaT.t[64:128, ks], qaT.t[64:128, p, q0:q0 + 128],
                                                        start=True, stop=True), [kaT.b(kt // 4), qaT.b(p, i // 4)], [sO.b()], mode="k64")
                        P = Pt[kt % 2]
                        A(lambda e, P=P: e.activation(P.t[:, 0:384], sE.t[:, 0:384], AF.Exp, scale=0.125), [sE.b()], [P.b()])
                        A(lambda e, P=P: e.activation(P.t[:, 384:768], sO.t[:, 0:384], AF.Exp, scale=0.125), [sO.b()], [P.b()])
                        G(lambda e, P=P, ks=ks: e.tensor_tensor(P.t[:, :].rearrange("p (h q) -> p h q", h=6),
                                                                P.t[:, :].rearrange("p (h q) -> p h q", h=6),
                                                                maskT.t[:, ks].unsqueeze(1).to_broadcast([128, 6, 128]), ALU.mult),
                          [maskT.b()], [P.b()])
                        for p in range(3):
                            PE(lambda e, p=p, P=P, kt=kt: e.matmul(oX.t[:, p * 128:(p + 1) * 128], va.t[:, kt, 0:128], P.t[:, p * 128:(p + 1) * 128],
                                                                    start=(kt == 0 and p == 0), stop=(kt == i), skip_group_check=True),
                               [va.b(kt), P.b()], [oX.b()])
                            PE(lambda e, p=p, P=P, kt=kt: e.matmul(oY.t[:, p * 128:(p + 1) * 128], va.t[:, kt, 64:192], P.t[:, 384 + p * 128:384 + (p + 1) * 128],
                                                                    start=(kt == 0 and p == 0), stop=(kt == i), skip_group_check=True),
                               [va.b(kt), P.b()], [oY.b()])
                    y = yA[i % 2]
                    r1, r2 = scratch(), scratch()
                    A(lambda e: e.activation(r1.t[0:64, 0:384], oX.t[64:128, 0:384], AF.Ln), [oX.b()], [r1.b()])
                    A(lambda e: e.activation(r1.t[0:64, 0:384], r1.t[0:64, 0:384], AF.Exp, scale=-1.0), [], [r1.b()])
                    V(lambda e: e.tensor_tensor(y.t[0:64, :, :], oX.t[0:64, 0:384].rearrange("p (a b) -> p a b", a=3),
                                                r1.t[0:64, 0:384].rearrange("p (a b) -> p a b", a=3), ALU.mult), [oX.b(), r1.b()], [y.b()])
                    A(lambda e: e.activation(r2.t[64:128, 0:384], oY.t[0:64, 0:384], AF.Ln), [oY.b()], [r2.b()])
                    A(lambda e: e.activation(r2.t[64:128, 0:384], r2.t[64:128, 0:384], AF.Exp, scale=-1.0), [], [r2.b()])
                    V(lambda e: e.tensor_tensor(y.t[64:128, :, :], oY.t[64:128, 0:384].rearrange("p (a b) -> p a b", a=3),
                                                r2.t[64:128, 0:384].rearrange("p (a b) -> p a b", a=3), ALU.mult), [oY.b(), r2.b()], [y.b()])
                    if "yA" in dbg and l == 0 and i == 5:
                        DBG("yA", y.t[:], [128, 3, 128], BF16, [y.b()])
                    out_proj(l, woA, 3, lambda p, y=y: y.t[:, p, :], [y.b()], q0, 128, 16)

                a_idx(0)
                a_T(0)
                for i in range(NT):
                    if i + 1 < NT:
                        a_idx(i + 1)
                    a_att(i)
                    if i + 1 < NT:
                        a_T(i + 1)

        def attn_core(c, o, qk_emit, scale, v_ap, v_buf, Pt):
            nk = 4 * c + 4
            batches = [list(range(k0, min(k0 + 2, nk))) for k0 in range(0, nk, 2)]

            def do_qk(bt):
                info = []
                for kt in bt:
                    qs0 = max(c * 512, kt * 128)
                    n = (c + 1) * 512 - qs0
                    off = qs0 - c * 512
                    s_ = ps("s4")
                    qk_emit(kt, s_, n, qs0, off)
                    info.append((kt, s_, n, off))
                for kt, s_, n, off in info:
                    P = Pt[kt % 4]
                    A(lambda e: e.activation(P.t[:, 0:n], s_.t[:, 0:n], AF.Exp, scale=scale), [s_.b()], [P.b()])
                    if kt >= 4 * c:
                        G(lambda e: e.tensor_tensor(P.t[:, 0:128], P.t[:, 0:128], trib.t[:], ALU.mult), [trib.b()], [P.b()])
                return info

            def do_pv(info):
                for kt, s_, n, off in info:
                    P = Pt[kt % 4]
                    PE(lambda e: e.matmul(o.t[:, off:off + n], v_ap(kt), P.t[:, 0:n],
                                          start=(kt == 0), stop=(kt == nk - 1), skip_group_check=True),
                       [v_buf(kt), P.b()], [o.b()])
            prev = None
            for bt in batches:
                cur = do_qk(bt)
                if prev is not None:
                    do_pv(prev)
                prev = cur
            do_pv(prev)

        def group_B(l):
            with ExitStack() as gs:
                qbT = sb("qbT", [128, 3, SEQ], BF16, gs)
                kbT = sb("kbT", [128, 3, SEQ], BF16, gs)
                vb = sb("vb", [128, NT, 384], BF16, gs)
                Pt = [sb(f"PB{i}", [128, 512], BF16, gs) for i in range(4)]
                yB = [sb(f"yB{i}", [128, 2, 512], BF16, gs) for i in range(2)]
                woB = sb("woB", [128, 2, 1024], BF16, gs)
                yd = [sb(f"yd{i}", [128, 512], F32, gs) for i in range(2)]
                y1 = sb("y1", [128, 512], F32, gs)
                for p in range(2):
                    load("dpool", woB.t[:, p, :], dr["wout"][l, 3 + p], [woB.b()], cast=True)
                for p in range(3):
                    proj_tile(l, 7 + p, lambda c, p=p: qbT.t[:, p, c * 512:(c + 1) * 512], lambda c, p=p: [qbT.b(p, c)], 32)
                for p in range(3):
                    proj_tile(l, 10 + p, lambda c, p=p: kbT.t[:, p, c * 512:(c + 1) * 512], lambda c, p=p: [kbT.b(p, c)], 32)
                G(lambda e: e.memset(vb.t[:, :, 64:128], 1.0), [], [vb.b(t) for t in range(NT)])
                G(lambda e: e.memset(vb.t[:, :, 256:320], 1.0), [], [vb.b(t) for t in range(NT)])

                def evac(t, pv):
                    A(lambda e: e.activation(vb.t[:, t, 0:64], pv.t[:, 0:64], AF.Copy), [pv.b()], [vb.b(t)])
                    A(lambda e: e.activation(vb.t[:, t, 128:192], pv.t[:, 64:128], AF.Copy), [pv.b()], [vb.b(t)])
                    A(lambda e: e.activation(vb.t[:, t, 192:256], pv.t[:, 128:192], AF.Copy), [pv.b()], [vb.b(t)])
                    A(lambda e: e.activation(vb.t[:, t, 320:384], pv.t[:, 192:256], AF.Copy), [pv.b()], [vb.b(t)])
                proj_v(l, "wvb", 256, evac)

                def kT_fn(g, ks):
                    return kbT.t[32 * (g % 3):32 * (g % 3) + 32, g // 3, ks]

                def qT_fn(g, qs):
                    return qbT.t[32 * (g % 3):32 * (g % 3) + 32, g // 3, qs]

                def v_fn(g, kt):
                    h = g // 2
                    base = 192 * (h // 2) + 64 * (h % 2)
                    return vb.t[:, kt, base:base + 128]

                def finish(c, g, o):
                    h, comp = g // 2, g % 2
                    val = slice(0, 64) if h % 2 == 0 else slice(64, 128)
                    den = slice(64, 128) if h % 2 == 0 else slice(0, 64)
                    ydt = yd[(h // 2) % 2]
                    r = scratch()
                    A(lambda e: e.activation(r.t[val, :], o.t[den, :], AF.Ln), [o.b()], [r.b()])
                    A(lambda e: e.activation(r.t[val, :], r.t[val, :], AF.Exp, scale=-1.0), [], [r.b()])
                    if comp == 0:
                        V(lambda e: e.tensor_tensor(y1.t[val, :], o.t[val, :], r.t[val, :], ALU.mult), [o.b(), r.b()], [y1.b()])
                    else:
                        r2 = scratch()
                        V(lambda e: e.tensor_tensor(r2.t[val, :], o.t[val, :], r.t[val, :], ALU.mult), [o.b(), r.b()], [r2.b()])
                        V(lambda e: e.scalar_tensor_tensor(ydt.t[val, :], r2.t[val, :], nlam.t[val, l:l + 1], y1.t[val, :],
                                                           op0=ALU.mult, op1=ALU.add), [r2.b(), y1.b(), nlam.b()], [ydt.b()])
                        if h % 2 == 1:
                            pair = h // 2
                            sq = scratch()
                            A(lambda e: e.activation(sq.t[:], ydt.t[:], AF.Square), [ydt.b()], [sq.b()])
                            ss = ps("m1")
                            PE(lambda e: e.matmul(ss.t[:], bonesf.t[:], sq.t[:], start=True, stop=True), [sq.b(), bonesf.b()], [ss.b()], mode="f32")
                            rs = scratch()
                            V(lambda e: e.tensor_scalar(rs.t[:], ss.t[:], 1.0 / 64, 1e-5, op0=ALU.mult, op1=ALU.add), [ss.b()], [rs.b()])
                            A(lambda e: e.activation(rs.t[:], rs.t[:], AF.Ln), [], [rs.b()])
                            A(lambda e: e.activation(rs.t[:], rs.t[:], AF.Exp, scale=-0.5), [], [rs.b()])
                            yb = yB[c % 2]
                            V(lambda e: e.scalar_tensor_tensor(yb.t[:, pair, :], ydt.t[:], gsub.t[:, l:l + 1], rs.t[:],
                                                               op0=ALU.mult, op1=ALU.mult), [ydt.b(), rs.b(), gsub.b()], [yb.b()])
                            if pair == 1:
                                if "yB" in dbg and l == 0 and c == 1:
                                    DBG("yB", yb.t[:], [128, 2, 512], BF16, [yb.b()])
                                out_proj(l, woB, 2, lambda p, yb=yb: yb.t[:, p, :], [yb.b()], c * 512, 512, 16, ring="m1")

                for c in range(NCH):
                    for g in range(8):
                        o = ps("acc2")

                        def qk_emit(kt, s_, n, qs0, off, g=g, c=c):
                            ks = slice(kt * 128, (kt + 1) * 128)
                            PE(lambda e: e.matmul(s_.t[:, 0:n], kT_fn(g, ks), qT_fn(g, slice(qs0, qs0 + n)), start=True, stop=True),
                               [kbT.b(g // 3, kt // 4), qbT.b(g // 3, c)], [s_.b()], mode="k32")
                        attn_core(c, o, qk_emit, 32 ** -0.5, lambda kt, g=g: v_fn(g, kt), lambda kt: vb.b(kt), Pt)
                        finish(c, g, o)

        def group_C(l):
            with ExitStack() as gs:
                qcT = sb("qcT", [128, 3, SEQ], BF16, gs)
                kcT = sb("kcT", [128, 3, SEQ], BF16, gs)
                vc = sb("vc", [128, NT, 576], BF16, gs)
                Pt = [sb(f"PC{i}", [128, 512], BF16, gs) for i in range(4)]
                yC = [sb(f"yC{i}", [128, 3, 512], BF16, gs) for i in range(1)]
                woC = sb("woC", [128, 3, 1024], BF16, gs)
                ind = sb("ind", [128, SEQ], BF16, gs)
                biasT = sb("biasT", [128, 3, 512], BF16, gs)
                kmf = sb("kmf", [128, 3, 8], F32, gs)
                kmb = sb("kmb", [128, 3, 8], BF16, gs)
                btm = sb("btm", [128, 6, 64], F32, gs)
                gt_ = sb("gt_", [128, 6, 8], F32, gs)
                m8 = sb("m8c", [128, 8], F32, gs)
                load("dpool", ind.t[:], dr["ind"][:, :], [ind.b()], cast=True)
                for p in range(3):
                    load("dpool", woC.t[:, p, :], dr["wout"][l, 5 + p], [woC.b()], cast=True)
                for p in range(3):
                    proj_tile(l, 13 + p, lambda c, p=p: qcT.t[:, p, c * 512:(c + 1) * 512], lambda c, p=p: [qcT.b(p, c)], 64)
                for p in range(3):
                    proj_tile(l, 16 + p, lambda c, p=p: kcT.t[:, p, c * 512:(c + 1) * 512], lambda c, p=p: [kcT.b(p, c)], 64)
                for pr in range(3):
                    G(lambda e, pr=pr: e.memset(vc.t[:, :, 192 * pr + 64:192 * pr + 128], 1.0), [], [vc.b(t) for t in range(NT)])

                def evac(t, pv):
                    for h in range(6):
                        base = 192 * (h // 2) + 128 * (h % 2)
                        A(lambda e, h=h, base=base: e.activation(vc.t[:, t, base:base + 64], pv.t[:, 64 * h:64 * h + 64], AF.Copy),
                          [pv.b()], [vc.b(t)])
                proj_v(l, "wvc", 384, evac)
                for p in range(3):
                    V(lambda e, p=p: e.tensor_reduce(kmf.t[:, p, :], kcT.t[:, p, :].rearrange("p (n k) -> p n k", n=8),
                                                     mybir.AxisListType.X, ALU.add), [kcT.b(p, c) for c in range(4)], [kmf.b()])
                V(lambda e: e.tensor_scalar(kmb.t[:], kmf.t[:], 1.0 / 256, None, op0=ALU.mult), [kmf.b()], [kmb.b()])
                if "kcT" in dbg and l == 0:
                    DBG("kcT", kcT.t[:], [128, 3, SEQ], BF16, [kcT.b(p, c) for p in range(3) for c in range(4)])
                    DBG("qcT", qcT.t[:], [128, 3, SEQ], BF16, [qcT.b(p, c) for p in range(3) for c in range(4)])

                def gating(c):
                    for tt in range(4):
                        t = 4 * c + tt
                        own = t // 2
                        gp = ps("m1")
                        for h in range(6):
                            half = slice(64 * (h % 2), 64 * (h % 2) + 64)
                            PE(lambda e, h=h, half=half: e.matmul(gp.t[:, h * 8:(h + 1) * 8], qcT.t[half, h // 2, t * 128:(t + 1) * 128],
                                                                   kmb.t[half, h // 2, :], start=True, stop=True),
                               [qcT.b(h // 2, c), kmb.b()], [gp.b()], mode="k64")
                        G(lambda e: e.memset(btm.t[:], 0.0), [], [btm.b()])
                        G(lambda e: e.memset(btm.t[:, :, 0:8], -MB), [], [btm.b()])
                        if own <= 3:
                            G(lambda e: e.memset(btm.t[:, :, 0:own + 1], 0.0), [], [btm.b()])
                        else:
                            V(lambda e: e.memset(gt_.t[:], NEG_BIG), [], [gt_.b()])
                            V(lambda e: e.tensor_copy(gt_.t[:, :, 0:own], gp.t[:, 0:48].rearrange("p (h n) -> p h n", h=6)[:, :, 0:own]),
                              [gp.b()], [gt_.b()])
                            for h in range(6):
                                V(lambda e, h=h: e.max(m8.t[:], gt_.t[:, h, :]), [gt_.b()], [m8.b()])
                                V(lambda e, h=h: e.tensor_scalar(btm.t[:, h, 0:own], gt_.t[:, h, 0:own], m8.t[:, 2:3], MB,
                                                                 op0=ALU.is_ge, op1=ALU.mult), [gt_.b(), m8.b()], [btm.b()])
                            V(lambda e: e.tensor_scalar(btm.t[:, :, 0:own], btm.t[:, :, 0:own], -MB, None, op0=ALU.add),
                              [], [btm.b()])
                            G(lambda e: e.memset(btm.t[:, :, own:own + 1], 0.0), [], [btm.b()])
                        for s2 in range(3):
                            tp = ps("r2")
                            PE(lambda e, s2=s2: e.transpose(tp.t[:, 0:128], btm.t[:, 2 * s2:2 * s2 + 2, :].rearrange("p a b -> p (a b)"), identf.t[:]),
                               [btm.b(), identf.b()], [tp.b()], mode="Tf")
                            A(lambda e, s2=s2: e.activation(biasT.t[:, s2, tt * 128:(tt + 1) * 128], tp.t[:, 0:128], AF.Copy),
                              [tp.b()], [biasT.b()])
                    if "biasT" in dbg and l == 0 and c == 3:
                        DBG("biasT", biasT.t[:], [128, 3, 512], BF16, [biasT.b()])

                for c in range(NCH):
                    if c >= 2:
                        gating(c)
                    for h in range(6):
                        o = ps("acc2")
                        hp = slice(64 * (h % 2), 64 * (h % 2) + 64)
                        base = 192 * (h // 2) + 64 * (h % 2)

                        def qk_emit(kt, s_, n, qs0, off, h=h, c=c, hp=hp):
                            ks = slice(kt * 128, (kt + 1) * 128)
                            qs = slice(qs0, qs0 + n)
                            nb = (c >= 2) and ((kt // 2) < 2 * c + 1)
                            PE(lambda e: e.matmul(s_.t[:, 0:n], kcT.t[hp, h // 2, ks], qcT.t[hp, h // 2, qs], start=True, stop=(not nb)),
                               [kcT.b(h // 2, kt // 4), qcT.b(h // 2, c)], [s_.b()], mode="k64")
                            if nb:
                                PE(lambda e: e.matmul(s_.t[:, 0:n], ind.t[hp, ks], biasT.t[hp, h // 2, off:off + n], start=False, stop=True),
                                   [ind.b(), biasT.b()], [s_.b()], mode="k64")
                        attn_core(c, o, qk_emit, 0.125, lambda kt, base=base: vc.t[:, kt, base:base + 128], lambda kt: vc.b(kt), Pt)
                        val = slice(0, 64) if h % 2 == 0 else slice(64, 128)
                        den = slice(64, 128) if h % 2 == 0 else slice(0, 64)
                        yc = yC[0]
                        r = scratch()
                        A(lambda e: e.activation(r.t[val, :], o.t[den, :], AF.Ln), [o.b()], [r.b()])
                        A(lambda e: e.activation(r.t[val, :], r.t[val, :], AF.Exp, scale=-1.0), [], [r.b()])
                        V(lambda e: e.tensor_tensor(yc.t[val, h // 2, :], o.t[val, :], r.t[val, :], ALU.mult), [o.b(), r.b()], [yc.b()])
                        if h == 5:
                            if "yC" in dbg and l == 0 and c == 3:
                                DBG("yC", yc.t[:], [128, 3, 512], BF16, [yc.b()])
                            out_proj(l, woC, 3, lambda p, yc=yc: yc.t[:, p, :], [yc.b()], c * 512, 512, 16, ring="m1")

        def ffn(l, gu_fn, dn_fn, comb=None, gT=None):
            with ExitStack() as gs:
                if gT is None:
                    gT = sb("gT", [128, NF, 1024], BF16, gs)
                for half in range(2):
                    for j in range(NF):
                        wt = wbuf()
                        wv = wt.t[:, 0:2048].rearrange("p (t k n) -> p t k n", t=2, k=8)
                        load("dpool", wt.t[:, 0:2048], gu_fn(j).rearrange("p t k n -> p (t k n)"), [wt.b()], cast=True)
                        for cc in range(2):
                            c = 2 * half + cc
                            cs = slice(c * 512, (c + 1) * 512)
                            pa, pu = ps("r4"), ps("r4")
                            for k in range(KC):
                                PE(lambda e, k=k: e.matmul(pa.t[:], wv[:, 0, k, :], hT.t[:, k, cs], start=(k == 0), stop=(k == KC - 1)),
                                   [wt.b(), hT.b(k, c)], [pa.b()])
                            for k in range(KC):
                                PE(lambda e, k=k: e.matmul(pu.t[:], wv[:, 1, k, :], hT.t[:, k, cs], start=(k == 0), stop=(k == KC - 1)),
                                   [wt.b(), hT.b(k, c)], [pu.b()])
                            sa = scratch()
                            A(lambda e: e.activation(sa.t[:], pa.t[:], AF.Silu), [pa.b()], [sa.b()])
                            V(lambda e, j=j, cc=cc: e.tensor_tensor(gT.t[:, j, cc * 512:(cc + 1) * 512], sa.t[:], pu.t[:], ALU.mult),
                              [sa.b(), pu.b()], [gT.b(j, cc)])
                    for d in range(KC):
                        wts = []
                        for hf in range(2):
                            wt = wbuf()
                            load("dpool", wt.t[:, 0:11 * 128], dn_fn(d, hf).rearrange("p j n -> p (j n)"), [wt.b()], cast=True)
                            wts.append(wt)
                        for cc in range(2):
                            c = 2 * half + cc
                            cs = slice(c * 512, (c + 1) * 512)
                            po = ps("r3")
                            for j in range(NF):
                                wt = wts[j // 11]
                                jj = j % 11
                                PE(lambda e, j=j, wt=wt, jj=jj: e.matmul(po.t[:], wt.t[:, jj * 128:(jj + 1) * 128], gT.t[:, j, cc * 512:(cc + 1) * 512],
                                                                          start=(j == 0), stop=(j == NF - 1)),
                                   [wt.b(), gT.b(j, cc)], [po.b()])
                            if comb is None:
                                V(lambda e, d=d: e.scalar_tensor_tensor(xT.t[:, d, cs], po.t[:], modc.t[:, l, 40 + d:41 + d], xT.t[:, d, cs],
                                                                        op0=ALU.mult, op1=ALU.add), [po.b(), modc.b()], [xT.b(d, c)])
                            else:
                                cb = comb(c)
                                tm = scratch()
                                V(lambda e, d=d, tm=tm: e.scalar_tensor_tensor(tm.t[:], po.t[:], modc.t[:, l, 40 + d:41 + d], cb.t[:, cs],
                                                                               op0=ALU.mult, op1=ALU.mult), [po.b(), modc.b(), cb.b(c)], [tm.b()])
                                V(lambda e, d=d, tm=tm: e.tensor_tensor(xT.t[:, d, cs], xT.t[:, d, cs], tm.t[:], ALU.add), [tm.b()], [xT.b(d, c)])

        def moe(l, j):
            with ExitStack() as gs:
                wr = sb("wrt", [128, 8, 8], F32, gs)
                ctm = sb("ctm", [128, NT, 8], F32, gs)
                combT = sb("combT", [8, SEQ], F32, gs)
                cbc = sb("cbc", [128, SEQ], F32, gs)
                selm = sb("selm", [8, NEXP * 128], F32, gs)
                m8 = sb("m8m", [128, 8], F32, gs)
                sm = sb("sm", [128, 8], F32, gs)
                load("dsync", wr.t[:], dr["wr"][j], [wr.b()])
                load("dsync", selm.t[:], dr["sel"][:, :], [selm.b()])
                lg = {}

                gTm = sb("gTm", [128, NF, 1024], BF16, gs)

                def hf_cb(c, k, hf):
                    if k == 0:
                        lg[c] = ps("acc")
                    pl = lg[c]
                    for tt in range(4):
                        PE(lambda e, tt=tt: e.matmul(pl.t[:, tt * 8:(tt + 1) * 8], hf.t[:, tt * 128:(tt + 1) * 128], wr.t[:, k, :],
                                                     start=(k == 0 and tt == 0), stop=(k == KC - 1), skip_group_check=True),
                           [hf.b(), wr.b()], [pl.b()], mode="f32")
                    if k == KC - 1:
                        for tt in range(4):
                            t = 4 * c + tt
                            lt_ = pl.t[:, tt * 8:(tt + 1) * 8]
                            V(lambda e, lt_=lt_: e.max(m8.t[:], lt_), [pl.b()], [m8.b()])
                            V(lambda e: e.tensor_scalar(sm.t[:, 0:1], m8.t[:, 0:1], -1.0, None, op0=ALU.mult), [m8.b()], [sm.b()])
                            A(lambda e, lt_=lt_, t=t: e.activation(ctm.t[:, t, :], lt_, AF.Exp, bias=sm.t[:, 0:1]), [pl.b(), sm.b()], [ctm.b(t)])
                            A(lambda e: e.activation(sm.t[:, 1:2], m8.t[:, 1:2], AF.Exp, bias=sm.t[:, 0:1]), [m8.b()], [sm.b()])
                            V(lambda e: e.tensor_scalar(sm.t[:, 2:3], sm.t[:, 1:2], 1.0, None, op0=ALU.add), [], [sm.b()])
                            V(lambda e: e.reciprocal(sm.t[:, 3:4], sm.t[:, 2:3]), [], [sm.b()])
                            V(lambda e, lt_=lt_: e.tensor_scalar(sm.t[:, 4:8], lt_[:, 0:4], m8.t[:, 1:2], None, op0=ALU.is_ge), [pl.b(), m8.b()], [sm.b()])
                            V(lambda e, lt_=lt_, t=t: e.scalar_tensor_tensor(ctm.t[:, t, 0:4], sm.t[:, 4:8], sm.t[:, 3:4], ctm.t[:, t, 0:4],
                                                                             op0=ALU.mult, op1=ALU.mult), [sm.b()], [ctm.b(t)])
                            V(lambda e, lt_=lt_: e.tensor_scalar(sm.t[:, 4:8], lt_[:, 4:8], m8.t[:, 1:2], None, op0=ALU.is_ge), [pl.b(), m8.b()], [sm.b()])
                            V(lambda e, lt_=lt_, t=t: e.scalar_tensor_tensor(ctm.t[:, t, 4:8], sm.t[:, 4:8], sm.t[:, 3:4], ctm.t[:, t, 4:8],
                                                                             op0=ALU.mult, op1=ALU.mult), [sm.b()], [ctm.b(t)])
                            tp = ps("r2")
                            PE(lambda e, t=t: e.transpose(tp.t[0:8, 0:128], ctm.t[:, t, :], identf.t[:]), [ctm.b(t), identf.b()], [tp.b()], mode="Tf")
                            A(lambda e, t=t: e.activation(combT.t[:, t * 128:(t + 1) * 128], tp.t[0:8, 0:128], AF.Copy), [tp.b()], [combT.b(t // 4)])

                norm_mod(scaleF.t[:, l, :], modc.t[:, l, 24:32], 1e-6, hf_cb=hf_cb)
                if "ctm" in dbg:
                    DBG("ctm", ctm.t[:], [128, NT, 8], F32, [ctm.b(t) for t in range(NT)])
                for ex in range(NEXP):
                    for c in range(NCH):
                        cs = slice(c * 512, (c + 1) * 512)
                        pb_ = ps("r2")
                        PE(lambda e, ex=ex: e.matmul(pb_.t[:], selm.t[:, ex * 128:(ex + 1) * 128], combT.t[:, cs], start=True, stop=True),
                           [selm.b(), combT.b(c)], [pb_.b()], mode="f32k8")
                        A(lambda e: e.activation(cbc.t[:, cs], pb_.t[:], AF.Copy), [pb_.b()], [cbc.b(c)])
                    ffn(l, lambda jf, ex=ex: dr["egu"][j, ex, jf], lambda d, hf, ex=ex: dr["edn"][j, ex, d, hf], comb=lambda c: cbc, gT=gTm)

        for l in range(nlayers):
            norm_mod(scaleA.t[:, l, :], modc.t[:, l, 0:8], 1e-6)
            if "hT" in dbg and l == 0:
                DBG("hT", hT.t[:], [128, KC, SEQ], BF16, hT_all())
            if stop == "norm":
                break
            group_A(l)
            if stop == "A":
                break
            S.barrier()
            group_B(l)
            if stop == "B":
                break
            S.barrier()
            group_C(l)
            if stop == "C":
                break
            S.barrier()
            if l % 2 == 0:
                norm_mod(scaleF.t[:, l, :], modc.t[:, l, 24:32], 1e-6)
                jd = l // 2
                ffn(l, lambda jf: dr["fgu"][jd, jf], lambda d, hf: dr["fdn"][jd, d, hf])
            else:
                moe(l, l // 2)
            S.barrier()
            if f"x{l}" in dbg:
                DBG(f"x{l}", xT.t[:], [128, KC, SEQ], F32, [xT.b(k, c) for k in range(KC) for c in range(NCH)])

        if stop is None:
            with ExitStack() as gs:
                of = sb("of", [128, KC, SEQ], F32, gs)
                norm_mod(gfin.t[:, :], None, 1e-6, out_f32=of)
                ov = out_d.rearrange("(k p) t -> p k t", p=128)
                for k in range(KC):
                    S.dma("dsync", lambda e, k=k: e.dma_start(out=ov[:, k, :], in_=of.t[:, k, :]),
                          reads=[of.b(k, c) for c in range(NCH)])
                S.finish()
        else:
            ov = out_d.rearrange("(k p) t -> p k t", p=128)
            for k in range(KC):
                S.dma("dsync", lambda e, k=k: e.dma_start(out=ov[:, k, :], in_=xT.t[:, k, :]),
                      reads=[xT.b(k, c) for c in range(NCH)])
            S.finish()
        print("instructions:", S.ninstr, {e: S.count[e] for e in COMPUTE})
    return nc, dbg_d


def make_in_maps(inputs):
    hl = host_layout(inputs)
    x = np.asarray(inputs["x"], np.float32)
    c = np.asarray(inputs["c"], np.float32)
    pos = np.asarray(inputs["positions"], np.int32)
    maps = []
    for b in range(8):
        m = dict(hl)
        m["xT"] = np.ascontiguousarray(x[b].T)
        m["ccol"] = np.ascontiguousarray(c[b].reshape(8, 128).T)
        m["pos"] = np.ascontiguousarray(pos[b:b + 1])
        maps.append(m)
    return maps


def shapes_of(m):
    sh = {}
    for k, v in m.items():
        sh[k] = (v.shape, I32 if v.dtype == np.int32 else F32)
    return sh


def kernel(**inputs):
    maps = make_in_maps(inputs)
    nc, _ = build(shapes_of(maps[0]))
    res = run_bass_kernel_spmd(nc, maps, core_ids=list(range(8)))
    out = np.stack([np.ascontiguousarray(res.results[b]["outT"].T) for b in range(8)])
    return out.astype(np.float32)
```
